# Optimizing a Trainium2 kernel written in Bass

```python
import math
import jax, jax.numpy as jnp
from jax import lax
import numpy as np

D_MODEL = 1024
BATCH = 8
SEQ = 2048
DEPTH = 4

GRID_W = 64
CTX_LEN = 256
MIX = D_MODEL
N_MIXERS = 4
W_GROUP = MIX // N_MIXERS
HEAD_DIM = 64
N_HEADS = W_GROUP // HEAD_DIM
ATT_KV_HEADS = 2
ATT_GROUPS = N_HEADS // ATT_KV_HEADS
WINDOW = 128
ATT_BLOCK = 128
CHUNK = 64
GLA_RANK = 16
GLA_NORMALIZER = 16.0
HYENA_BANDS = 16
HYENA_EMB = 1 + 2 * HYENA_BANDS
HYENA_FFN = 64
HYENA_MIN_DECAY = math.log(1e-2) / 1.5
HYENA_MAX_DECAY = math.log(1e-2) / 0.3
SHORT_CONV = 3
ROPE_BASE = 10000.0
ROPE_AXIS_FREQS = HEAD_DIM // 4
EPS = 1e-6

MLSTM_COLS = (W_GROUP, W_GROUP, W_GROUP, W_GROUP, W_GROUP, 4 * N_HEADS)
HYENA_COLS = (W_GROUP, W_GROUP, W_GROUP, W_GROUP)
GLA_COLS = (W_GROUP, W_GROUP, W_GROUP, W_GROUP, GLA_RANK, GLA_RANK)
ATT_COLS = (W_GROUP, ATT_KV_HEADS * HEAD_DIM, ATT_KV_HEADS * HEAD_DIM, W_GROUP)
GROUP_COLS = (sum(MLSTM_COLS), sum(HYENA_COLS), sum(GLA_COLS), sum(ATT_COLS))
N_IN = sum(GROUP_COLS)

kernel_name = "hybrid_parallel_heads_flow_backbone"

F32 = jnp.float32


def split_cols(p, sizes):
    out, start = [], 0
    for s in sizes:
        out.append(p[..., start:start + s])
        start += s
    return out


def rms_norm(x, g):
    x32 = x.astype(F32)
    y = x32 * lax.rsqrt(jnp.mean(x32 * x32, axis=-1, keepdims=True) + EPS)
    return (y * g.astype(F32)).astype(x.dtype)


def head_rms_norm(h, g):
    b_, l_, _ = h.shape
    hh = h.reshape(b_, l_, N_HEADS, HEAD_DIM)
    hh = hh * lax.rsqrt(jnp.mean(hh * hh, axis=-1, keepdims=True) + EPS)
    return hh.reshape(b_, l_, W_GROUP) * g.astype(F32)


def to_heads(a):
    b_, l_, _ = a.shape
    return a.reshape(b_, l_, -1, HEAD_DIM).transpose(0, 2, 1, 3)


def from_heads(h):
    b_, h_, l_, d_ = h.shape
    return h.transpose(0, 2, 1, 3).reshape(b_, l_, h_ * d_)


def to_chunks(a):
    nc = a.shape[2] // CHUNK
    return jnp.moveaxis(a.reshape(a.shape[:2] + (nc, CHUNK) + a.shape[3:]), 2, 0)


def from_chunks(h):
    h = jnp.moveaxis(h, 0, 2)
    return h.reshape(h.shape[:2] + (h.shape[2] * h.shape[3],) + h.shape[4:])


def mlstm_chunks(q, k, v, i_pre, f_pre, state):
    tri = jnp.tril(jnp.ones((CHUNK, CHUNK), bool))

    def step(carry, inp):
        c_prev, n_prev, m_prev = carry
        qb, kb, vb, ib, fb = inp
        b = jnp.cumsum(jax.nn.log_sigmoid(fb), axis=-1)
        d = jnp.where(tri, b[..., :, None] - b[..., None, :] + ib[..., None, :], -jnp.inf)
        inter = b + m_prev[..., None]
        m = jnp.maximum(inter, jnp.max(d, axis=-1))
        w_intra = jnp.exp(d - m[..., None])
        w_inter = jnp.exp(inter - m)
        s = jnp.einsum('bhtd,bhsd->bhts', qb, kb) * w_intra
        num = w_inter[..., None] * jnp.einsum('bhtd,bhde->bhte', qb, c_prev) + jnp.einsum('bhts,bhse->bhte', s, vb)
        den = w_inter * jnp.einsum('bhtd,bhd->bht', qb, n_prev) + jnp.sum(s, axis=-1)
        h = num / jnp.maximum(jnp.abs(den), jnp.exp(-m))[..., None]
        b_end = b[..., -1]
        g = b_end[..., None] - b + ib
        m_new = jnp.maximum(b_end + m_prev, jnp.max(g, axis=-1))
        a_prev = jnp.exp(b_end + m_prev - m_new)
        w_s = jnp.exp(g - m_new[..., None])
        c_new = a_prev[..., None, None] * c_prev + jnp.einsum('bhs,bhsd,bhse->bhde', w_s, kb, vb)
        n_new = a_prev[..., None] * n_prev + jnp.einsum('bhs,bhsd->bhd', w_s, kb)
        return (c_new, n_new, m_new), h

    state, h = lax.scan(step, state, tuple(to_chunks(a) for a in (q, k, v, i_pre, f_pre)))
    return from_chunks(h), state


def gla_chunks(q, k, v, log_a, state):
    tri = jnp.tril(jnp.ones((CHUNK, CHUNK), bool))[:, :, None]

    def step(s_prev, inp):
        qb, kb, vb, ab = inp
        b = jnp.cumsum(ab, axis=2)
        o_inter = jnp.einsum('bhtd,bhde->bhte', qb * jnp.exp(b), s_prev)
        decay = jnp.exp(jnp.where(tri, b[:, :, :, None, :] - b[:, :, None, :, :], -jnp.inf))
        att = jnp.einsum('bhtd,bhsd,bhtsd->bhts', qb, kb, decay)
        o = o_inter + jnp.einsum('bhts,bhse->bhte', att, vb)
        b_end = b[:, :, -1]
        s_new = jnp.exp(b_end)[..., None] * s_prev + jnp.einsum(
            'bhsd,bhse->bhde', kb * jnp.exp(b_end[:, :, None, :] - b), vb)
        return s_new, o

    state, o = lax.scan(step, state, tuple(to_chunks(a) for a in (q, k, v, log_a)))
    return from_chunks(o), state


def run_direction(scan_fn, ctx_in, lat_in, state0, reverse):
    if reverse:
        ctx_in = tuple(jnp.flip(a, axis=2) for a in ctx_in)
        lat_in = tuple(jnp.flip(a, axis=2) for a in lat_in)
    h_c, state = scan_fn(*ctx_in, state0)
    h_x, _ = scan_fn(*lat_in, state)
    if reverse:
        h_c, h_x = jnp.flip(h_c, axis=2), jnp.flip(h_x, axis=2)
    return h_c, h_x


def mlstm_branch(pc, px, gate_b, norm_g):
    def prep(p):
        q, k, v, o, gate, gts = split_cols(p, MLSTM_COLS)
        b_, l_, _ = p.shape
        gts = gts.astype(F32).reshape(b_, l_, 4, N_HEADS) + gate_b.astype(F32)
        gts = jnp.transpose(gts, (2, 0, 3, 1))
        qkv = (to_heads(q).astype(F32), to_heads(k).astype(F32) * HEAD_DIM ** -0.5, to_heads(v).astype(F32))
        return qkv, gts, o, gate

    qkv_c, gts_c, o_c, gate_c = prep(pc)
    qkv_x, gts_x, o_x, gate_x = prep(px)
    b_ = px.shape[0]
    state0 = (jnp.zeros((b_, N_HEADS, HEAD_DIM, HEAD_DIM), F32), jnp.zeros((b_, N_HEADS, HEAD_DIM), F32),
              jnp.zeros((b_, N_HEADS), F32))
    hc_f, hx_f = run_direction(mlstm_chunks, qkv_c + (gts_c[0], gts_c[1]), qkv_x + (gts_x[0], gts_x[1]), state0, False)
    hc_b, hx_b = run_direction(mlstm_chunks, qkv_c + (gts_c[2], gts_c[3]), qkv_x + (gts_x[2], gts_x[3]), state0, True)

    def finish(h, o, gate, dtype):
        h = from_heads(h) * jax.nn.sigmoid(o.astype(F32))
        return (head_rms_norm(h, norm_g) * jax.nn.silu(gate.astype(F32))).astype(dtype)

    return finish(hc_f + hc_b, o_c, gate_c, pc.dtype), finish(hx_f + hx_b, o_x, gate_x, px.dtype)


def gla_branch(pc, px, w_alpha, b_alpha, norm_g):
    def prep(p):
        q, k, v, gate, r_f, r_b = split_cols(p, GLA_COLS)
        qkv = (to_heads(q).astype(F32) * HEAD_DIM ** -0.5, to_heads(k).astype(F32), to_heads(v).astype(F32))
        log_a = [to_heads(jax.nn.log_sigmoid((r @ w_alpha[d] + b_alpha[d]).astype(F32)) / GLA_NORMALIZER)
                 for d, r in enumerate((r_f, r_b))]
        return qkv, log_a, gate

    qkv_c, la_c, gate_c = prep(pc)
    qkv_x, la_x, gate_x = prep(px)
    state0 = jnp.zeros((px.shape[0], N_HEADS, HEAD_DIM, HEAD_DIM), F32)
    hc_f, hx_f = run_direction(gla_chunks, qkv_c + (la_c[0],), qkv_x + (la_x[0],), state0, False)
    hc_b, hx_b = run_direction(gla_chunks, qkv_c + (la_c[1],), qkv_x + (la_x[1],), state0, True)

    def finish(h, gate, dtype):
        return (head_rms_norm(from_heads(h), norm_g) * jax.nn.silu(gate.astype(F32))).astype(dtype)

    return finish(hc_f + hc_b, gate_c, pc.dtype), finish(hx_f + hx_b, gate_x, px.dtype)


def short_conv(u, w, b):
    l_ = u.shape[1]
    up = jnp.pad(u, ((0, 0), (SHORT_CONV // 2, SHORT_CONV // 2), (0, 0)))
    return sum(w[j] * up[:, j:j + l_] for j in range(SHORT_CONV)) + b


def hyena_filter_spectrum(l_, w1, b1, w2, b2, w3, b3, freq):
    pos = jnp.arange(l_, dtype=F32)
    t = pos / max(l_ - 1, 1)
    w = 2.0 * math.pi * pos / l_
    f = jnp.linspace(1e-4, HYENA_BANDS - 1, HYENA_BANDS, dtype=F32)
    z = jnp.concatenate([t[:, None], jnp.cos(w[:, None] * f), -jnp.sin(w[:, None] * f)], axis=-1)
    h = jnp.sin(freq[0].astype(F32) * (z @ w1.astype(F32) + b1.astype(F32)))
    h = jnp.sin(freq[1].astype(F32) * (h @ w2.astype(F32) + b2.astype(F32)))
    h = (h @ w3.astype(F32) + b3.astype(F32)).reshape(l_, 2, W_GROUP)
    deltas = jnp.abs(jnp.linspace(HYENA_MIN_DECAY, HYENA_MAX_DECAY, W_GROUP, dtype=F32))
    h = h * jnp.exp(-t[:, None, None] * deltas)
    kernel = jnp.concatenate([h[:, 0], jnp.zeros((1, W_GROUP), F32), h[:0:-1, 1]], axis=0)
    return jnp.fft.rfft(kernel, n=2 * l_, axis=0)


def fftconv(u, k_f, d):
    l_ = u.shape[1]
    u32 = u.astype(F32)
    y = jnp.fft.irfft(jnp.fft.rfft(u32, n=2 * l_, axis=1) * k_f, n=2 * l_, axis=1)[:, :l_]
    return y + u32 * d.astype(F32)


def hyena_branch(p, conv_w, conv_b, w1, b1, w2, b2, w3, b3, freq, d):
    v, x1, x0, gate = split_cols(p, HYENA_COLS)
    u = short_conv(jnp.concatenate([v, x1, x0], axis=-1), conv_w, conv_b)
    v, x1, x0 = split_cols(u, (W_GROUP, W_GROUP, W_GROUP))
    k_f = hyena_filter_spectrum(p.shape[1], w1, b1, w2, b2, w3, b3, freq)
    y = x0.astype(F32) * fftconv(v * x1, k_f, d)
    return (y * jax.nn.silu(gate.astype(F32))).astype(p.dtype)


def rope_2d(x):
    l_ = x.shape[1]
    rows = l_ // GRID_W
    row = jnp.repeat(jnp.arange(rows, dtype=F32), GRID_W)
    col = jnp.tile(jnp.arange(GRID_W, dtype=F32), rows)
    inv = ROPE_BASE ** (-jnp.arange(ROPE_AXIS_FREQS, dtype=F32) / ROPE_AXIS_FREQS)
    ang = jnp.concatenate([row[:, None] * inv, col[:, None] * inv], axis=-1)[:, None, :]
    cos, sin = jnp.cos(ang), jnp.sin(ang)
    x32 = x.astype(F32)
    x1, x2 = x32[..., :HEAD_DIM // 2], x32[..., HEAD_DIM // 2:]
    return jnp.concatenate([x1 * cos - x2 * sin, x2 * cos + x1 * sin], axis=-1).astype(x.dtype)


def attn_branch(pc, px, sink, need_ctx_out):
    b_, l_, _ = px.shape
    lc = pc.shape[1]
    q, k, v, gate = split_cols(px, ATT_COLS)
    q_c, k_c, v_c, gate_c = split_cols(pc, ATT_COLS)
    scale = HEAD_DIM ** -0.5
    q = rope_2d(q.reshape(b_, l_, N_HEADS, HEAD_DIM))
    k = rope_2d(k.reshape(b_, l_, ATT_KV_HEADS, HEAD_DIM))
    v = v.reshape(b_, l_, ATT_KV_HEADS, HEAD_DIM)
    kc = k_c.reshape(b_, lc, ATT_KV_HEADS, HEAD_DIM)
    vc = v_c.reshape(b_, lc, ATT_KV_HEADS, HEAD_DIM).astype(F32)
    sink_g = sink.astype(F32).reshape(ATT_KV_HEADS, ATT_GROUPS)

    nb = l_ // ATT_BLOCK
    qb = q.reshape(b_, nb, ATT_BLOCK, ATT_KV_HEADS, ATT_GROUPS, HEAD_DIM)

    def band(a):
        ap = jnp.pad(a, ((0, 0), (WINDOW, WINDOW), (0, 0), (0, 0))).reshape(b_, nb + 2, ATT_BLOCK, ATT_KV_HEADS, HEAD_DIM)
        return jnp.concatenate([ap[:, :-2], ap[:, 1:-1], ap[:, 2:]], axis=2)

    kb, vb = band(k), band(v).astype(F32)
    qi = jnp.arange(ATT_BLOCK)
    si = jnp.arange(3 * ATT_BLOCK)
    blk = jnp.arange(nb)
    rel = si[None, :] - ATT_BLOCK - qi[:, None]
    key_pos = blk[:, None] * ATT_BLOCK - ATT_BLOCK + si[None, :]
    mask = (jnp.abs(rel) <= WINDOW)[None] & ((key_pos >= 0) & (key_pos < l_))[:, None, :]
    s_loc = jnp.einsum('bnqkgd,bnskd->bnkgqs', qb, kb).astype(F32) * scale
    s_loc = jnp.where(mask[None, :, None, None], s_loc, -jnp.inf)
    s_ctx = jnp.einsum('bnqkgd,bckd->bnkgqc', qb, kc).astype(F32) * scale
    s_sink = jnp.broadcast_to(sink_g[None, None, :, :, None, None], s_loc.shape[:-1] + (1,))
    p = jax.nn.softmax(jnp.concatenate([s_loc, s_ctx, s_sink], axis=-1), axis=-1)
    o = jnp.einsum('bnkgqs,bnskd->bnqkgd', p[..., :3 * ATT_BLOCK], vb) + jnp.einsum(
        'bnkgqc,bckd->bnqkgd', p[..., 3 * ATT_BLOCK:3 * ATT_BLOCK + lc], vc)
    o = o.reshape(b_, l_, W_GROUP) * jax.nn.silu(gate.astype(F32))
    out_x = o.astype(px.dtype)

    out_c = None
    if need_ctx_out:
        qc = q_c.reshape(b_, lc, ATT_KV_HEADS, ATT_GROUPS, HEAD_DIM)
        s = jnp.einsum('bqkgd,bckd->bkgqc', qc, kc).astype(F32) * scale
        s_sink_c = jnp.broadcast_to(sink_g[None, :, :, None, None], s.shape[:-1] + (1,))
        pcx = jax.nn.softmax(jnp.concatenate([s, s_sink_c], axis=-1), axis=-1)
        oc = jnp.einsum('bkgqc,bckd->bqkgd', pcx[..., :lc], vc).reshape(b_, lc, W_GROUP)
        out_c = (oc * jax.nn.silu(gate_c.astype(F32))).astype(pc.dtype)
    return out_c, out_x


def setup_inputs(seed: int = 0) -> dict:
    key = jax.random.key(seed)
    ks = jax.random.split(key, 32)

    def nrm(k, shape, s):
        return jax.random.normal(k, shape, jnp.float32) * s

    f_bias = jnp.linspace(3.0, 6.0, N_HEADS, dtype=jnp.float32)
    mlstm_gate_b = jnp.stack([nrm(ks[9], (DEPTH, N_HEADS), 0.1),
                              f_bias + nrm(ks[10], (DEPTH, N_HEADS), 0.1),
                              nrm(ks[11], (DEPTH, N_HEADS), 0.1),
                              f_bias + nrm(ks[12], (DEPTH, N_HEADS), 0.1)], axis=1)
    return {
        "x": nrm(ks[0], (BATCH, SEQ, D_MODEL), 1.0),
        "c": nrm(ks[1], (BATCH, D_MODEL), 1.0),
        "ctx": nrm(ks[2], (BATCH, CTX_LEN, D_MODEL), 1.0),
        "c_ctx": nrm(ks[3], (D_MODEL,), 1.0),
        "w_ada": nrm(ks[4], (DEPTH, D_MODEL, 3 * D_MODEL), 0.5 * D_MODEL ** -0.5),
        "b_ada": nrm(ks[5], (DEPTH, 3 * D_MODEL), 0.02),
        "g_pre": 1.0 + nrm(ks[6], (DEPTH, D_MODEL), 0.05),
        "g_post": 1.0 + nrm(ks[7], (DEPTH, D_MODEL), 0.05),
        "w_in": nrm(ks[8], (DEPTH, D_MODEL, N_IN), D_MODEL ** -0.5),
        "mlstm_gate_b": mlstm_gate_b,
        "mlstm_norm_g": 1.0 + nrm(ks[13], (DEPTH, W_GROUP), 0.05),
        "hyena_conv_w": nrm(ks[14], (DEPTH, SHORT_CONV, 3 * W_GROUP), 0.5),
        "hyena_conv_b": nrm(ks[15], (DEPTH, 3 * W_GROUP), 0.02),
        "hyena_w1": nrm(ks[16], (DEPTH, HYENA_EMB, HYENA_FFN), HYENA_EMB ** -0.5),
        "hyena_b1": nrm(ks[17], (DEPTH, HYENA_FFN), 0.1),
        "hyena_w2": nrm(ks[18], (DEPTH, HYENA_FFN, HYENA_FFN), HYENA_FFN ** -0.5),
        "hyena_b2": nrm(ks[19], (DEPTH, HYENA_FFN), 0.1),
        "hyena_w3": nrm(ks[20], (DEPTH, HYENA_FFN, 2 * W_GROUP), 0.02),
        "hyena_b3": nrm(ks[21], (DEPTH, 2 * W_GROUP), 0.01),
        "hyena_freq": 1.0 + nrm(ks[22], (DEPTH, 2, HYENA_FFN), 0.1),
        "hyena_d": nrm(ks[23], (DEPTH, W_GROUP), 0.5),
        "gla_w_alpha": nrm(ks[24], (DEPTH, 2, GLA_RANK, W_GROUP), GLA_RANK ** -0.5),
        "gla_b_alpha": nrm(ks[25], (DEPTH, 2, W_GROUP), 0.1),
        "gla_norm_g": 1.0 + nrm(ks[26], (DEPTH, W_GROUP), 0.05),
        "attn_sink": nrm(ks[27], (DEPTH, N_HEADS), 0.5),
        "w_out": nrm(ks[28], (DEPTH, MIX, D_MODEL), MIX ** -0.5),
    }


def reference(x, c, ctx, c_ctx, w_ada, b_ada, g_pre, g_post, w_in, mlstm_gate_b, mlstm_norm_g,
              hyena_conv_w, hyena_conv_b, hyena_w1, hyena_b1, hyena_w2, hyena_b2, hyena_w3, hyena_b3,
              hyena_freq, hyena_d, gla_w_alpha, gla_b_alpha, gla_norm_g, attn_sink, w_out):
    for l in range(DEPTH):
        need_ctx_out = l < DEPTH - 1
        mod_x = jax.nn.silu(c) @ w_ada[l] + b_ada[l]
        mod_c = jax.nn.silu(c_ctx) @ w_ada[l] + b_ada[l]
        sh_x, sc_x, gt_x = split_cols(mod_x[:, None, :], (D_MODEL, D_MODEL, D_MODEL))
        sh_c, sc_c, gt_c = split_cols(mod_c, (D_MODEL, D_MODEL, D_MODEL))
        hx = rms_norm(x, g_pre[l]) * (1.0 + sc_x) + sh_x
        hc = rms_norm(ctx, g_pre[l]) * (1.0 + sc_c) + sh_c
        px_a, px_h, px_g, px_d = split_cols(hx @ w_in[l], GROUP_COLS)
        pc_a, pc_h, pc_g, pc_d = split_cols(hc @ w_in[l], GROUP_COLS)

        a_c, a_x = mlstm_branch(pc_a, px_a, mlstm_gate_b[l], mlstm_norm_g[l])
        hy_args = (hyena_conv_w[l], hyena_conv_b[l], hyena_w1[l], hyena_b1[l], hyena_w2[l], hyena_b2[l],
                   hyena_w3[l], hyena_b3[l], hyena_freq[l], hyena_d[l])
        h_x = hyena_branch(px_h, *hy_args)
        g_c, g_x = gla_branch(pc_g, px_g, gla_w_alpha[l], gla_b_alpha[l], gla_norm_g[l])
        d_c, d_x = attn_branch(pc_d, px_d, attn_sink[l], need_ctx_out)

        y_x = jnp.concatenate([a_x, h_x, g_x, d_x], axis=-1) @ w_out[l]
        x = x + gt_x * rms_norm(y_x, g_post[l])
        if need_ctx_out:
            h_c = hyena_branch(pc_h, *hy_args)
            y_c = jnp.concatenate([a_c, h_c, g_c, d_c], axis=-1) @ w_out[l]
            ctx = ctx + gt_c * rms_norm(y_c, g_post[l])
    return x
```

```python
import numpy as np
from contextlib import ExitStack
import concourse.bass as bass
import concourse.mybir as mybir
from concourse.bass_utils import run_bass_kernel_spmd

F32 = mybir.dt.float32
BF16 = mybir.dt.bfloat16
AF = mybir.ActivationFunctionType
ALU = mybir.AluOpType
AX = mybir.AxisListType

ND_SEMS = 40
VERBOSE = False
STOP = None


class Buf:
    def __init__(self, name, ap=None):
        self.name = name
        self.ap = ap
        self.writer = None
        self.readers = {}
        self.excl = False

    def __getitem__(self, k):
        return self.ap[k]


class Prog:
    def __init__(self, nc, es):
        self.nc = nc
        self.es = es
        self.engs = ['pe', 'act', 'dve', 'pool', 'sp']
        self.lists = {e: [] for e in self.engs}
        self.cnt = {e: 0 for e in self.engs}
        self.sem = {e: es.enter_context(nc.semaphore("s_" + e)) for e in ['pe', 'act', 'dve', 'pool']}
        self.dsem = [es.enter_context(nc.semaphore("d%d" % i)) for i in range(ND_SEMS)]
        self.dcnt = [0] * ND_SEMS
        self.dnext = 0
        self.dnext_sw = 0
        self.waited = {e: {} for e in self.engs}
        self.out_tokens = []
        self.nbuf = 0
        self.dead = False

    def sb(self, name, shape, dtype):
        self.nbuf += 1
        name = "%s_u%d" % (name, self.nbuf)
        t = self.es.enter_context(self.nc.sbuf_tensor(name, shape, dtype))
        assert self.nc.sbuf_bytes_remaining >= 16384 + 256, (name, self.nc.sbuf_bytes_remaining)
        return Buf(name, t)

    def ps(self, name, shape, dtype):
        t = self.es.enter_context(self.nc.psum_tensor(name, shape, dtype))
        b = Buf(name, t)
        b.excl = True
        return b

    def vb(self, name, ap=None):
        return Buf(name, ap)

    def _collect(self, eng, reads, writes):
        deps = []
        for b in reads:
            if b.writer is not None:
                deps.append(b.writer)
        for b in writes:
            if b.writer is not None:
                deps.append(b.writer)
            deps.extend(b.readers.values())
        waits = []
        w = self.waited[eng]
        for tok in deps:
            if tok[0] == 'e':
                if tok[1] == 'pe' and eng == 'pe':
                    continue
                key = tok[1]
                sem = self.sem[tok[1]]
            else:
                key = ('d', tok[1])
                sem = self.dsem[tok[1]]
            if w.get(key, 0) >= tok[2]:
                continue
            w[key] = tok[2]
            waits.append((sem, tok[2], key))
        best = {}
        for sem, val, key in waits:
            if key not in best or best[key][1] < val:
                best[key] = (sem, val)
        return list(best.values())

    def _mark(self, tok, rkey, reads, writes):
        for b in reads:
            b.readers[rkey] = tok
        for b in writes:
            b.writer = tok
            b.readers = {}

    def ckpt(self, name):
        if STOP is not None and STOP == name:
            self.dead = True

    def op(self, eng, fn, reads=(), writes=()):
        if self.dead:
            return
        if eng != 'pe':
            ex = [b for b in reads if b.excl]
            if ex:
                writes = list(writes) + [b for b in ex if b not in writes]
        waits = self._collect(eng, reads, writes)
        self.cnt[eng] += 1
        tok = ('e', eng, self.cnt[eng])
        self.lists[eng].append((waits, fn, ('inc', self.sem[eng])))
        self._mark(tok, eng, reads, writes)

    def dma(self, q, out_ap, in_ap, reads=(), writes=(), out=False, **kw):
        if self.dead and not out:
            return
        waits = self._collect(q, reads, writes)
        if q == 'pool':
            i = ND_SEMS - 8 + self.dnext_sw
            self.dnext_sw = (self.dnext_sw + 1) % 8
        else:
            i = self.dnext
            self.dnext = (self.dnext + 1) % (ND_SEMS - 8)
        if self.dcnt[i] > 0:
            key = ('d', i)
            val = self.dcnt[i] * 16
            if self.waited[q].get(key, 0) < val:
                self.waited[q][key] = val
                waits.append((self.dsem[i], val))
        self.dcnt[i] += 1
        tok = ('d', i, self.dcnt[i] * 16)
        fn = lambda e, o=out_ap, s=in_ap, k=kw: e.dma_start(out=o, in_=s, **k)
        self.lists[q].append((waits, fn, ('dinc', self.dsem[i])))
        self._mark(tok, ('d', i), reads, writes)
        if out:
            self.out_tokens.append(tok)

    def finish(self):
        waits = []
        for tok in self.out_tokens:
            waits.append((self.dsem[tok[1]], tok[2]))
        self.lists['sp'].append((waits, None, None))
        nc = self.nc
        lists = self.lists

        def replay(name, e):
            for waits, fn, inc in lists[name]:
                for sem, val in waits:
                    e.wait_ge(sem, val)
                if fn is None:
                    continue
                ins = fn(e)
                if inc[0] == 'inc':
                    ins.then_inc(inc[1], 1)
                else:
                    ins.then_inc(inc[1], 16)

        with nc.Block() as block:
            @block.sync
            def _(e):
                replay('sp', e)

            @block.tensor
            def _(e):
                replay('pe', e)

            @block.scalar
            def _(e):
                replay('act', e)

            @block.vector
            def _(e):
                replay('dve', e)

            @block.gpsimd
            def _(e):
                replay('pool', e)


    def mm(self, out, lhsT, rhs, start, stop, reads, writes):
        self.op('pe', lambda e: e.matmul(out, lhsT, rhs, start=start, stop=stop), reads, writes)

    def tr(self, out, in_, ident, reads, writes):
        self.op('pe', lambda e: e.transpose(out, in_, ident), reads, writes)

    def act(self, out, in_, func, reads, writes, **kw):
        self.op('act', lambda e: e.activation(out, in_, func, **kw), reads, writes)

    def tt(self, eng, out, in0, in1, op, reads, writes):
        self.op(eng, lambda e: e.tensor_tensor(out, in0, in1, op), reads, writes)

    def ts(self, eng, out, in0, s1, s2, op0, op1, reads, writes):
        self.op(eng, lambda e: e.tensor_scalar(out, in0, s1, s2, op0, op1), reads, writes)

    def stt(self, out, in0, scalar, in1, op0, op1, reads, writes):
        self.op('dve', lambda e: e.scalar_tensor_tensor(out, in0, scalar, in1, op0, op1), reads, writes)

    def cp(self, eng, out, in_, reads, writes):
        if eng == 'act':
            self.op('act', lambda e: e.copy(out, in_), reads, writes)
        else:
            self.op(eng, lambda e: e.tensor_copy(out, in_), reads, writes)

    def fence(self):
        for e in self.engs:
            waits = []
            for o in ['pe', 'act', 'dve', 'pool']:
                if not (o == e == 'pe') and self.cnt[o] > 0 and self.waited[e].get(o, 0) < self.cnt[o]:
                    self.waited[e][o] = self.cnt[o]
                    waits.append((self.sem[o], self.cnt[o]))
            for i in range(ND_SEMS):
                if self.dcnt[i] > 0 and self.waited[e].get(('d', i), 0) < self.dcnt[i] * 16:
                    self.waited[e][('d', i)] = self.dcnt[i] * 16
                    waits.append((self.dsem[i], self.dcnt[i] * 16))
            self.lists[e].append((waits, None, None))


D = 1024
NT = 18
TOK = NT * 128
EPS = 1e-6
N_IN = 4144
COL_A, COL_H, COL_G, COL_D = 0, 1296, 2320, 3376


def host_consts():
    c = {}
    pos = np.arange(2048)
    row = (pos // 64).astype(np.float64)
    col = (pos % 64).astype(np.float64)
    inv = 10000.0 ** (-np.arange(16, dtype=np.float64) / 16)
    ang = np.concatenate([row[:, None] * inv, col[:, None] * inv], -1)
    c["ropec"] = np.ascontiguousarray(np.cos(ang).reshape(16, 128, 32).transpose(1, 0, 2)).astype(np.float32)
    c["ropes"] = np.ascontiguousarray(np.sin(ang).reshape(16, 128, 32).transpose(1, 0, 2)).astype(np.float32)
    t = np.arange(128)[:, None]
    s_ = np.arange(128)[None, :]
    m = np.zeros((128, 384), np.float32)
    m[:, 0:128] = np.where(s_ >= t, 0.0, -30000.0)
    m[:, 256:384] = np.where(s_ <= t, 0.0, -30000.0)
    c["amask"] = m
    import ml_dtypes
    bf = ml_dtypes.bfloat16
    for tag, Lh in (("l", 2048), ("c", 256)):
        pos = np.arange(Lh, dtype=np.float32)
        t = (pos / np.float32(max(Lh - 1, 1))).astype(np.float32)
        w = (np.float32(2.0 * np.pi) * pos / np.float32(Lh)).astype(np.float32)
        f = np.linspace(1e-4, 15, 16, dtype=np.float32)
        z = np.concatenate([t[:, None], np.cos(w[:, None] * f), -np.sin(w[:, None] * f)], -1).astype(np.float32)
        c["zT_" + tag] = np.ascontiguousarray(z.T)
        nt_ = Lh // 128
        c["ntcol_" + tag] = np.ascontiguousarray((-t).reshape(nt_, 128).T)
        N = 2 * Lh - 1
        idx = (np.outer(np.arange(Lh, dtype=np.int64), np.arange(Lh, dtype=np.int64)) % N).astype(np.float64)
        ang = 2.0 * np.pi * idx / N
        C = np.cos(ang)
        S = np.sin(ang)
        c["Cn_" + tag] = np.ascontiguousarray(C.astype(np.float32).astype(bf))
        c["Sn_" + tag] = np.ascontiguousarray(S.astype(np.float32).astype(bf))
        c["Ct_" + tag] = np.ascontiguousarray(C.reshape(nt_, 128, nt_, 128).transpose(2, 1, 0, 3).astype(np.float32).astype(bf))
        c["St_" + tag] = np.ascontiguousarray(S.reshape(nt_, 128, nt_, 128).transpose(2, 1, 0, 3).astype(np.float32).astype(bf))
        wf = np.full(Lh, 2.0 / N, np.float32)
        wf[0] = 1.0 / N
        c["wf_" + tag] = np.ascontiguousarray(wf.reshape(nt_, 128).T)
    import math
    dmin, dmax = math.log(1e-2) / 1.5, math.log(1e-2) / 0.3
    c["deltas"] = np.abs(np.linspace(dmin, dmax, 256, dtype=np.float32)).reshape(1, 256).astype(np.float32)
    return c


def build(n_layers=4, tap=None, branches=("att", "mlstm", "gla", "hyena")):
    nc = bass.Bass("TRN2", target_bir_lowering=False)

    def din(name, shape, dt=F32):
        return nc.dram_tensor(name, shape, dt, kind="ExternalInput").ap()

    x_in = din("x", [2048, D])
    ctx_in = din("ctx", [256, D])
    cc_in = din("cc", [16, 128])
    w_ada = din("w_ada", [4, D, 3 * D])
    b_ada = din("b_ada", [4, 3 * D])
    g_pre = din("g_pre", [4, D])
    g_post = din("g_post", [4, D])
    w_in = din("w_in", [4, D, N_IN])
    w_out = din("w_out", [4, D, D])
    mlstm_gate_b = din("mlstm_gate_b", [4, 16])
    mlstm_norm_g = din("mlstm_norm_g", [4, 256])
    gla_norm_g = din("gla_norm_g", [4, 256])
    gla_w_alpha = din("gla_w_alpha", [4, 2, 16, 256])
    gla_b_alpha = din("gla_b_alpha", [4, 2, 256])
    attn_sink = din("attn_sink", [4, 4])
    ropec_in = din("ropec", [128, 16, 32])
    ropes_in = din("ropes", [128, 16, 32])
    amask_in = din("amask", [128, 384])
    hy = {}
    for nm, shp in (("hyena_conv_w", [4, 3, 768]), ("hyena_conv_b", [4, 768]), ("hyena_w1", [4, 33, 64]),
                    ("hyena_b1", [4, 64]), ("hyena_w2", [4, 64, 64]), ("hyena_b2", [4, 64]), ("hyena_w3", [4, 64, 512]),
                    ("hyena_b3", [4, 512]), ("hyena_freq", [4, 2, 64]), ("hyena_d", [4, 256]), ("deltas", [1, 256])):
        hy[nm] = din(nm, shp)
    for tag, Lh in (("l", 2048), ("c", 256)):
        nt_ = Lh // 128
        hy["zT_" + tag] = din("zT_" + tag, [33, Lh])
        hy["ntcol_" + tag] = din("ntcol_" + tag, [128, nt_])
        hy["wf_" + tag] = din("wf_" + tag, [128, nt_])
        hy["Cn_" + tag] = din("Cn_" + tag, [Lh, Lh], BF16)
        hy["Sn_" + tag] = din("Sn_" + tag, [Lh, Lh], BF16)
        hy["Ct_" + tag] = din("Ct_" + tag, [nt_, 128, nt_, 128], BF16)
        hy["St_" + tag] = din("St_" + tag, [nt_, 128, nt_, 128], BF16)
    y_out = nc.dram_tensor("y", [2048, D], F32, kind="ExternalOutput").ap()
    ctxs = nc.dram_tensor("ctxs", [256, D], F32).ap()
    tap_out = None
    if tap is not None:
        tap_out = nc.dram_tensor("tap", [128, 8, TOK], BF16, kind="ExternalOutput").ap()

    w_in_v = [w_in[l].rearrange("(kc p) n -> p kc n", p=128) for l in range(4)]
    w_out_v = [w_out[l].rearrange("(kc p) n -> p kc n", p=128) for l in range(4)]
    w_ada_v = [w_ada[l].rearrange("(kc p) n -> p kc n", p=128) for l in range(4)]

    with ExitStack() as es:
        P = Prog(nc, es)
        ident_bf = P.sb("ident_bf", [128, 128], BF16)
        ident_f = P.sb("ident_f", [128, 128], F32)
        tri_le = P.sb("tri_le", [128, 128], F32)
        tri_ge = P.sb("tri_ge", [128, 128], F32)
        tri_gt = P.sb("tri_gt", [128, 128], F32)
        tri_lt = P.sb("tri_lt", [128, 128], F32)
        ropec = P.sb("ropec_sb", [128, 16, 32], F32)
        ropes = P.sb("ropes_sb", [128, 16, 32], F32)
        amask = P.sb("amask_sb", [128, 384], F32)

        def mk_affine(buf, pattern, cm, cmp_op, fill_in=1.0):
            P.op('pool', lambda e: e.memset(buf.ap[:], fill_in), writes=[buf])
            P.op('pool', lambda e: e.affine_select(buf.ap[:], buf.ap[:], pattern=pattern, compare_op=cmp_op,
                                                   fill=0.0, base=0, channel_multiplier=cm), reads=[buf], writes=[buf])

        mk_affine(ident_bf, [[-1, 128]], 1, ALU.is_equal)
        mk_affine(ident_f, [[-1, 128]], 1, ALU.is_equal)
        mk_affine(tri_le, [[1, 128]], -1, ALU.is_ge)
        mk_affine(tri_ge, [[-1, 128]], 1, ALU.is_ge)
        mk_affine(tri_gt, [[-1, 128]], 1, ALU.is_gt)
        mk_affine(tri_lt, [[1, 128]], -1, ALU.is_gt)
        P.dma('sp', ropec.ap[:], ropec_in, writes=[ropec])
        P.dma('sp', ropes.ap[:], ropes_in, writes=[ropes])
        P.dma('sp', amask.ap[:], amask_in, writes=[amask])

        pT = [P.ps("pT%d" % i, [128, 1024], BF16) for i in range(2)]
        pf = [P.ps("pf%d" % i, [128, 512], F32) for i in range(6)]

        MA = P.sb("MA", [128, 4, 8, 2], F32)
        MB = P.sb("MB", [128, 4, 8, 2], F32)
        MG = P.sb("MG", [128, 4, 8, 2], F32)

        with ExitStack() as es2:
            P.es = es2
            ccr = P.sb("ccr", [16, 128], F32)
            scs = P.sb("scs", [128, 16], F32)
            sc2 = P.sb("sc2", [128, 8, 2], F32)
            VR1 = P.sb("VR1", [128, 128], F32)
            VR2 = P.sb("VR2", [32, 128], F32)
            VC1 = P.sb("VC1", [128, 128], F32)
            VC2 = P.sb("VC2", [128, 32], F32)
            modc = P.sb("modc", [128, 24, 2], F32)
            wa = [P.sb("wa%d" % i, [128, 8, 512], F32) for i in range(2)]
            P.dma('sp', ccr.ap[:], cc_in, writes=[ccr])
            P.dma('sp', VR1.ap[0:96, :], b_ada.rearrange("l (c p) -> (l c) p", p=128), writes=[VR1])
            P.dma('sp', VR1.ap[96:128, :], g_pre.rearrange("l (c p) -> (l c) p", p=128), writes=[VR1])
            P.dma('sp', VR2.ap[:], g_post.rearrange("l (c p) -> (l c) p", p=128), writes=[VR2])
            P.tr(pf[0].ap[:, 0:16], ccr.ap[:], ident_f.ap[0:16, 0:16], [ccr, ident_f], [pf[0]])
            P.act(scs.ap[:], pf[0].ap[:, 0:16], AF.Silu, [pf[0]], [scs])
            P.cp('dve', sc2.ap[:, :, 0], scs.ap[:, 0:8], [scs], [sc2])
            P.cp('dve', sc2.ap[:, :, 1], scs.ap[:, 8:16], [scs], [sc2])
            P.tr(pf[1].ap[:, 0:128], VR1.ap[:], ident_f.ap[:], [VR1, ident_f], [pf[1]])
            P.cp('dve', VC1.ap[:], pf[1].ap[:, 0:128], [pf[1]], [VC1])
            P.tr(pf[2].ap[:, 0:32], VR2.ap[:], ident_f.ap[0:32, 0:32], [VR2, ident_f], [pf[2]])
            P.cp('dve', VC2.ap[:], pf[2].ap[:, 0:32], [pf[2]], [VC2])
            gi = 0
            for l in range(n_layers):
                pm = pf[3 + (l % 2)]
                for g6 in range(6):
                    wb_ = wa[gi % 2]
                    gi += 1
                    P.dma('sp', wb_.ap[:], w_ada_v[l][:, :, g6 * 512:(g6 + 1) * 512], writes=[wb_])
                    for jj in range(4):
                        j = g6 * 4 + jj
                        for kc in range(8):
                            P.mm(pm.ap[:, j * 2:j * 2 + 2], wb_.ap[:, kc, jj * 128:(jj + 1) * 128], sc2.ap[:, kc, :],
                                 kc == 0, kc == 7, [wb_, sc2], [pm])
                pm3 = pm.ap[:, 0:48].rearrange("p (j s) -> p j s", s=2)
                P.tt('dve', modc.ap[:], pm3, VC1.ap[:, l * 24:(l + 1) * 24].unsqueeze(2).to_broadcast([128, 24, 2]),
                     ALU.add, [pm, VC1], [modc])
                gpre_bc = VC1.ap[:, 96 + l * 8:96 + (l + 1) * 8].unsqueeze(2).to_broadcast([128, 8, 2])
                gpost_bc = VC2.ap[:, l * 8:(l + 1) * 8].unsqueeze(2).to_broadcast([128, 8, 2])
                P.stt(MA.ap[:, l], modc.ap[:, 8:16, :], 1.0, gpre_bc, ALU.add, ALU.mult, [modc, VC1], [MA])
                P.cp('dve', MB.ap[:, l], modc.ap[:, 0:8, :], [modc], [MB])
                P.tt('dve', MG.ap[:, l], modc.ap[:, 16:24, :], gpost_bc, ALU.mult, [modc, VC2], [MG])
            P.fence()
        P.es = es

        hT = P.sb("hT", [128, 8, TOK], BF16)
        mixT = P.sb("mixT", [128, 8, TOK], BF16)
        hTb = [P.vb("hT_t%d" % i) for i in range(NT)]
        mixb = [[P.vb("mix_%d_%d" % (g, i)) for i in range(NT)] for g in range(4)]
        if tap is not None:
            P.op('pool', lambda e: e.memset(mixT.ap[:], 0.0), writes=[b for g in mixb for b in g])
        wbuf = []
        wrot = [0]

        def alloc_wbuf():
            wbuf[:] = [P.sb("wbuf%d" % i, [128, 8, 512], BF16) for i in range(2)]
        xs = [P.vb("xs%d" % i) for i in range(NT)]
        if VERBOSE:
            print("sbuf bytes remaining after main alloc:", nc.sbuf_bytes_remaining)

        def load_w(l, c0, ncols, q='pool'):
            b = wbuf[wrot[0] % 2]
            wrot[0] += 1
            P.dma(q, b.ap[:, :, 0:ncols], w_in_v[l][:, :, c0:c0 + ncols], writes=[b])
            return b

        def proj_tm(i, wb_, c0, ncols, out_ap, outbuf):
            for kc in range(8):
                P.mm(out_ap, hT.ap[:, kc, i * 128:(i + 1) * 128], wb_.ap[:, kc, c0:c0 + ncols], kc == 0, kc == 7,
                     [hTb[i], wb_], [outbuf])

        def proj_fm(wb_, c0, ncols, t0, nt, out_ap, outbuf):
            rd = [wb_] + [hTb[i] for i in range(t0 // 128, (t0 + nt + 127) // 128)]
            for kc in range(8):
                P.mm(out_ap, wb_.ap[:, kc, c0:c0 + ncols], hT.ap[:, kc, t0:t0 + nt], kc == 0, kc == 7, rd, [outbuf])

        def state_src(l, i):
            if i < 2:
                return (ctx_in if l == 0 else ctxs)[i * 128:(i + 1) * 128, :]
            return (x_in if l == 0 else y_out)[(i - 2) * 128:(i - 1) * 128, :]

        def state_dst(i):
            if i < 2:
                return ctxs[i * 128:(i + 1) * 128, :]
            return y_out[(i - 2) * 128:(i - 1) * 128, :]

        for l in range(n_layers):
            last = (l == 3)
            esC = ExitStack()
            P.es = esC
            xt = [P.sb("xt%d" % i, [128, 1024], F32) for i in range(2)]
            xn = [P.sb("xn%d" % i, [128, 1024], BF16) for i in range(2)]
            tmpf = P.sb("tmpf", [128, 1024], F32)
            st4 = [P.sb("st4_%d" % i, [128, 8], F32) for i in range(2)]
            for i in range(NT):
                s = 1 if i < 2 else 0
                xb_, xnb, stb = xt[i % 2], xn[i % 2], st4[i % 2]
                P.dma('sp', xb_.ap[:], state_src(l, i), reads=[xs[i]], writes=[xb_])
                P.act(tmpf.ap[:], xb_.ap[:], AF.Square, [xb_], [tmpf, stb], accum_out=stb.ap[:, 0:1])
                P.act(stb.ap[:, 1:2], stb.ap[:, 0:1], AF.Sqrt, [stb], [stb], bias=EPS, scale=1.0 / D)
                P.op('dve', lambda e, o=stb.ap[:, 2:3], a=stb.ap[:, 1:2]: e.reciprocal(o, a), [stb], [stb])
                P.act(xnb.ap[:], xb_.ap[:], AF.Identity, [xb_, stb], [xnb], scale=stb.ap[:, 2:3])
                pt = pT[i % 2]
                for kc in range(8):
                    P.tr(pt.ap[:, kc * 128:(kc + 1) * 128], xnb.ap[:, kc * 128:(kc + 1) * 128], ident_bf.ap[:],
                         [xnb, ident_bf], [pt])
                pt3 = pt.ap[:, :].rearrange("p (k t) -> p k t", t=128)
                tm3 = tmpf.ap[:, :].rearrange("p (k t) -> p k t", t=128)
                P.tt('dve', tm3, pt3, MA.ap[:, l, :, s:s + 1].to_broadcast([128, 8, 128]), ALU.mult, [pt, MA], [tmpf])
                P.tt('dve', hT.ap[:, :, i * 128:(i + 1) * 128], tm3, MB.ap[:, l, :, s:s + 1].to_broadcast([128, 8, 128]),
                     ALU.add, [tmpf, MB], [hTb[i]])
            if tap == "h%d" % l:
                P.dma('sp', tap_out, hT.ap[:], reads=hTb, out=True)

            P.fence()
            esC.close()
            P.es = es

            BRANCH_BUILDERS["att"](locals()) if "att" in branches else None
            BRANCH_BUILDERS["mlstm"](locals()) if "mlstm" in branches else None
            BRANCH_BUILDERS["gla"](locals()) if "gla" in branches else None
            BRANCH_BUILDERS["hyena"](locals()) if "hyena" in branches else None
            if tap == "mix%d" % l:
                P.dma('sp', tap_out, mixT.ap[:], reads=[b for g in mixb for b in g], out=True)

            esZ = ExitStack()
            P.es = esZ
            woutb = P.sb("woutb", [128, 8, 1024], BF16)
            Gbc = [P.sb("Gbc%d" % s, [128, 1024], F32) for s in range(2)]
            gbt = P.sb("gbt", [128, 128], F32)
            xt = [P.sb("xt%d" % i, [128, 1024], F32) for i in range(2)]
            tmpf = P.sb("tmpf", [128, 1024], F32)
            st4 = [P.sb("st4_%d" % i, [128, 8], F32) for i in range(2)]
            for h2 in range(2):
                P.dma('pool', woutb.ap[:, :, h2 * 512:(h2 + 1) * 512], w_out_v[l][:, :, h2 * 512:(h2 + 1) * 512],
                      writes=[woutb])
            for s in range(2):
                for j in range(8):
                    P.cp('dve', gbt.ap[:], MG.ap[:, l, j, s:s + 1].to_broadcast([128, 128]), [MG], [gbt])
                    pg = pf[j // 4]
                    P.mm(pg.ap[:, (j % 4) * 128:(j % 4 + 1) * 128], gbt.ap[:], ident_f.ap[:], True, True,
                         [gbt, ident_f], [pg])
                for h2 in range(2):
                    P.cp('act', Gbc[s].ap[:, h2 * 512:(h2 + 1) * 512], pf[h2].ap[:], [pf[h2]], [Gbc[s]])
            for i in range(NT):
                if last and i < 2:
                    continue
                s = 1 if i < 2 else 0
                xb_, stb = xt[i % 2], st4[i % 2]
                P.dma('sp', xb_.ap[:], state_src(l, i), reads=[xs[i]], writes=[xb_])
                py = [pf[2 + 2 * (i % 2)], pf[3 + 2 * (i % 2)]]
                mrd = [mixb[g][i] for g in range(4)]
                for h2 in range(2):
                    for kc in range(8):
                        P.mm(py[h2].ap[:], mixT.ap[:, kc, i * 128:(i + 1) * 128], woutb.ap[:, kc, h2 * 512:(h2 + 1) * 512],
                             kc == 0, kc == 7, mrd + [woutb], [py[h2]])
                for h2 in range(2):
                    P.act(tmpf.ap[:, h2 * 512:(h2 + 1) * 512], py[h2].ap[:], AF.Square, [py[h2]], [tmpf, stb],
                          accum_out=stb.ap[:, 4 + h2:5 + h2])
                P.tt('dve', stb.ap[:, 6:7], stb.ap[:, 4:5], stb.ap[:, 5:6], ALU.add, [stb], [stb])
                P.act(stb.ap[:, 7:8], stb.ap[:, 6:7], AF.Sqrt, [stb], [stb], bias=EPS, scale=1.0 / D)
                P.op('dve', lambda e, o=stb.ap[:, 3:4], a=stb.ap[:, 7:8]: e.reciprocal(o, a), [stb], [stb])
                for h2 in range(2):
                    sl = slice(h2 * 512, (h2 + 1) * 512)
                    P.stt(tmpf.ap[:, sl], py[h2].ap[:], stb.ap[:, 3:4], Gbc[s].ap[:, sl], ALU.mult, ALU.mult,
                          [py[h2], stb, Gbc[s]], [tmpf])
                P.tt('dve', xb_.ap[:], xb_.ap[:], tmpf.ap[:], ALU.add, [xb_, tmpf], [xb_])
                P.dma('sp', state_dst(i), xb_.ap[:], reads=[xb_], writes=[xs[i]], out=(l == n_layers - 1 and i >= 2))
            P.fence()
            esZ.close()
            P.es = es
        P.finish()
    return nc


BRANCH_BUILDERS = {}


def branch_att(L):
    P, nc, l, last = L["P"], L["nc"], L["l"], L["last"]
    hT, hTb, mixT, mixb, pT, pf = L["hT"], L["hTb"], L["mixT"], L["mixb"], L["pT"], L["pf"]
    ident_bf, ropec, ropes, amask = L["ident_bf"], L["ropec"], L["ropes"], L["amask"]
    load_w, proj_tm = L["load_w"], L["proj_tm"]
    with ExitStack() as es2:
        P.es = es2
        L["alloc_wbuf"]()
        QKT = P.sb("QKT", [128, 4, TOK], BF16)
        QKb = [P.vb("QKT_%d" % i) for i in range(NT)]
        v_tm = P.sb("v_tm", [128, NT, 128], BF16)
        sg_tm = P.sb("sg_tm", [128, NT, 256], BF16)
        vb_ = [P.vb("vsg_%d" % i) for i in range(NT)]
        qkf = P.sb("qkf", [128, 384], F32)
        rt = [P.sb("rt%d" % i, [128, 192], F32) for i in range(2)]
        rq = P.sb("rq", [128, 512], BF16)
        sink_bc = P.sb("sink_bc", [128, 4], F32)
        sm = P.sb("sm", [128, 640], F32)
        Pb = P.sb("Pb", [128, 640], BF16)
        PTs = P.sb("PTs", [128, 5, 128], BF16)
        stt_ = P.sb("att_st", [128, 8], F32)
        rden = P.sb("rden", [128, 4], F32)
        og = P.sb("og", [128, 256], BF16)
        P.dma('sp', sink_bc.ap[:], L["attn_sink"][l:l + 1, :].partition_broadcast(128), writes=[sink_bc])
        w1 = load_w(l, COL_D, 384)
        w2 = load_w(l, COL_D + 384, 384)
        for i in range(NT):
            pq, pv = pf[0], pf[1]
            proj_tm(i, w1, 0, 384, pq.ap[:, 0:384], pq)
            proj_tm(i, w2, 0, 384, pv.ap[:, 0:384], pv)
            P.cp('act', v_tm.ap[:, i, :], pv.ap[:, 0:128], [pv], [vb_[i]])
            P.act(sg_tm.ap[:, i, :], pv.ap[:, 128:384], AF.Silu, [pv], [vb_[i]])
            if i >= 2:
                P.cp('act', qkf.ap[:], pq.ap[:, 0:384], [pq], [qkf])
                q4 = qkf.ap[:, :].rearrange("p (h two j) -> p h two j", two=2, j=32)
                r4 = rq.ap[:, 0:384].rearrange("p (h two j) -> p h two j", two=2, j=32)
                x1, x2 = q4[:, :, 0, :], q4[:, :, 1, :]
                cosb = ropec.ap[:, i - 2, :].unsqueeze(1).to_broadcast([128, 6, 32])
                sinb = ropes.ap[:, i - 2, :].unsqueeze(1).to_broadcast([128, 6, 32])
                ta = rt[0].ap[:, :].rearrange("p (h j) -> p h j", j=32)
                tb = rt[1].ap[:, :].rearrange("p (h j) -> p h j", j=32)
                P.tt('dve', ta, x1, cosb, ALU.mult, [qkf, ropec], [rt[0]])
                P.tt('dve', tb, x2, sinb, ALU.mult, [qkf, ropes], [rt[1]])
                P.tt('dve', r4[:, :, 0, :], ta, tb, ALU.subtract, [rt[0], rt[1]], [rq])
                P.tt('dve', ta, x2, cosb, ALU.mult, [qkf, ropec], [rt[0]])
                P.tt('dve', tb, x1, sinb, ALU.mult, [qkf, ropes], [rt[1]])
                P.tt('dve', r4[:, :, 1, :], ta, tb, ALU.add, [rt[0], rt[1]], [rq])
            else:
                P.cp('act', rq.ap[:, 0:384], pq.ap[:, 0:384], [pq], [rq])
            kd_src = rq.ap[:, 256:384].rearrange("p (g o d) -> p g o d", o=1, d=64).to_broadcast([128, 2, 2, 64])
            P.cp('dve', rt[0].ap[:, 0:128].bitcast(BF16).rearrange("p (g o d) -> p g o d", o=2, d=64), kd_src,
                 [rq], [rt[0]])
            P.cp('dve', rq.ap[:, 256:512], rt[0].ap[:, 0:128].bitcast(BF16), [rt[0]], [rq])
            pt = pT[i % 2]
            for c4 in range(4):
                P.tr(pt.ap[:, c4 * 128:(c4 + 1) * 128], rq.ap[:, c4 * 128:(c4 + 1) * 128], ident_bf.ap[:],
                     [rq, ident_bf], [pt])
            P.cp('dve', QKT.ap[:, :, i * 128:(i + 1) * 128], pt.ap[:, 0:512].rearrange("p (c t) -> p c t", t=128),
                 [pt], [QKb[i]])

        def attn_block(i):
            has_local = i >= 2
            n = i - 2
            if has_local:
                nlo, nhi = max(n - 1, 0), min(n + 1, 15)
                c0 = (nlo - (n - 1)) * 128
                c1 = c0 + (nhi - nlo + 1) * 128
                ktiles = list(range(2 + nlo, 2 + nhi + 1))
            else:
                c0 = c1 = 384
                ktiles = []
            d0 = 384 - (c1 - c0)
            blocks = [(kt, d0 + 128 * bi) for bi, kt in enumerate(ktiles)] + [(0, 384), (1, 512)]
            pO = pf[2 + (i % 2)]
            for h in range(4):
                g, base = h // 2, (h % 2) * 64
                pl, pc = pf[4], pf[5]
                qa = QKT.ap[base:base + 64, g, i * 128:(i + 1) * 128]
                if has_local:
                    P.mm(pl.ap[:, d0:384], qa, QKT.ap[base:base + 64, 2 + g, ktiles[0] * 128:(ktiles[-1] + 1) * 128],
                         True, True, [QKb[i]] + [QKb[k] for k in ktiles], [pl])
                    P.tt('dve', sm.ap[:, d0:384], pl.ap[:, d0:384], amask.ap[:, c0:c1], ALU.add, [pl, amask], [sm])
                P.mm(pc.ap[:, 0:256], qa, QKT.ap[base:base + 64, 2 + g, 0:256], True, True, [QKb[i], QKb[0], QKb[1]], [pc])
                P.cp('act', sm.ap[:, 384:640], pc.ap[:, 0:256], [pc], [sm])
                P.op('dve', lambda e, o=stt_.ap[:, 0:1], a=sm.ap[:, d0:640]: e.reduce_max(o, a, AX.X), [sm], [stt_])
                P.ts('dve', stt_.ap[:, 1:2], stt_.ap[:, 0:1], -0.125, None, ALU.mult, ALU.bypass, [stt_], [stt_])
                P.act(Pb.ap[:, d0:640], sm.ap[:, d0:640], AF.Exp, [sm, stt_], [Pb, stt_], bias=stt_.ap[:, 1:2], scale=0.125,
                      accum_out=stt_.ap[:, 2:3])
                P.act(stt_.ap[:, 3:4], stt_.ap[:, 1:2], AF.Exp, [stt_, sink_bc], [stt_], bias=sink_bc.ap[:, h:h + 1], scale=1.0)
                P.tt('dve', stt_.ap[:, 4:5], stt_.ap[:, 2:3], stt_.ap[:, 3:4], ALU.add, [stt_], [stt_])
                P.op('dve', lambda e, o=rden.ap[:, h:h + 1], a=stt_.ap[:, 4:5]: e.reciprocal(o, a), [stt_], [rden])
                pt = pT[h % 2]
                for bi, (kt, cc) in enumerate(blocks):
                    P.tr(pt.ap[:, bi * 128:(bi + 1) * 128], Pb.ap[:, cc:cc + 128], ident_bf.ap[:], [Pb, ident_bf], [pt])
                nb = len(blocks)
                P.cp('act', PTs.ap[:, 0:nb, :], pt.ap[:, 0:nb * 128].rearrange("p (c t) -> p c t", t=128), [pt], [PTs])
                for bi, (kt, cc) in enumerate(blocks):
                    P.mm(pO.ap[:, h * 64:(h + 1) * 64], PTs.ap[:, bi, :], v_tm.ap[:, kt, g * 64:(g + 1) * 64],
                         bi == 0, bi == nb - 1, [PTs, vb_[kt]], [pO])
            for h in range(4):
                P.stt(og.ap[:, h * 64:(h + 1) * 64], pO.ap[:, h * 64:(h + 1) * 64], rden.ap[:, h:h + 1],
                      sg_tm.ap[:, i, h * 64:(h + 1) * 64], ALU.mult, ALU.mult, [pO, rden, vb_[i]], [og])
            pt = pT[i % 2]
            for c2 in range(2):
                P.tr(pt.ap[:, c2 * 128:(c2 + 1) * 128], og.ap[:, c2 * 128:(c2 + 1) * 128], ident_bf.ap[:], [og, ident_bf], [pt])
            P.cp('act', mixT.ap[:, 6:8, i * 128:(i + 1) * 128], pt.ap[:, 0:256].rearrange("p (c t) -> p c t", t=128),
                 [pt], [mixb[3][i]])

        for i in range(NT):
            if last and i < 2:
                continue
            attn_block(i)
        P.fence()
    P.es = L["es"]


BRANCH_BUILDERS["att"] = branch_att


def make_in_maps(inputs):
    c = host_consts()
    shared = {}
    for k in ["w_ada", "b_ada", "g_pre", "g_post", "w_in", "w_out", "mlstm_norm_g", "gla_norm_g", "gla_w_alpha",
              "gla_b_alpha", "attn_sink"]:
        shared[k] = np.ascontiguousarray(np.asarray(inputs[k], dtype=np.float32))
    for k in ["hyena_conv_w", "hyena_conv_b", "hyena_w1", "hyena_b1", "hyena_w2", "hyena_b2", "hyena_w3", "hyena_b3",
              "hyena_freq", "hyena_d"]:
        shared[k] = np.ascontiguousarray(np.asarray(inputs[k], dtype=np.float32))
    shared["mlstm_gate_b"] = np.ascontiguousarray(np.asarray(inputs["mlstm_gate_b"], dtype=np.float32).reshape(4, 16))
    shared.update(c)
    maps = []
    c_ctx = np.asarray(inputs["c_ctx"], dtype=np.float32)
    for b in range(8):
        m = dict(shared)
        m["x"] = np.ascontiguousarray(np.asarray(inputs["x"][b], dtype=np.float32))
        m["ctx"] = np.ascontiguousarray(np.asarray(inputs["ctx"][b], dtype=np.float32))
        m["cc"] = np.ascontiguousarray(np.concatenate([np.asarray(inputs["c"][b], dtype=np.float32), c_ctx]).reshape(16, 128))
        maps.append(m)
    return maps


def branch_scan(L, kind):
    ml = kind == "mlstm"
    P, nc, l, last = L["P"], L["nc"], L["l"], L["last"]
    hT, hTb, mixT, mixb, pT, pf = L["hT"], L["hTb"], L["mixT"], L["mixb"], L["pT"], L["pf"]
    ident_bf, ident_f = L["ident_bf"], L["ident_f"]
    load_w, proj_tm, proj_fm = L["load_w"], L["proj_tm"], L["proj_fm"]
    base = COL_A if ml else COL_G
    cdec = 1.0 if ml else 1.0 / 16
    mc0, mg = (0, 0) if ml else (4, 2)
    qscale, kscale = (1.0, 0.125) if ml else (0.125, 1.0)
    with ExitStack() as es2:
        P.es = es2
        L["alloc_wbuf"]()
        qT = P.sb("s_qT", [128, 2, TOK], BF16)
        kT = P.sb("s_kT", [128, 2, TOK], BF16)
        k_tm = P.sb("s_ktm", [128, NT, 256], BF16)
        v_aug = P.sb("s_vaug", [128, NT, 512], BF16)
        Hh = P.sb("s_H", [128, NT, 256], F32)
        gr = P.sb("s_gr", [128, NT, 32], F32)
        qkb = [P.vb("s_qkb%d" % i) for i in range(NT)]
        kvb = [P.vb("s_kvb%d" % i) for i in range(NT)]
        Hb = [P.vb("s_Hb%d" % i) for i in range(NT)]
        grb = [P.vb("s_grb%d" % i) for i in range(NT)]
        normg = P.sb("s_normg", [128, 256], F32)
        P.dma('sp', normg.ap[:], (L["mlstm_norm_g"] if ml else L["gla_norm_g"])[l:l + 1, :].partition_broadcast(128),
              writes=[normg])
        if ml:
            gbb = P.sb("s_gbb", [128, 16], F32)
            P.dma('sp', gbb.ap[:], L["mlstm_gate_b"][l:l + 1, :].partition_broadcast(128), writes=[gbb])
            posi = P.sb("s_posi", [128, 256], F32)
            negi = P.sb("s_negi", [128, 256], F32)
            e4 = P.sb("s_e4", [128, 8], F32)
        else:
            wal = P.sb("s_wal", [17, 2, 256], F32)
            P.dma('sp', wal.ap[0:16, :, :], L["gla_w_alpha"][l].rearrange("d r c -> r d c"), writes=[wal])
            P.dma('sp', wal.ap[16:17, :, :], L["gla_b_alpha"][l:l + 1, :, :], writes=[wal])
            rTa = P.sb("s_rTa", [17, 128], F32)
            P.op('pool', lambda e: e.memset(rTa.ap[:], 1.0), writes=[rTa])
            e_tm = P.sb("s_etm", [128, 256], F32)
        sp_tm = P.sb("s_sp", [128, 256], F32)
        EqT = P.sb("s_EqT", [128, 256], F32)
        EkT = P.sb("s_EkT", [128, 256], F32)
        Ex = P.sb("s_Ex", [128, 256], F32)
        qp = P.sb("s_qp", [128, 2, 128], BF16)
        kpz = [P.sb("s_kpz%d" % h, [128, 128], BF16) for h in range(4)]
        for h in range(4):
            P.op('pool', lambda e, a=kpz[h].ap[:]: e.memset(a, 0.0), writes=[kpz[h]])
        kpp = P.sb("s_kpp", [128, 256], BF16)
        S_m = P.sb("s_Sm", [128, 512], BF16)
        St_f = [P.sb("s_Stf%d" % j, [128, 128], F32) for j in range(2)]
        St_b = [P.sb("s_Stb%d" % h, [128, 128], BF16) for h in range(4)]
        dn = P.sb("s_dn", [128, 8], F32)
        tmpH = P.sb("s_tmpH", [128, 256], F32)

        wqk = load_w(l, base, 512)
        groups = [(0, 512), (512, 512), (1024, 512), (1536, 512), (2048, 256)]
        cnt = 0
        for j in range(2):
            for (t0, nt) in groups:
                tiles = list(range(t0 // 128, (t0 + nt) // 128))
                for which, dst, sc_ in ((0, qT, qscale), (1, kT, kscale)):
                    pq = pf[cnt % 2]
                    cnt += 1
                    proj_fm(wqk, which * 256 + j * 128, 128, t0, nt, pq.ap[:, 0:nt], pq)
                    P.act(dst.ap[:, j, t0:t0 + nt], pq.ap[:, 0:nt], AF.Identity, [pq], [qkb[t] for t in tiles], scale=sc_)
        wkv = load_w(l, base + 256, 512)
        ng = 16 if ml else 32
        wg = load_w(l, base + (1024 if ml else 768), 256 + ng)
        P.op('pool', lambda e: e.memset(v_aug.ap[:], 1.0), writes=kvb)
        for i in range(NT):
            pk = pf[2 + i % 2]
            proj_tm(i, wkv, 0, 512, pk.ap[:], pk)
            P.act(k_tm.ap[:, i, :], pk.ap[:, 0:256], AF.Identity, [pk], [kvb[i]], scale=kscale)
            P.cp('dve', v_aug.ap[:, i, :].rearrange("p (h e) -> p h e", e=128)[:, :, 0:64],
                 pk.ap[:, 256:512].rearrange("p (h e) -> p h e", e=64), [pk], [kvb[i]])
            pg = pf[4 + i % 2]
            proj_tm(i, wg, 256, 64, pg.ap[:, 0:64], pg)
            if ml:
                P.tt('dve', gr.ap[:, i, 0:16], pg.ap[:, 0:16], gbb.ap[:], ALU.add, [pg, gbb], [grb[i]])
            else:
                P.cp('dve', gr.ap[:, i, 0:32], pg.ap[:, 0:32], [pg], [grb[i]])

        P.ckpt("pre")
        for dirn in range(2):
            order = ([0, 1] + list(range(2, NT))) if dirn == 0 else ([1, 0] + list(range(NT - 1, 1, -1)))
            tri_c = L["tri_le"] if dirn == 0 else L["tri_ge"]
            tri_x = L["tri_gt"] if dirn == 0 else L["tri_lt"]
            dcol = 127 if dirn == 0 else 0
            for j in range(2):
                P.op('pool', lambda e, a=St_f[j].ap[:]: e.memset(a, 0.0), writes=[St_f[j]])
            for h in range(4):
                P.op('pool', lambda e, a=St_b[h].ap[:]: e.memset(a, 0.0), writes=[St_b[h]])
            for step, i in enumerate(order):
                tsl = slice(i * 128, (i + 1) * 128)
                if ml:
                    ic, fc = dirn * 8, dirn * 8 + 4
                    P.act(e4.ap[:, 0:4], gr.ap[:, i, fc:fc + 4], AF.Exp, [grb[i]], [e4], scale=-1.0)
                    P.act(e4.ap[:, 4:8], e4.ap[:, 0:4], AF.Ln, [e4], [e4], bias=1.0)
                    P.cp('dve', sp_tm.ap[:, :].rearrange("p (h d) -> p h d", d=64),
                         e4.ap[:, 4:8].unsqueeze(2).to_broadcast([128, 4, 64]), [e4], [sp_tm])
                    ib = gr.ap[:, i, ic:ic + 4].unsqueeze(2).to_broadcast([128, 4, 64])
                    P.cp('dve', posi.ap[:, :].rearrange("p (h d) -> p h d", d=64), ib, [grb[i]], [posi])
                    P.ts('dve', negi.ap[:, :].rearrange("p (h d) -> p h d", d=64), ib, -1.0, None, ALU.mult, ALU.bypass,
                         [grb[i]], [negi])
                else:
                    P.tr(pf[3].ap[0:16, 256:384], gr.ap[:, i, dirn * 16:(dirn + 1) * 16], ident_f.ap[:],
                         [grb[i], ident_f], [pf[3]])
                    P.cp('act', rTa.ap[0:16, :], pf[3].ap[0:16, 256:384], [pf[3]], [rTa])
                    P.mm(pf[3].ap[:, 256:512], rTa.ap[0:17, :], wal.ap[0:17, dirn, :], True, True, [rTa, wal], [pf[3]])
                    P.act(e_tm.ap[:], pf[3].ap[:, 256:512], AF.Exp, [pf[3]], [e_tm], scale=-1.0)
                    P.act(sp_tm.ap[:], e_tm.ap[:], AF.Ln, [e_tm], [sp_tm], bias=1.0)
                P.ckpt("s1")
                pc, px = pf[4], pf[3]
                for j in range(2):
                    P.mm(pc.ap[:, j * 128:(j + 1) * 128], sp_tm.ap[:, j * 128:(j + 1) * 128], tri_c.ap[:], True, True,
                         [sp_tm, tri_c], [pc])
                if ml:
                    for j in range(2):
                        P.mm(pc.ap[:, 256 + j * 128:384 + j * 128], sp_tm.ap[:, j * 128:(j + 1) * 128], tri_c.ap[:],
                             True, False, [sp_tm, tri_c], [pc])
                        P.mm(pc.ap[:, 256 + j * 128:384 + j * 128], posi.ap[:, j * 128:(j + 1) * 128], ident_f.ap[:],
                             False, True, [posi, ident_f], [pc])
                P.mm(px.ap[:, 0:256], tri_x.ap[:], sp_tm.ap[:], True, not ml, [tri_x, sp_tm], [px])
                if ml:
                    P.mm(px.ap[:, 0:256], ident_f.ap[:], negi.ap[:], False, True, [ident_f, negi], [px])
                P.ckpt("s2")
                P.act(EqT.ap[:], pc.ap[:, 0:256], AF.Exp, [pc], [EqT], scale=-cdec)
                P.act(EkT.ap[:], pc.ap[:, 256:512] if ml else pc.ap[:, 0:256], AF.Exp, [pc], [EkT], scale=cdec)
                P.act(Ex.ap[:], px.ap[:, 0:256], AF.Exp, [px], [Ex], scale=-cdec)
                P.tt('dve', qp.ap[:], qT.ap[:, :, tsl], EqT.ap[:, :].rearrange("p (j t) -> p j t", t=128), ALU.mult,
                     [qkb[i], EqT], [qp])
                for h in range(4):
                    j, b0 = h // 2, (h % 2) * 64
                    P.tt('dve', kpz[h].ap[b0:b0 + 64, :], kT.ap[b0:b0 + 64, j, tsl], EkT.ap[b0:b0 + 64, j * 128:(j + 1) * 128],
                         ALU.mult, [qkb[i], EkT], [kpz[h]])
                P.tt('dve', kpp.ap[:], k_tm.ap[:, i, :], Ex.ap[:], ALU.mult, [kvb[i], Ex], [kpp])
                P.ckpt("s3")
                pS = pf[2]
                for h in range(4):
                    j, b0 = h // 2, (h % 2) * 64
                    P.mm(pS.ap[:, h * 128:(h + 1) * 128], kpz[h].ap[:], qp.ap[:, j, :], True, True, [kpz[h], qp], [pS])
                P.tt('dve', S_m.ap[:, :].rearrange("p (h t) -> p h t", t=128),
                     pS.ap[:, :].rearrange("p (h t) -> p h t", t=128),
                     tri_c.ap[:, :].unsqueeze(1).to_broadcast([128, 4, 128]), ALU.mult, [pS, tri_c], [S_m])
                P.ckpt("s4")
                pO = pf[step % 2]
                for h in range(4):
                    j, b0 = h // 2, (h % 2) * 64
                    P.mm(pO.ap[:, h * 128:(h + 1) * 128], S_m.ap[:, h * 128:(h + 1) * 128], v_aug.ap[:, i, h * 128:(h + 1) * 128],
                         True, False, [S_m, kvb[i]], [pO])
                    P.mm(pO.ap[:, h * 128:(h + 1) * 128], qp.ap[:, j, :], St_b[h].ap[:], False, True, [qp, St_b[h]], [pO])
                P.ckpt("s5")
                pD = pf[5]
                for j in range(2):
                    P.mm(pD.ap[:, j * 256:(j + 1) * 256], kpp.ap[:, j * 128:(j + 1) * 128], v_aug.ap[:, i, j * 256:(j + 1) * 256],
                         True, True, [kpp, kvb[i]], [pD])
                for h in range(4):
                    j, b0 = h // 2, (h % 2) * 64
                    P.stt(St_f[j].ap[b0:b0 + 64, :], St_f[j].ap[b0:b0 + 64, :], EqT.ap[b0:b0 + 64, j * 128 + dcol:j * 128 + dcol + 1],
                          pD.ap[b0:b0 + 64, j * 256 + (h % 2) * 128:j * 256 + (h % 2) * 128 + 128], ALU.mult, ALU.add,
                          [St_f[j], EqT, pD], [St_f[j]])
                for h in range(4):
                    j, b0 = h // 2, (h % 2) * 64
                    P.cp('act', St_b[h].ap[b0:b0 + 64, :], St_f[j].ap[b0:b0 + 64, :], [St_f[j]], [St_b[h]])
                P.ckpt("s6")
                pO3 = pO.ap[:, 0:512].rearrange("p (h e) -> p h e", e=128)
                H3 = Hh.ap[:, i, :].rearrange("p (h d) -> p h d", d=64)
                if ml:
                    P.act(dn.ap[:, 0:4], pO3[:, :, 64], AF.Abs, [pO], [dn])
                    P.ts('dve', dn.ap[:, 0:4], dn.ap[:, 0:4], 1.0, None, ALU.max, ALU.bypass, [dn], [dn])
                    P.op('dve', lambda e, o=dn.ap[:, 4:8], a=dn.ap[:, 0:4]: e.reciprocal(o, a), [dn], [dn])
                    rb = dn.ap[:, 4:8].unsqueeze(2).to_broadcast([128, 4, 64])
                    if dirn == 0:
                        P.tt('dve', H3, pO3[:, :, 0:64], rb, ALU.mult, [pO, dn], [Hb[i]])
                    else:
                        P.tt('dve', tmpH.ap[:, :].rearrange("p (h d) -> p h d", d=64), pO3[:, :, 0:64], rb, ALU.mult,
                             [pO, dn], [tmpH])
                        P.tt('dve', Hh.ap[:, i, :], Hh.ap[:, i, :], tmpH.ap[:], ALU.add, [Hb[i], tmpH], [Hb[i]])
                else:
                    if dirn == 0:
                        P.cp('act', H3, pO3[:, :, 0:64], [pO], [Hb[i]])
                    else:
                        P.tt('dve', H3, pO3[:, :, 0:64], H3, ALU.add, [pO, Hb[i]], [Hb[i]])

        P.ckpt("scan")
        ncol = 512 if ml else 256
        wfin = load_w(l, base + 768, ncol)
        sig = P.sb("s_sig", [128, 256], F32)
        sgt = P.sb("s_sgt", [128, 256], F32)
        gs = P.sb("s_gs", [128, 256], F32)
        hh = P.sb("s_hh", [128, 256], F32)
        sq = P.sb("s_sq", [128, 256], F32)
        og = P.sb("s_og", [128, 256], BF16)
        for i in range(NT):
            if last and i < 2:
                continue
            pg = pf[i % 2]
            proj_tm(i, wfin, 0, ncol, pg.ap[:, 0:ncol], pg)
            if ml:
                P.act(sig.ap[:], pg.ap[:, 0:256], AF.Sigmoid, [pg], [sig])
                P.tt('dve', hh.ap[:], Hh.ap[:, i, :], sig.ap[:], ALU.mult, [Hb[i], sig], [hh])
                gate_ap = pg.ap[:, 256:512]
            else:
                P.cp('dve', hh.ap[:], Hh.ap[:, i, :], [Hb[i]], [hh])
                gate_ap = pg.ap[:, 0:256]
            P.act(sgt.ap[:], gate_ap, AF.Silu, [pg], [sgt])
            P.tt('dve', gs.ap[:], sgt.ap[:], normg.ap[:], ALU.mult, [sgt, normg], [gs])
            P.tt('dve', sq.ap[:], hh.ap[:], hh.ap[:], ALU.mult, [hh], [sq])
            P.op('dve', lambda e, o=dn.ap[:, 0:4], a=sq.ap[:, :].rearrange("p (h d) -> p h d", d=64): e.reduce_sum(o, a, AX.X),
                 [sq], [dn])
            P.act(dn.ap[:, 4:8], dn.ap[:, 0:4], AF.Sqrt, [dn], [dn], bias=EPS, scale=1.0 / 64)
            P.op('dve', lambda e, o=dn.ap[:, 0:4], a=dn.ap[:, 4:8]: e.reciprocal(o, a), [dn], [dn])
            P.tt('dve', sq.ap[:, :].rearrange("p (h d) -> p h d", d=64), hh.ap[:, :].rearrange("p (h d) -> p h d", d=64),
                 dn.ap[:, 0:4].unsqueeze(2).to_broadcast([128, 4, 64]), ALU.mult, [hh, dn], [sq])
            P.tt('dve', og.ap[:], sq.ap[:], gs.ap[:], ALU.mult, [sq, gs], [og])
            pt = pT[i % 2]
            for c2 in range(2):
                P.tr(pt.ap[:, c2 * 128:(c2 + 1) * 128], og.ap[:, c2 * 128:(c2 + 1) * 128], ident_bf.ap[:], [og, ident_bf], [pt])
            P.cp('act', mixT.ap[:, mc0:mc0 + 2, i * 128:(i + 1) * 128], pt.ap[:, 0:256].rearrange("p (c t) -> p c t", t=128),
                 [pt], [mixb[mg][i]])
        P.fence()
    P.es = L["es"]


BRANCH_BUILDERS["mlstm"] = lambda L: branch_scan(L, "mlstm")
BRANCH_BUILDERS["gla"] = lambda L: branch_scan(L, "gla")


def hcol(i):
    return 2 + i * 128 if i < 2 else 262 + (i - 2) * 128


def branch_hyena(L):
    P, nc, l, last, hy = L["P"], L["nc"], L["l"], L["last"], L["hy"]
    hT, hTb, mixT, mixb, pT, pf = L["hT"], L["hTb"], L["mixT"], L["mixb"], L["pT"], L["pf"]
    ident_bf, ident_f = L["ident_bf"], L["ident_f"]
    load_w, proj_fm = L["load_w"], L["proj_fm"]
    PI = 3.1415925
    TWO_PI = 6.283185307179586
    W = 2312
    with ExitStack() as es2:
        P.es = es2
        RC = P.sb("h_RC", [128, 16, 512], BF16)
        RS = P.sb("h_RS", [128, 16, 512], BF16)
        RCc = P.sb("h_RCc", [128, 2, 512], BF16)
        RSc = P.sb("h_RSc", [128, 2, 512], BF16)
        uT = P.sb("h_uT", [128, 2, W], BF16)
        x0c = P.sb("h_x0c", [128, 2, W], BF16)
        sgT = P.sb("h_sgT", [128, 2, W], BF16)
        vcol = P.sb("h_vcol", [128, 26], F32)
        fb = P.sb("h_fb", [64, 6], F32)
        wfl = P.sb("h_wfl", [128, 16], F32)
        wfc = P.sb("h_wfc", [128, 2], F32)
        ntl = P.sb("h_ntl", [128, 16], F32)
        ntc = P.sb("h_ntc", [128, 2], F32)
        delt = P.sb("h_delt", [128, 256], F32)
        P.dma('sp', wfl.ap[:], hy["wf_l"], writes=[wfl])
        P.dma('sp', wfc.ap[:], hy["wf_c"], writes=[wfc])
        P.dma('sp', ntl.ap[:], hy["ntcol_l"], writes=[ntl])
        P.dma('sp', ntc.ap[:], hy["ntcol_c"], writes=[ntc])
        P.dma('sp', delt.ap[:], hy["deltas"].partition_broadcast(128), writes=[delt])
        do_ctx = not last

        with ExitStack() as es3:
            P.es = es3
            vr = P.sb("h_vr", [26, 128], F32)
            fr = P.sb("h_fr", [4, 64], F32)
            P.dma('sp', vr.ap[0:18, :], hy["hyena_conv_w"][l].rearrange("j (c p) -> (j c) p", p=128), writes=[vr])
            P.dma('sp', vr.ap[18:24, :], hy["hyena_conv_b"][l].rearrange("(c p) -> c p", p=128), writes=[vr])
            P.dma('sp', vr.ap[24:26, :], hy["hyena_d"][l].rearrange("(c p) -> c p", p=128), writes=[vr])
            P.dma('sp', fr.ap[0:1, :], hy["hyena_b1"][l:l + 1, :], writes=[fr])
            P.dma('sp', fr.ap[1:2, :], hy["hyena_b2"][l:l + 1, :], writes=[fr])
            P.dma('sp', fr.ap[2:4, :], hy["hyena_freq"][l], writes=[fr])
            P.tr(pf[0].ap[:, 0:26], vr.ap[:], ident_f.ap[0:26, 0:26], [vr, ident_f], [pf[0]])
            P.cp('dve', vcol.ap[:], pf[0].ap[:, 0:26], [pf[0]], [vcol])
            P.tr(pf[1].ap[0:64, 0:4], fr.ap[:], ident_f.ap[0:4, 0:4], [fr, ident_f], [pf[1]])
            P.cp('dve', fb.ap[:, 0:4], pf[1].ap[0:64, 0:4], [pf[1]], [fb])
            P.tt('dve', fb.ap[:, 4:6], fb.ap[:, 0:2], fb.ap[:, 2:4], ALU.mult, [fb], [fb])

            P.ckpt("hv")
            zTl = P.sb("h_zTl", [33, 2048], F32)
            zTc = P.sb("h_zTc", [33, 256], F32)
            w1s = P.sb("h_w1", [33, 64], F32)
            w2s = P.sb("h_w2", [64, 64], F32)
            w3a = P.sb("h_w3a", [65, 512], F32)
            arg = P.sb("h_arg", [64, 512], F32)
            t1 = P.sb("h_t1", [64, 512], F32)
            t2 = P.sb("h_t2", [64, 512], F32)
            h1 = P.sb("h_h1", [64, 512], F32)
            h2 = P.sb("h_h2", [65, 512], F32)
            win = P.sb("h_win", [128, 256], F32)
            hf = P.sb("h_hf", [128, 256], F32)
            hb = P.sb("h_hb", [128, 256], F32)
            P.dma('sp', zTl.ap[:], hy["zT_l"], writes=[zTl])
            P.dma('sp', zTc.ap[:], hy["zT_c"], writes=[zTc])
            P.dma('sp', w1s.ap[:], hy["hyena_w1"][l], writes=[w1s])
            P.dma('sp', w2s.ap[:], hy["hyena_w2"][l], writes=[w2s])
            P.dma('sp', w3a.ap[0:64, :], hy["hyena_w3"][l], writes=[w3a])
            P.dma('sp', w3a.ap[64:65, :], hy["hyena_b3"][l:l + 1, :], writes=[w3a])
            P.op('pool', lambda e: e.memset(h2.ap[:], 1.0), writes=[h2])

            def sin_layer(ps_buf, ps_ap, fc_, fbc_, out_ap, outbuf, nn):
                P.act(arg.ap[:, 0:nn], ps_ap, AF.Identity, [ps_buf, fb], [arg], scale=fb.ap[:, fc_:fc_ + 1],
                      bias=fb.ap[:, fbc_:fbc_ + 1])
                P.ts('dve', t1.ap[:, 0:nn], arg.ap[:, 0:nn], PI, TWO_PI, ALU.is_gt, ALU.mult, [arg], [t1])
                P.ts('dve', t2.ap[:, 0:nn], arg.ap[:, 0:nn], -PI, TWO_PI, ALU.is_lt, ALU.mult, [arg], [t2])
                P.tt('dve', arg.ap[:, 0:nn], arg.ap[:, 0:nn], t1.ap[:, 0:nn], ALU.subtract, [arg, t1], [arg])
                P.tt('dve', arg.ap[:, 0:nn], arg.ap[:, 0:nn], t2.ap[:, 0:nn], ALU.add, [arg, t2], [arg])
                P.act(out_ap, arg.ap[:, 0:nn], AF.Sin, [arg], [outbuf])

            def mlp_block(zsrc, n0, nn, ntc_, RCd, RSd, tile0):
                P.mm(pf[0].ap[0:64, 0:nn], w1s.ap[:], zsrc.ap[:, n0:n0 + nn], True, True, [w1s, zsrc], [pf[0]])
                sin_layer(pf[0], pf[0].ap[0:64, 0:nn], 2, 4, h1.ap[:, 0:nn], h1, nn)
                P.mm(pf[1].ap[0:64, 0:nn], w2s.ap[:], h1.ap[:, 0:nn], True, True, [w2s, h1], [pf[1]])
                sin_layer(pf[1], pf[1].ap[0:64, 0:nn], 3, 5, h2.ap[0:64, 0:nn], h2, nn)
                for tt_ in range(nn // 128):
                    dt = tile0 + tt_
                    pp = pf[2 + tt_ % 2]
                    P.mm(pp.ap[:, 0:512], h2.ap[0:65, tt_ * 128:(tt_ + 1) * 128], w3a.ap[:], True, True, [h2, w3a], [pp])
                    P.act(win.ap[:], delt.ap[:], AF.Exp, [delt, ntc_], [win], scale=ntc_.ap[:, dt:dt + 1])
                    P.tt('dve', hf.ap[:], pp.ap[:, 0:256], win.ap[:], ALU.mult, [pp, win], [hf])
                    P.tt('dve', hb.ap[:], pp.ap[:, 256:512], win.ap[:], ALU.mult, [pp, win], [hb])
                    if dt == 0:
                        P.op('pool', lambda e: e.memset(hb.ap[0:1, :], 0.0), reads=[hb], writes=[hb])
                    P.tt('dve', RCd.ap[:, dt, 256:512], hf.ap[:], hb.ap[:], ALU.add, [hf, hb], [RCd])
                    P.tt('dve', RSd.ap[:, dt, 256:512], hf.ap[:], hb.ap[:], ALU.subtract, [hf, hb], [RSd])

            for blk in range(4):
                mlp_block(zTl, blk * 512, 512, ntl, RC, RS, blk * 4)
                P.ckpt("hm%d" % blk)
            if do_ctx:
                mlp_block(zTc, 0, 256, ntc, RCc, RSc, 0)
            P.fence()
        P.es = es2

        with ExitStack() as es3:
            P.es = es3
            L["alloc_wbuf"]()
            raw = P.sb("h_raw", [128, W], F32)
            cv = P.sb("h_cv", [128, W], F32)
            P.op('pool', lambda e: e.memset(raw.ap[:], 0.0), writes=[raw])
            wA = load_w(l, COL_H, 512)
            wB = load_w(l, COL_H + 512, 512)
            groups = [(0, 256), (256, 512), (768, 512), (1280, 512), (1792, 512)]
            cnt = 0
            for c6 in range(8):
                wsel = wA if c6 < 4 else wB
                coff = (c6 % 4) * 128
                for (t0, nt) in groups:
                    pp = pf[cnt % 2]
                    cnt += 1
                    proj_fm(wsel, coff, 128, t0, nt, pp.ap[:, 0:nt], pp)
                    c0 = 2 + t0 if t0 < 256 else t0 + 6
                    if c6 < 6:
                        P.cp('act', raw.ap[:, c0:c0 + nt], pp.ap[:, 0:nt], [pp], [raw])
                    else:
                        P.act(sgT.ap[:, c6 - 6, c0:c0 + nt], pp.ap[:, 0:nt], AF.Silu, [pp], [sgT])
                if c6 >= 6:
                    continue
                w0c, w1c, w2c = (vcol.ap[:, j * 6 + c6:j * 6 + c6 + 1] for j in range(3))
                bc = vcol.ap[:, 18 + c6:19 + c6]
                P.ts('dve', cv.ap[:, 1:W - 1], raw.ap[:, 1:W - 1], w1c, bc, ALU.mult, ALU.add, [raw, vcol], [cv])
                P.stt(cv.ap[:, 1:W - 1], raw.ap[:, 0:W - 2], w0c, cv.ap[:, 1:W - 1], ALU.mult, ALU.add, [raw, vcol, cv], [cv])
                if c6 < 2:
                    P.stt(uT.ap[:, c6, 1:W - 1], raw.ap[:, 2:W], w2c, cv.ap[:, 1:W - 1], ALU.mult, ALU.add, [raw, vcol, cv], [uT])
                elif c6 < 4:
                    P.stt(cv.ap[:, 1:W - 1], raw.ap[:, 2:W], w2c, cv.ap[:, 1:W - 1], ALU.mult, ALU.add, [raw, vcol, cv], [cv])
                    P.tt('dve', uT.ap[:, c6 - 2, 1:W - 1], uT.ap[:, c6 - 2, 1:W - 1], cv.ap[:, 1:W - 1], ALU.mult, [uT, cv], [uT])
                else:
                    P.stt(x0c.ap[:, c6 - 4, 1:W - 1], raw.ap[:, 2:W], w2c, cv.ap[:, 1:W - 1], ALU.mult, ALU.add,
                          [raw, vcol, cv], [x0c])
            P.fence()
        P.es = es2

        P.ckpt("hp")
        for i in range(NT):
            if i < 2 and not do_ctx:
                continue
            c = hcol(i)
            pt = pT[i % 2]
            for cj in range(2):
                P.tr(pt.ap[:, cj * 128:(cj + 1) * 128], uT.ap[:, cj, c:c + 128], ident_bf.ap[:], [uT, ident_bf], [pt])
            dC, dS, dt = (RCc, RSc, i) if i < 2 else (RC, RS, i - 2)
            P.cp('act', dC.ap[:, dt, 0:256], pt.ap[:, 0:256], [pt], [dC])
            P.cp('dve', dS.ap[:, dt, 0:256], pt.ap[:, 0:256], [pt], [dS])

        P.ckpt("ht")
        Ysp = P.sb("h_Y", [128, 16, 512], BF16)
        Yc = P.sb("h_Yc", [128, 2, 512], BF16)
        kre = P.sb("h_kre", [128, 256], F32)
        kim = P.sb("h_kim", [128, 256], F32)
        ta = [P.sb("h_ta%d" % i, [128, 256], F32) for i in range(4)]
        fin = P.sb("h_fin", [128, 512], F32)
        cbuf = [P.sb("h_cb%d" % i, [128, 16, 128], BF16) for i in range(2)]
        sbuf_ = [P.sb("h_sb%d" % i, [128, 16, 128], BF16) for i in range(2)]
        ibc = [P.sb("h_ibc%d" % i, [128, 512], BF16) for i in range(2)]
        ibs = [P.sb("h_ibs%d" % i, [128, 512], BF16) for i in range(2)]
        rot = [0]

        def dft_conv(RCx, RSx, Yx, nT, tag, wfx, tgroups, col_base, tok_base):
            Ct, St, Cn, Sn = hy["Ct_" + tag], hy["St_" + tag], hy["Cn_" + tag], hy["Sn_" + tag]
            for fc in range(nT):
                b = fc % 2
                P.dma('sp', cbuf[b].ap[:, 0:nT, :], Ct[fc], writes=[cbuf[b]])
                P.dma('sp', sbuf_[b].ap[:, 0:nT, :], St[fc], writes=[sbuf_[b]])
                pc, ps_ = pf[2 * b], pf[2 * b + 1]
                for tc in range(nT):
                    P.mm(pc.ap[:, 0:512], cbuf[b].ap[:, tc, :], RCx.ap[:, tc, :], tc == 0, tc == nT - 1, [cbuf[b], RCx], [pc])
                for tc in range(nT):
                    P.mm(ps_.ap[:, 0:512], sbuf_[b].ap[:, tc, :], RSx.ap[:, tc, :], tc == 0, tc == nT - 1, [sbuf_[b], RSx], [ps_])
                P.act(kre.ap[:], pc.ap[:, 256:512], AF.Identity, [pc, wfx], [kre], scale=wfx.ap[:, fc:fc + 1])
                P.act(kim.ap[:], ps_.ap[:, 256:512], AF.Identity, [ps_, wfx], [kim], scale=wfx.ap[:, fc:fc + 1])
                P.tt('dve', ta[0].ap[:], pc.ap[:, 0:256], kre.ap[:], ALU.mult, [pc, kre], [ta[0]])
                P.tt('dve', ta[1].ap[:], ps_.ap[:, 0:256], kim.ap[:], ALU.mult, [ps_, kim], [ta[1]])
                P.tt('dve', Yx.ap[:, fc, 0:256], ta[0].ap[:], ta[1].ap[:], ALU.subtract, [ta[0], ta[1]], [Yx])
                P.tt('dve', ta[2].ap[:], pc.ap[:, 0:256], kim.ap[:], ALU.mult, [pc, kim], [ta[2]])
                P.tt('dve', ta[3].ap[:], ps_.ap[:, 0:256], kre.ap[:], ALU.mult, [ps_, kre], [ta[3]])
                P.tt('dve', Yx.ap[:, fc, 256:512], ta[2].ap[:], ta[3].ap[:], ALU.add, [ta[2], ta[3]], [Yx])
            P.ckpt("hf" + tag)
            for gi, (t0, nt) in enumerate(tgroups):
                py = [pf[4], pf[5]]
                for fc in range(nT):
                    bb = rot[0] % 2
                    rot[0] += 1
                    P.dma('sp', ibc[bb].ap[:, 0:nt], Cn[fc * 128:(fc + 1) * 128, t0:t0 + nt], writes=[ibc[bb]])
                    P.dma('sp', ibs[bb].ap[:, 0:nt], Sn[fc * 128:(fc + 1) * 128, t0:t0 + nt], writes=[ibs[bb]])
                    for cj in range(2):
                        P.mm(py[cj].ap[:, 0:nt], Yx.ap[:, fc, cj * 128:(cj + 1) * 128], ibc[bb].ap[:, 0:nt], fc == 0, False,
                             [Yx, ibc[bb]], [py[cj]])
                        P.mm(py[cj].ap[:, 0:nt], Yx.ap[:, fc, 256 + cj * 128:384 + cj * 128], ibs[bb].ap[:, 0:nt], False,
                             fc == nT - 1, [Yx, ibs[bb]], [py[cj]])
                for cj in range(2):
                    cs = slice(col_base + t0, col_base + t0 + nt)
                    tk0 = tok_base + t0
                    tiles = list(range(tk0 // 128, (tk0 + nt) // 128))
                    P.stt(fin.ap[:, 0:nt], uT.ap[:, cj, cs], vcol.ap[:, 24 + cj:25 + cj], py[cj].ap[:, 0:nt], ALU.mult, ALU.add,
                          [uT, vcol, py[cj]], [fin])
                    P.tt('dve', fin.ap[:, 0:nt], fin.ap[:, 0:nt], x0c.ap[:, cj, cs], ALU.mult, [fin, x0c], [fin])
                    P.tt('dve', mixT.ap[:, 2 + cj, tk0:tk0 + nt], fin.ap[:, 0:nt], sgT.ap[:, cj, cs], ALU.mult, [fin, sgT],
                         [mixb[1][t] for t in tiles])

        dft_conv(RC, RS, Ysp, 16, "l", wfl, [(0, 512), (512, 512), (1024, 512), (1536, 512)], 262, 256)
        if do_ctx:
            dft_conv(RCc, RSc, Yc, 2, "c", wfc, [(0, 256)], 2, 0)
        P.fence()
    P.es = L["es"]


BRANCH_BUILDERS["hyena"] = branch_hyena


def kernel(**inputs):
    nc = build(n_layers=4)
    maps = make_in_maps(inputs)
    res = run_bass_kernel_spmd(nc, maps, core_ids=list(range(8)))
    out = np.stack([np.asarray(res.results[b]["y"]).astype(np.float32) for b in range(8)], 0)
    return out
```

```python
import numpy as np
from contextlib import ExitStack
import concourse.bass as bass
import concourse.mybir as mybir
from concourse.bass_utils import run_bass_kernel_spmd

F32 = mybir.dt.float32
BF16 = mybir.dt.bfloat16
AF = mybir.ActivationFunctionType
ALU = mybir.AluOpType
AX = mybir.AxisListType

ND_SEMS = 40
VERBOSE = False
STOP = None


class Buf:
    def __init__(self, name, ap=None):
        self.name = name
        self.ap = ap
        self.writer = None
        self.readers = {}
        self.excl = False

    def __getitem__(self, k):
        return self.ap[k]


class Prog:
    def __init__(self, nc, es):
        self.nc = nc
        self.es = es
        self.engs = ['pe', 'act', 'dve', 'pool', 'sp']
        self.lists = {e: [] for e in self.engs}
        self.cnt = {e: 0 for e in self.engs}
        self.sem = {e: es.enter_context(nc.semaphore("s_" + e)) for e in ['pe', 'act', 'dve', 'pool']}
        self.dsem = [es.enter_context(nc.semaphore("d%d" % i)) for i in range(ND_SEMS)]
        self.dcnt = [0] * ND_SEMS
        self.dnext = 0
        self.dnext_sw = 0
        self.waited = {e: {} for e in self.engs}
        self.out_tokens = []
        self.nbuf = 0
        self.dead = False

    def sb(self, name, shape, dtype):
        self.nbuf += 1
        name = "%s_u%d" % (name, self.nbuf)
        t = self.es.enter_context(self.nc.sbuf_tensor(name, shape, dtype))
        assert self.nc.sbuf_bytes_remaining >= 16384 + 256, (name, self.nc.sbuf_bytes_remaining)
        return Buf(name, t)

    def ps(self, name, shape, dtype):
        t = self.es.enter_context(self.nc.psum_tensor(name, shape, dtype))
        b = Buf(name, t)
        b.excl = True
        return b

    def vb(self, name, ap=None):
        return Buf(name, ap)

    def _collect(self, eng, reads, writes):
        deps = []
        for b in reads:
            if b.writer is not None:
                deps.append(b.writer)
        for b in writes:
            if b.writer is not None:
                deps.append(b.writer)
            deps.extend(b.readers.values())
        waits = []
        w = self.waited[eng]
        for tok in deps:
            if tok[0] == 'e':
                if tok[1] == 'pe' and eng == 'pe':
                    continue
                key = tok[1]
                sem = self.sem[tok[1]]
            else:
                key = ('d', tok[1])
                sem = self.dsem[tok[1]]
            if w.get(key, 0) >= tok[2]:
                continue
            w[key] = tok[2]
            waits.append((sem, tok[2], key))
        best = {}
        for sem, val, key in waits:
            if key not in best or best[key][1] < val:
                best[key] = (sem, val)
        return list(best.values())

    def _mark(self, tok, rkey, reads, writes):
        for b in reads:
            b.readers[rkey] = tok
        for b in writes:
            b.writer = tok
            b.readers = {}

    def ckpt(self, name):
        if STOP is not None and STOP == name:
            self.dead = True

    def op(self, eng, fn, reads=(), writes=()):
        if self.dead:
            return
        if eng != 'pe':
            ex = [b for b in reads if b.excl]
            if ex:
                writes = list(writes) + [b for b in ex if b not in writes]
        waits = self._collect(eng, reads, writes)
        self.cnt[eng] += 1
        tok = ('e', eng, self.cnt[eng])
        self.lists[eng].append((waits, fn, ('inc', self.sem[eng])))
        self._mark(tok, eng, reads, writes)

    def dma(self, q, out_ap, in_ap, reads=(), writes=(), out=False, **kw):
        if self.dead and not out:
            return
        waits = self._collect(q, reads, writes)
        if q == 'pool':
            i = ND_SEMS - 8 + self.dnext_sw
            self.dnext_sw = (self.dnext_sw + 1) % 8
        else:
            i = self.dnext
            self.dnext = (self.dnext + 1) % (ND_SEMS - 8)
        if self.dcnt[i] > 0:
            key = ('d', i)
            val = self.dcnt[i] * 16
            if self.waited[q].get(key, 0) < val:
                self.waited[q][key] = val
                waits.append((self.dsem[i], val))
        self.dcnt[i] += 1
        tok = ('d', i, self.dcnt[i] * 16)
        fn = lambda e, o=out_ap, s=in_ap, k=kw: e.dma_start(out=o, in_=s, **k)
        self.lists[q].append((waits, fn, ('dinc', self.dsem[i])))
        self._mark(tok, ('d', i), reads, writes)
        if out:
            self.out_tokens.append(tok)

    def finish(self):
        waits = []
        for tok in self.out_tokens:
            waits.append((self.dsem[tok[1]], tok[2]))
        self.lists['sp'].append((waits, None, None))
        nc = self.nc
        lists = self.lists

        def replay(name, e):
            for waits, fn, inc in lists[name]:
                for sem, val in waits:
                    e.wait_ge(sem, val)
                if fn is None:
                    continue
                ins = fn(e)
                if inc[0] == 'inc':
                    ins.then_inc(inc[1], 1)
                else:
                    ins.then_inc(inc[1], 16)

        with nc.Block() as block:
            @block.sync
            def _(e):
                replay('sp', e)

            @block.tensor
            def _(e):
                replay('pe', e)

            @block.scalar
            def _(e):
                replay('act', e)

            @block.vector
            def _(e):
                replay('dve', e)

            @block.gpsimd
            def _(e):
                replay('pool', e)


    def mm(self, out, lhsT, rhs, start, stop, reads, writes):
        self.op('pe', lambda e: e.matmul(out, lhsT, rhs, start=start, stop=stop), reads, writes)

    def tr(self, out, in_, ident, reads, writes):
        self.op('pe', lambda e: e.transpose(out, in_, ident), reads, writes)

    def act(self, out, in_, func, reads, writes, **kw):
        self.op('act', lambda e: e.activation(out, in_, func, **kw), reads, writes)

    def tt(self, eng, out, in0, in1, op, reads, writes):
        self.op(eng, lambda e: e.tensor_tensor(out, in0, in1, op), reads, writes)

    def ts(self, eng, out, in0, s1, s2, op0, op1, reads, writes):
        self.op(eng, lambda e: e.tensor_scalar(out, in0, s1, s2, op0, op1), reads, writes)

    def stt(self, out, in0, scalar, in1, op0, op1, reads, writes):
        self.op('dve', lambda e: e.scalar_tensor_tensor(out, in0, scalar, in1, op0, op1), reads, writes)

    def cp(self, eng, out, in_, reads, writes):
        if eng == 'act':
            self.op('act', lambda e: e.copy(out, in_), reads, writes)
        else:
            self.op(eng, lambda e: e.tensor_copy(out, in_), reads, writes)

    def fence(self):
        for e in self.engs:
            waits = []
            for o in ['pe', 'act', 'dve', 'pool']:
                if not (o == e == 'pe') and self.cnt[o] > 0 and self.waited[e].get(o, 0) < self.cnt[o]:
                    self.waited[e][o] = self.cnt[o]
                    waits.append((self.sem[o], self.cnt[o]))
            for i in range(ND_SEMS):
                if self.dcnt[i] > 0 and self.waited[e].get(('d', i), 0) < self.dcnt[i] * 16:
                    self.waited[e][('d', i)] = self.dcnt[i] * 16
                    waits.append((self.dsem[i], self.dcnt[i] * 16))
            self.lists[e].append((waits, None, None))


D = 1024
NT = 18
TOK = NT * 128
EPS = 1e-6
N_IN = 4144
COL_A, COL_H, COL_G, COL_D = 0, 1296, 2320, 3376


def host_consts():
    c = {}
    pos = np.arange(2048)
    row = (pos // 64).astype(np.float64)
    col = (pos % 64).astype(np.float64)
    inv = 10000.0 ** (-np.arange(16, dtype=np.float64) / 16)
    ang = np.concatenate([row[:, None] * inv, col[:, None] * inv], -1)
    c["ropec"] = np.ascontiguousarray(np.cos(ang).reshape(16, 128, 32).transpose(1, 0, 2)).astype(np.float32)
    c["ropes"] = np.ascontiguousarray(np.sin(ang).reshape(16, 128, 32).transpose(1, 0, 2)).astype(np.float32)
    t = np.arange(128)[:, None]
    s_ = np.arange(128)[None, :]
    m = np.zeros((128, 384), np.float32)
    m[:, 0:128] = np.where(s_ >= t, 0.0, -30000.0)
    m[:, 256:384] = np.where(s_ <= t, 0.0, -30000.0)
    c["amask"] = m
    import ml_dtypes
    bf = ml_dtypes.bfloat16
    for tag, Lh in (("l", 2048), ("c", 256)):
        pos = np.arange(Lh, dtype=np.float32)
        t = (pos / np.float32(max(Lh - 1, 1))).astype(np.float32)
        w = (np.float32(2.0 * np.pi) * pos / np.float32(Lh)).astype(np.float32)
        f = np.linspace(1e-4, 15, 16, dtype=np.float32)
        z = np.concatenate([t[:, None], np.cos(w[:, None] * f), -np.sin(w[:, None] * f)], -1).astype(np.float32)
        c["zT_" + tag] = np.ascontiguousarray(z.T)
        nt_ = Lh // 128
        c["ntcol_" + tag] = np.ascontiguousarray((-t).reshape(nt_, 128).T)
        N = 2 * Lh - 1
        idx = (np.outer(np.arange(Lh, dtype=np.int64), np.arange(Lh, dtype=np.int64)) % N).astype(np.float64)
        ang = 2.0 * np.pi * idx / N
        C = np.cos(ang)
        S = np.sin(ang)
        c["Cn_" + tag] = np.ascontiguousarray(C.astype(np.float32).astype(bf))
        c["Sn_" + tag] = np.ascontiguousarray(S.astype(np.float32).astype(bf))
        c["Ct_" + tag] = np.ascontiguousarray(C.reshape(nt_, 128, nt_, 128).transpose(2, 1, 0, 3).astype(np.float32).astype(bf))
        c["St_" + tag] = np.ascontiguousarray(S.reshape(nt_, 128, nt_, 128).transpose(2, 1, 0, 3).astype(np.float32).astype(bf))
        wf = np.full(Lh, 2.0 / N, np.float32)
        wf[0] = 1.0 / N
        c["wf_" + tag] = np.ascontiguousarray(wf.reshape(nt_, 128).T)
    import math
    dmin, dmax = math.log(1e-2) / 1.5, math.log(1e-2) / 0.3
    c["deltas"] = np.abs(np.linspace(dmin, dmax, 256, dtype=np.float32)).reshape(1, 256).astype(np.float32)
    return c


def build(n_layers=4, tap=None, branches=("att", "mlstm", "gla", "hyena")):
    nc = bass.Bass("TRN2", target_bir_lowering=False)

    def din(name, shape, dt=F32):
        return nc.dram_tensor(name, shape, dt, kind="ExternalInput").ap()

    x_in = din("x", [2048, D])
    ctx_in = din("ctx", [256, D])
    cc_in = din("cc", [16, 128])
    w_ada = din("w_ada", [4, D, 3 * D])
    b_ada = din("b_ada", [4, 3 * D])
    g_pre = din("g_pre", [4, D])
    g_post = din("g_post", [4, D])
    w_in = din("w_in", [4, D, N_IN])
    w_out = din("w_out", [4, D, D])
    mlstm_gate_b = din("mlstm_gate_b", [4, 16])
    mlstm_norm_g = din("mlstm_norm_g", [4, 256])
    gla_norm_g = din("gla_norm_g", [4, 256])
    gla_w_alpha = din("gla_w_alpha", [4, 2, 16, 256])
    gla_b_alpha = din("gla_b_alpha", [4, 2, 256])
    attn_sink = din("attn_sink", [4, 4])
    ropec_in = din("ropec", [128, 16, 32])
    ropes_in = din("ropes", [128, 16, 32])
    amask_in = din("amask", [128, 384])
    hy = {}
    for nm, shp in (("hyena_conv_w", [4, 3, 768]), ("hyena_conv_b", [4, 768]), ("hyena_w1", [4, 33, 64]),
                    ("hyena_b1", [4, 64]), ("hyena_w2", [4, 64, 64]), ("hyena_b2", [4, 64]), ("hyena_w3", [4, 64, 512]),
                    ("hyena_b3", [4, 512]), ("hyena_freq", [4, 2, 64]), ("hyena_d", [4, 256]), ("deltas", [1, 256])):
        hy[nm] = din(nm, shp)
    for tag, Lh in (("l", 2048), ("c", 256)):
        nt_ = Lh // 128
        hy["zT_" + tag] = din("zT_" + tag, [33, Lh])
        hy["ntcol_" + tag] = din("ntcol_" + tag, [128, nt_])
        hy["wf_" + tag] = din("wf_" + tag, [128, nt_])
        hy["Cn_" + tag] = din("Cn_" + tag, [Lh, Lh], BF16)
        hy["Sn_" + tag] = din("Sn_" + tag, [Lh, Lh], BF16)
        hy["Ct_" + tag] = din("Ct_" + tag, [nt_, 128, nt_, 128], BF16)
        hy["St_" + tag] = din("St_" + tag, [nt_, 128, nt_, 128], BF16)
    y_out = nc.dram_tensor("y", [2048, D], F32, kind="ExternalOutput").ap()
    ctxs = nc.dram_tensor("ctxs", [256, D], F32).ap()
    tap_out = None
    if tap is not None:
        tap_out = nc.dram_tensor("tap", [128, 8, TOK], BF16, kind="ExternalOutput").ap()

    w_in_v = [w_in[l].rearrange("(kc p) n -> p kc n", p=128) for l in range(4)]
    w_out_v = [w_out[l].rearrange("(kc p) n -> p kc n", p=128) for l in range(4)]
    w_ada_v = [w_ada[l].rearrange("(kc p) n -> p kc n", p=128) for l in range(4)]

    with ExitStack() as es:
        P = Prog(nc, es)
        ident_bf = P.sb("ident_bf", [128, 128], BF16)
        ident_f = P.sb("ident_f", [128, 128], F32)
        tri_le = P.sb("tri_le", [128, 128], F32)
        tri_ge = P.sb("tri_ge", [128, 128], F32)
        tri_gt = P.sb("tri_gt", [128, 128], F32)
        tri_lt = P.sb("tri_lt", [128, 128], F32)
        ropec = P.sb("ropec_sb", [128, 16, 32], F32)
        ropes = P.sb("ropes_sb", [128, 16, 32], F32)
        amask = P.sb("amask_sb", [128, 384], F32)

        def mk_affine(buf, pattern, cm, cmp_op, fill_in=1.0):
            P.op('pool', lambda e: e.memset(buf.ap[:], fill_in), writes=[buf])
            P.op('pool', lambda e: e.affine_select(buf.ap[:], buf.ap[:], pattern=pattern, compare_op=cmp_op,
                                                   fill=0.0, base=0, channel_multiplier=cm), reads=[buf], writes=[buf])

        mk_affine(ident_bf, [[-1, 128]], 1, ALU.is_equal)
        mk_affine(ident_f, [[-1, 128]], 1, ALU.is_equal)
        mk_affine(tri_le, [[1, 128]], -1, ALU.is_ge)
        mk_affine(tri_ge, [[-1, 128]], 1, ALU.is_ge)
        mk_affine(tri_gt, [[-1, 128]], 1, ALU.is_gt)
        mk_affine(tri_lt, [[1, 128]], -1, ALU.is_gt)
        P.dma('sp', ropec.ap[:], ropec_in, writes=[ropec])
        P.dma('sp', ropes.ap[:], ropes_in, writes=[ropes])
        P.dma('sp', amask.ap[:], amask_in, writes=[amask])

        pT = [P.ps("pT%d" % i, [128, 1024], BF16) for i in range(2)]
        pf = [P.ps("pf%d" % i, [128, 512], F32) for i in range(6)]

        MA = P.sb("MA", [128, 4, 8, 2], F32)
        MB = P.sb("MB", [128, 4, 8, 2], F32)
        MG = P.sb("MG", [128, 4, 8, 2], F32)

        with ExitStack() as es2:
            P.es = es2
            ccr = P.sb("ccr", [16, 128], F32)
            scs = P.sb("scs", [128, 16], F32)
            sc2 = P.sb("sc2", [128, 8, 2], F32)
            VR1 = P.sb("VR1", [128, 128], F32)
            VR2 = P.sb("VR2", [32, 128], F32)
            VC1 = P.sb("VC1", [128, 128], F32)
            VC2 = P.sb("VC2", [128, 32], F32)
            modc = P.sb("modc", [128, 24, 2], F32)
            wa = [P.sb("wa%d" % i, [128, 8, 512], F32) for i in range(2)]
            P.dma('sp', ccr.ap[:], cc_in, writes=[ccr])
            P.dma('sp', VR1.ap[0:96, :], b_ada.rearrange("l (c p) -> (l c) p", p=128), writes=[VR1])
            P.dma('sp', VR1.ap[96:128, :], g_pre.rearrange("l (c p) -> (l c) p", p=128), writes=[VR1])
            P.dma('sp', VR2.ap[:], g_post.rearrange("l (c p) -> (l c) p", p=128), writes=[VR2])
            P.tr(pf[0].ap[:, 0:16], ccr.ap[:], ident_f.ap[0:16, 0:16], [ccr, ident_f], [pf[0]])
            P.act(scs.ap[:], pf[0].ap[:, 0:16], AF.Silu, [pf[0]], [scs])
            P.cp('dve', sc2.ap[:, :, 0], scs.ap[:, 0:8], [scs], [sc2])
            P.cp('dve', sc2.ap[:, :, 1], scs.ap[:, 8:16], [scs], [sc2])
            P.tr(pf[1].ap[:, 0:128], VR1.ap[:], ident_f.ap[:], [VR1, ident_f], [pf[1]])
            P.cp('dve', VC1.ap[:], pf[1].ap[:, 0:128], [pf[1]], [VC1])
            P.tr(pf[2].ap[:, 0:32], VR2.ap[:], ident_f.ap[0:32, 0:32], [VR2, ident_f], [pf[2]])
            P.cp('dve', VC2.ap[:], pf[2].ap[:, 0:32], [pf[2]], [VC2])
            gi = 0
            for l in range(n_layers):
                pm = pf[3 + (l % 2)]
                for g6 in range(6):
                    wb_ = wa[gi % 2]
                    gi += 1
                    P.dma('sp', wb_.ap[:], w_ada_v[l][:, :, g6 * 512:(g6 + 1) * 512], writes=[wb_])
                    for jj in range(4):
                        j = g6 * 4 + jj
                        for kc in range(8):
                            P.mm(pm.ap[:, j * 2:j * 2 + 2], wb_.ap[:, kc, jj * 128:(jj + 1) * 128], sc2.ap[:, kc, :],
                                 kc == 0, kc == 7, [wb_, sc2], [pm])
                pm3 = pm.ap[:, 0:48].rearrange("p (j s) -> p j s", s=2)
                P.tt('dve', modc.ap[:], pm3, VC1.ap[:, l * 24:(l + 1) * 24].unsqueeze(2).to_broadcast([128, 24, 2]),
                     ALU.add, [pm, VC1], [modc])
                gpre_bc = VC1.ap[:, 96 + l * 8:96 + (l + 1) * 8].unsqueeze(2).to_broadcast([128, 8, 2])
                gpost_bc = VC2.ap[:, l * 8:(l + 1) * 8].unsqueeze(2).to_broadcast([128, 8, 2])
                P.stt(MA.ap[:, l], modc.ap[:, 8:16, :], 1.0, gpre_bc, ALU.add, ALU.mult, [modc, VC1], [MA])
                P.cp('dve', MB.ap[:, l], modc.ap[:, 0:8, :], [modc], [MB])
                P.tt('dve', MG.ap[:, l], modc.ap[:, 16:24, :], gpost_bc, ALU.mult, [modc, VC2], [MG])
            P.fence()
        P.es = es

        hT = P.sb("hT", [128, 8, TOK], BF16)
        mixT = P.sb("mixT", [128, 8, TOK], BF16)
        hTb = [P.vb("hT_t%d" % i) for i in range(NT)]
        mixb = [[P.vb("mix_%d_%d" % (g, i)) for i in range(NT)] for g in range(4)]
        if tap is not None:
            P.op('pool', lambda e: e.memset(mixT.ap[:], 0.0), writes=[b for g in mixb for b in g])
        wbuf = []
        wrot = [0]

        def alloc_wbuf():
            wbuf[:] = [P.sb("wbuf%d" % i, [128, 8, 512], BF16) for i in range(2)]
        xs = [P.vb("xs%d" % i) for i in range(NT)]
        if VERBOSE:
            print("sbuf bytes remaining after main alloc:", nc.sbuf_bytes_remaining)

        def load_w(l, c0, ncols, q='pool'):
            b = wbuf[wrot[0] % 2]
            wrot[0] += 1
            P.dma(q, b.ap[:, :, 0:ncols], w_in_v[l][:, :, c0:c0 + ncols], writes=[b])
            return b

        def proj_tm(i, wb_, c0, ncols, out_ap, outbuf):
            for kc in range(8):
                P.mm(out_ap, hT.ap[:, kc, i * 128:(i + 1) * 128], wb_.ap[:, kc, c0:c0 + ncols], kc == 0, kc == 7,
                     [hTb[i], wb_], [outbuf])

        def proj_fm(wb_, c0, ncols, t0, nt, out_ap, outbuf):
            rd = [wb_] + [hTb[i] for i in range(t0 // 128, (t0 + nt + 127) // 128)]
            for kc in range(8):
                P.mm(out_ap, wb_.ap[:, kc, c0:c0 + ncols], hT.ap[:, kc, t0:t0 + nt], kc == 0, kc == 7, rd, [outbuf])

        def state_src(l, i):
            if i < 2:
                return (ctx_in if l == 0 else ctxs)[i * 128:(i + 1) * 128, :]
            return (x_in if l == 0 else y_out)[(i - 2) * 128:(i - 1) * 128, :]

        def state_dst(i):
            if i < 2:
                return ctxs[i * 128:(i + 1) * 128, :]
            return y_out[(i - 2) * 128:(i - 1) * 128, :]

        for l in range(n_layers):
            last = (l == 3)
            esC = ExitStack()
            P.es = esC
            xt = [P.sb("xt%d" % i, [128, 1024], F32) for i in range(2)]
            xn = [P.sb("xn%d" % i, [128, 1024], BF16) for i in range(2)]
            tmpf = P.sb("tmpf", [128, 1024], F32)
            st4 = [P.sb("st4_%d" % i, [128, 8], F32) for i in range(2)]
            for i in range(NT):
                s = 1 if i < 2 else 0
                xb_, xnb, stb = xt[i % 2], xn[i % 2], st4[i % 2]
                P.dma('sp', xb_.ap[:], state_src(l, i), reads=[xs[i]], writes=[xb_])
                P.act(tmpf.ap[:], xb_.ap[:], AF.Square, [xb_], [tmpf, stb], accum_out=stb.ap[:, 0:1])
                P.act(stb.ap[:, 1:2], stb.ap[:, 0:1], AF.Sqrt, [stb], [stb], bias=EPS, scale=1.0 / D)
                P.op('dve', lambda e, o=stb.ap[:, 2:3], a=stb.ap[:, 1:2]: e.reciprocal(o, a), [stb], [stb])
                P.act(xnb.ap[:], xb_.ap[:], AF.Identity, [xb_, stb], [xnb], scale=stb.ap[:, 2:3])
                pt = pT[i % 2]
                for kc in range(8):
                    P.tr(pt.ap[:, kc * 128:(kc + 1) * 128], xnb.ap[:, kc * 128:(kc + 1) * 128], ident_bf.ap[:],
                         [xnb, ident_bf], [pt])
                pt3 = pt.ap[:, :].rearrange("p (k t) -> p k t", t=128)
                tm3 = tmpf.ap[:, :].rearrange("p (k t) -> p k t", t=128)
                P.tt('dve', tm3, pt3, MA.ap[:, l, :, s:s + 1].to_broadcast([128, 8, 128]), ALU.mult, [pt, MA], [tmpf])
                P.tt('dve', hT.ap[:, :, i * 128:(i + 1) * 128], tm3, MB.ap[:, l, :, s:s + 1].to_broadcast([128, 8, 128]),
                     ALU.add, [tmpf, MB], [hTb[i]])
            if tap == "h%d" % l:
                P.dma('sp', tap_out, hT.ap[:], reads=hTb, out=True)

            P.fence()
            esC.close()
            P.es = es

            BRANCH_BUILDERS["att"](locals()) if "att" in branches else None
            BRANCH_BUILDERS["mlstm"](locals()) if "mlstm" in branches else None
            BRANCH_BUILDERS["gla"](locals()) if "gla" in branches else None
            BRANCH_BUILDERS["hyena"](locals()) if "hyena" in branches else None
            if tap == "mix%d" % l:
                P.dma('sp', tap_out, mixT.ap[:], reads=[b for g in mixb for b in g], out=True)

            esZ = ExitStack()
            P.es = esZ
            woutb = P.sb("woutb", [128, 8, 1024], BF16)
            Gbc = [P.sb("Gbc%d" % s, [128, 1024], F32) for s in range(2)]
            gbt = P.sb("gbt", [128, 128], F32)
            xt = [P.sb("xt%d" % i, [128, 1024], F32) for i in range(2)]
            tmpf = P.sb("tmpf", [128, 1024], F32)
            st4 = [P.sb("st4_%d" % i, [128, 8], F32) for i in range(2)]
            for h2 in range(2):
                P.dma('pool', woutb.ap[:, :, h2 * 512:(h2 + 1) * 512], w_out_v[l][:, :, h2 * 512:(h2 + 1) * 512],
                      writes=[woutb])
            for s in range(2):
                for j in range(8):
                    P.cp('dve', gbt.ap[:], MG.ap[:, l, j, s:s + 1].to_broadcast([128, 128]), [MG], [gbt])
                    pg = pf[j // 4]
                    P.mm(pg.ap[:, (j % 4) * 128:(j % 4 + 1) * 128], gbt.ap[:], ident_f.ap[:], True, True,
                         [gbt, ident_f], [pg])
                for h2 in range(2):
                    P.cp('act', Gbc[s].ap[:, h2 * 512:(h2 + 1) * 512], pf[h2].ap[:], [pf[h2]], [Gbc[s]])
            for i in range(NT):
                if last and i < 2:
                    continue
                s = 1 if i < 2 else 0
                xb_, stb = xt[i % 2], st4[i % 2]
                P.dma('sp', xb_.ap[:], state_src(l, i), reads=[xs[i]], writes=[xb_])
                py = [pf[2 + 2 * (i % 2)], pf[3 + 2 * (i % 2)]]
                mrd = [mixb[g][i] for g in range(4)]
                for h2 in range(2):
                    for kc in range(8):
                        P.mm(py[h2].ap[:], mixT.ap[:, kc, i * 128:(i + 1) * 128], woutb.ap[:, kc, h2 * 512:(h2 + 1) * 512],
                             kc == 0, kc == 7, mrd + [woutb], [py[h2]])
                for h2 in range(2):
                    P.act(tmpf.ap[:, h2 * 512:(h2 + 1) * 512], py[h2].ap[:], AF.Square, [py[h2]], [tmpf, stb],
                          accum_out=stb.ap[:, 4 + h2:5 + h2])
                P.tt('dve', stb.ap[:, 6:7], stb.ap[:, 4:5], stb.ap[:, 5:6], ALU.add, [stb], [stb])
                P.act(stb.ap[:, 7:8], stb.ap[:, 6:7], AF.Sqrt, [stb], [stb], bias=EPS, scale=1.0 / D)
                P.op('dve', lambda e, o=stb.ap[:, 3:4], a=stb.ap[:, 7:8]: e.reciprocal(o, a), [stb], [stb])
                for h2 in range(2):
                    sl = slice(h2 * 512, (h2 + 1) * 512)
                    P.stt(tmpf.ap[:, sl], py[h2].ap[:], stb.ap[:, 3:4], Gbc[s].ap[:, sl], ALU.mult, ALU.mult,
                          [py[h2], stb, Gbc[s]], [tmpf])
                P.tt('dve', xb_.ap[:], xb_.ap[:], tmpf.ap[:], ALU.add, [xb_, tmpf], [xb_])
                P.dma('sp', state_dst(i), xb_.ap[:], reads=[xb_], writes=[xs[i]], out=(l == n_layers - 1 and i >= 2))
            P.fence()
            esZ.close()
            P.es = es
        P.finish()
    return nc


BRANCH_BUILDERS = {}


def branch_att(L):
    P, nc, l, last = L["P"], L["nc"], L["l"], L["last"]
    hT, hTb, mixT, mixb, pT, pf = L["hT"], L["hTb"], L["mixT"], L["mixb"], L["pT"], L["pf"]
    ident_bf, ropec, ropes, amask = L["ident_bf"], L["ropec"], L["ropes"], L["amask"]
    load_w, proj_tm = L["load_w"], L["proj_tm"]
    with ExitStack() as es2:
        P.es = es2
        L["alloc_wbuf"]()
        QKT = P.sb("QKT", [128, 4, TOK], BF16)
        QKb = [P.vb("QKT_%d" % i) for i in range(NT)]
        v_tm = P.sb("v_tm", [128, NT, 128], BF16)
        sg_tm = P.sb("sg_tm", [128, NT, 256], BF16)
        vb_ = [P.vb("vsg_%d" % i) for i in range(NT)]
        qkf = P.sb("qkf", [128, 384], F32)
        rt = [P.sb("rt%d" % i, [128, 192], F32) for i in range(2)]
        rq = P.sb("rq", [128, 512], BF16)
        sink_bc = P.sb("sink_bc", [128, 4], F32)
        sm2 = [P.sb("sm%d" % r, [128, 640], F32) for r in range(2)]
        Pb2 = [P.sb("Pb%d" % r, [128, 640], BF16) for r in range(2)]
        PTs2 = [P.sb("PTs%d" % r, [128, 5, 128], BF16) for r in range(2)]
        stt2 = [P.sb("att_st%d" % r, [128, 8], F32) for r in range(2)]
        rden2 = [P.sb("rden%d" % r, [128, 4], F32) for r in range(2)]
        og2 = [P.sb("og%d" % r, [128, 256], BF16) for r in range(2)]
        P.dma('sp', sink_bc.ap[:], L["attn_sink"][l:l + 1, :].partition_broadcast(128), writes=[sink_bc])
        w1 = load_w(l, COL_D, 384)
        w2 = load_w(l, COL_D + 384, 384)
        for i in range(NT):
            pq, pv = pf[0], pf[1]
            proj_tm(i, w1, 0, 384, pq.ap[:, 0:384], pq)
            proj_tm(i, w2, 0, 384, pv.ap[:, 0:384], pv)
            P.cp('act', v_tm.ap[:, i, :], pv.ap[:, 0:128], [pv], [vb_[i]])
            P.act(sg_tm.ap[:, i, :], pv.ap[:, 128:384], AF.Silu, [pv], [vb_[i]])
            if i >= 2:
                P.cp('act', qkf.ap[:], pq.ap[:, 0:384], [pq], [qkf])
                q4 = qkf.ap[:, :].rearrange("p (h two j) -> p h two j", two=2, j=32)
                r4 = rq.ap[:, 0:384].rearrange("p (h two j) -> p h two j", two=2, j=32)
                x1, x2 = q4[:, :, 0, :], q4[:, :, 1, :]
                cosb = ropec.ap[:, i - 2, :].unsqueeze(1).to_broadcast([128, 6, 32])
                sinb = ropes.ap[:, i - 2, :].unsqueeze(1).to_broadcast([128, 6, 32])
                ta = rt[0].ap[:, :].rearrange("p (h j) -> p h j", j=32)
                tb = rt[1].ap[:, :].rearrange("p (h j) -> p h j", j=32)
                P.tt('dve', ta, x1, cosb, ALU.mult, [qkf, ropec], [rt[0]])
                P.tt('dve', tb, x2, sinb, ALU.mult, [qkf, ropes], [rt[1]])
                P.tt('dve', r4[:, :, 0, :], ta, tb, ALU.subtract, [rt[0], rt[1]], [rq])
                P.tt('dve', ta, x2, cosb, ALU.mult, [qkf, ropec], [rt[0]])
                P.tt('dve', tb, x1, sinb, ALU.mult, [qkf, ropes], [rt[1]])
                P.tt('dve', r4[:, :, 1, :], ta, tb, ALU.add, [rt[0], rt[1]], [rq])
            else:
                P.cp('act', rq.ap[:, 0:384], pq.ap[:, 0:384], [pq], [rq])
            kd_src = rq.ap[:, 256:384].rearrange("p (g o d) -> p g o d", o=1, d=64).to_broadcast([128, 2, 2, 64])
            P.cp('dve', rt[0].ap[:, 0:128].bitcast(BF16).rearrange("p (g o d) -> p g o d", o=2, d=64), kd_src,
                 [rq], [rt[0]])
            P.cp('dve', rq.ap[:, 256:512], rt[0].ap[:, 0:128].bitcast(BF16), [rt[0]], [rq])
            pt = pT[i % 2]
            for c4 in range(4):
                P.tr(pt.ap[:, c4 * 128:(c4 + 1) * 128], rq.ap[:, c4 * 128:(c4 + 1) * 128], ident_bf.ap[:],
                     [rq, ident_bf], [pt])
            P.cp('dve', QKT.ap[:, :, i * 128:(i + 1) * 128], pt.ap[:, 0:512].rearrange("p (c t) -> p c t", t=128),
                 [pt], [QKb[i]])

        def attn_block(i):
            has_local = i >= 2
            n = i - 2
            if has_local:
                nlo, nhi = max(n - 1, 0), min(n + 1, 15)
                c0 = (nlo - (n - 1)) * 128
                c1 = c0 + (nhi - nlo + 1) * 128
                ktiles = list(range(2 + nlo, 2 + nhi + 1))
            else:
                c0 = c1 = 384
                ktiles = []
            d0 = 384 - (c1 - c0)
            blocks = [(kt, d0 + 128 * bi) for bi, kt in enumerate(ktiles)] + [(0, 384), (1, 512)]
            pO = pf[2 + (i % 2)]
            rden, og = rden2[i % 2], og2[i % 2]
            for h in range(4):
                g, base = h // 2, (h % 2) * 64
                r = h % 2
                sm, Pb, PTs, stt_ = sm2[r], Pb2[r], PTs2[r], stt2[r]
                pl, pc = (pf[4], pf[5]) if r == 0 else (pf[0], pf[1])
                qa = QKT.ap[base:base + 64, g, i * 128:(i + 1) * 128]
                if has_local:
                    P.mm(pl.ap[:, d0:384], qa, QKT.ap[base:base + 64, 2 + g, ktiles[0] * 128:(ktiles[-1] + 1) * 128],
                         True, True, [QKb[i]] + [QKb[k] for k in ktiles], [pl])
                    P.tt('dve', sm.ap[:, d0:384], pl.ap[:, d0:384], amask.ap[:, c0:c1], ALU.add, [pl, amask], [sm])
                P.mm(pc.ap[:, 0:256], qa, QKT.ap[base:base + 64, 2 + g, 0:256], True, True, [QKb[i], QKb[0], QKb[1]], [pc])
                P.cp('act', sm.ap[:, 384:640], pc.ap[:, 0:256], [pc], [sm])
                P.op('dve', lambda e, o=stt_.ap[:, 0:1], a=sm.ap[:, d0:640]: e.reduce_max(o, a, AX.X), [sm], [stt_])
                P.ts('dve', stt_.ap[:, 1:2], stt_.ap[:, 0:1], -0.125, None, ALU.mult, ALU.bypass, [stt_], [stt_])
                P.act(Pb.ap[:, d0:640], sm.ap[:, d0:640], AF.Exp, [sm, stt_], [Pb, stt_], bias=stt_.ap[:, 1:2], scale=0.125,
                      accum_out=stt_.ap[:, 2:3])
                P.act(stt_.ap[:, 3:4], stt_.ap[:, 1:2], AF.Exp, [stt_, sink_bc], [stt_], bias=sink_bc.ap[:, h:h + 1], scale=1.0)
                P.tt('dve', stt_.ap[:, 4:5], stt_.ap[:, 2:3], stt_.ap[:, 3:4], ALU.add, [stt_], [stt_])
                P.op('dve', lambda e, o=rden.ap[:, h:h + 1], a=stt_.ap[:, 4:5]: e.reciprocal(o, a), [stt_], [rden])
                pt = pT[h % 2]
                for bi, (kt, cc) in enumerate(blocks):
                    P.tr(pt.ap[:, bi * 128:(bi + 1) * 128], Pb.ap[:, cc:cc + 128], ident_bf.ap[:], [Pb, ident_bf], [pt])
                nb = len(blocks)
                P.cp('act', PTs.ap[:, 0:nb, :], pt.ap[:, 0:nb * 128].rearrange("p (c t) -> p c t", t=128), [pt], [PTs])
                for bi, (kt, cc) in enumerate(blocks):
                    P.mm(pO.ap[:, h * 64:(h + 1) * 64], PTs.ap[:, bi, :], v_tm.ap[:, kt, g * 64:(g + 1) * 64],
                         bi == 0, bi == nb - 1, [PTs, vb_[kt]], [pO])
            for h in range(4):
                P.stt(og.ap[:, h * 64:(h + 1) * 64], pO.ap[:, h * 64:(h + 1) * 64], rden.ap[:, h:h + 1],
                      sg_tm.ap[:, i, h * 64:(h + 1) * 64], ALU.mult, ALU.mult, [pO, rden, vb_[i]], [og])
            pt = pT[i % 2]
            for c2 in range(2):
                P.tr(pt.ap[:, c2 * 128:(c2 + 1) * 128], og.ap[:, c2 * 128:(c2 + 1) * 128], ident_bf.ap[:], [og, ident_bf], [pt])
            P.cp('act', mixT.ap[:, 6:8, i * 128:(i + 1) * 128], pt.ap[:, 0:256].rearrange("p (c t) -> p c t", t=128),
                 [pt], [mixb[3][i]])

        for i in range(NT):
            if last and i < 2:
                continue
            attn_block(i)
        P.fence()
    P.es = L["es"]


BRANCH_BUILDERS["att"] = branch_att


def make_in_maps(inputs):
    c = host_consts()
    shared = {}
    for k in ["w_ada", "b_ada", "g_pre", "g_post", "w_in", "w_out", "mlstm_norm_g", "gla_norm_g", "gla_w_alpha",
              "gla_b_alpha", "attn_sink"]:
        shared[k] = np.ascontiguousarray(np.asarray(inputs[k], dtype=np.float32))
    for k in ["hyena_conv_w", "hyena_conv_b", "hyena_w1", "hyena_b1", "hyena_w2", "hyena_b2", "hyena_w3", "hyena_b3",
              "hyena_freq", "hyena_d"]:
        shared[k] = np.ascontiguousarray(np.asarray(inputs[k], dtype=np.float32))
    shared["mlstm_gate_b"] = np.ascontiguousarray(np.asarray(inputs["mlstm_gate_b"], dtype=np.float32).reshape(4, 16))
    shared.update(c)
    maps = []
    c_ctx = np.asarray(inputs["c_ctx"], dtype=np.float32)
    for b in range(8):
        m = dict(shared)
        m["x"] = np.ascontiguousarray(np.asarray(inputs["x"][b], dtype=np.float32))
        m["ctx"] = np.ascontiguousarray(np.asarray(inputs["ctx"][b], dtype=np.float32))
        m["cc"] = np.ascontiguousarray(np.concatenate([np.asarray(inputs["c"][b], dtype=np.float32), c_ctx]).reshape(16, 128))
        maps.append(m)
    return maps


def branch_scan(L, kind):
    ml = kind == "mlstm"
    P, nc, l, last = L["P"], L["nc"], L["l"], L["last"]
    hT, hTb, mixT, mixb, pT, pf = L["hT"], L["hTb"], L["mixT"], L["mixb"], L["pT"], L["pf"]
    ident_bf, ident_f = L["ident_bf"], L["ident_f"]
    load_w, proj_tm, proj_fm = L["load_w"], L["proj_tm"], L["proj_fm"]
    base = COL_A if ml else COL_G
    cdec = 1.0 if ml else 1.0 / 16
    mc0, mg = (0, 0) if ml else (4, 2)
    qscale, kscale = (1.0, 0.125) if ml else (0.125, 1.0)
    with ExitStack() as es2:
        P.es = es2
        L["alloc_wbuf"]()
        qT = P.sb("s_qT", [128, 2, TOK], BF16)
        kT = P.sb("s_kT", [128, 2, TOK], BF16)
        k_tm = P.sb("s_ktm", [128, NT, 256], BF16)
        v_aug = P.sb("s_vaug", [128, NT, 512], BF16)
        Hh = P.sb("s_H", [128, NT, 256], F32)
        gr = P.sb("s_gr", [128, NT, 32], F32)
        qkb = [P.vb("s_qkb%d" % i) for i in range(NT)]
        kvb = [P.vb("s_kvb%d" % i) for i in range(NT)]
        Hb = [P.vb("s_Hb%d" % i) for i in range(NT)]
        grb = [P.vb("s_grb%d" % i) for i in range(NT)]
        normg = P.sb("s_normg", [128, 256], F32)
        P.dma('sp', normg.ap[:], (L["mlstm_norm_g"] if ml else L["gla_norm_g"])[l:l + 1, :].partition_broadcast(128),
              writes=[normg])
        if ml:
            gbb = P.sb("s_gbb", [128, 16], F32)
            P.dma('sp', gbb.ap[:], L["mlstm_gate_b"][l:l + 1, :].partition_broadcast(128), writes=[gbb])
            posi = P.sb("s_posi", [128, 256], F32)
            negi = P.sb("s_negi", [128, 256], F32)
            e4 = P.sb("s_e4", [128, 8], F32)
        else:
            wal = P.sb("s_wal", [17, 2, 256], F32)
            P.dma('sp', wal.ap[0:16, :, :], L["gla_w_alpha"][l].rearrange("d r c -> r d c"), writes=[wal])
            P.dma('sp', wal.ap[16:17, :, :], L["gla_b_alpha"][l:l + 1, :, :], writes=[wal])
            rTa = P.sb("s_rTa", [17, 128], F32)
            P.op('pool', lambda e: e.memset(rTa.ap[:], 1.0), writes=[rTa])
            e_tm = P.sb("s_etm", [128, 256], F32)
        sp_tm = P.sb("s_sp", [128, 256], F32)
        EqT2 = [P.sb("s_EqT%d" % r, [128, 256], F32) for r in range(2)]
        EkT = P.sb("s_EkT", [128, 256], F32)
        Ex = P.sb("s_Ex", [128, 256], F32)
        qp2 = [P.sb("s_qp%d" % r, [128, 2, 128], BF16) for r in range(2)]
        kpz = [P.sb("s_kpz%d" % h, [128, 128], BF16) for h in range(4)]
        for h in range(4):
            P.op('pool', lambda e, a=kpz[h].ap[:]: e.memset(a, 0.0), writes=[kpz[h]])
        kpp2 = [P.sb("s_kpp%d" % r, [128, 256], BF16) for r in range(2)]
        S_m2 = [P.sb("s_Sm%d" % r, [128, 512], BF16) for r in range(2)]
        St_f = [P.sb("s_Stf%d" % j, [128, 128], F32) for j in range(2)]
        St_b = [P.sb("s_Stb%d" % h, [128, 128], BF16) for h in range(4)]
        dn = P.sb("s_dn", [128, 8], F32)
        tmpH = P.sb("s_tmpH", [128, 256], F32)

        wqk = load_w(l, base, 512)
        groups = [(0, 512), (512, 512), (1024, 512), (1536, 512), (2048, 256)]
        cnt = 0
        for j in range(2):
            for (t0, nt) in groups:
                tiles = list(range(t0 // 128, (t0 + nt) // 128))
                for which, dst, sc_ in ((0, qT, qscale), (1, kT, kscale)):
                    pq = pf[cnt % 2]
                    cnt += 1
                    proj_fm(wqk, which * 256 + j * 128, 128, t0, nt, pq.ap[:, 0:nt], pq)
                    P.act(dst.ap[:, j, t0:t0 + nt], pq.ap[:, 0:nt], AF.Identity, [pq], [qkb[t] for t in tiles], scale=sc_)
        wkv = load_w(l, base + 256, 512)
        ng = 16 if ml else 32
        wg = load_w(l, base + (1024 if ml else 768), 256 + ng)
        P.op('pool', lambda e: e.memset(v_aug.ap[:], 1.0), writes=kvb)
        for i in range(NT):
            pk = pf[2 + i % 2]
            proj_tm(i, wkv, 0, 512, pk.ap[:], pk)
            P.act(k_tm.ap[:, i, :], pk.ap[:, 0:256], AF.Identity, [pk], [kvb[i]], scale=kscale)
            P.cp('dve', v_aug.ap[:, i, :].rearrange("p (h e) -> p h e", e=128)[:, :, 0:64],
                 pk.ap[:, 256:512].rearrange("p (h e) -> p h e", e=64), [pk], [kvb[i]])
            pg = pf[4 + i % 2]
            proj_tm(i, wg, 256, 64, pg.ap[:, 0:64], pg)
            if ml:
                P.tt('dve', gr.ap[:, i, 0:16], pg.ap[:, 0:16], gbb.ap[:], ALU.add, [pg, gbb], [grb[i]])
            else:
                P.cp('dve', gr.ap[:, i, 0:32], pg.ap[:, 0:32], [pg], [grb[i]])

        P.ckpt("pre")
        for dirn in range(2):
            order = ([0, 1] + list(range(2, NT))) if dirn == 0 else ([1, 0] + list(range(NT - 1, 1, -1)))
            tri_c = L["tri_le"] if dirn == 0 else L["tri_ge"]
            tri_x = L["tri_gt"] if dirn == 0 else L["tri_lt"]
            dcol = 127 if dirn == 0 else 0
            for j in range(2):
                P.op('pool', lambda e, a=St_f[j].ap[:]: e.memset(a, 0.0), writes=[St_f[j]])
            for h in range(4):
                P.op('pool', lambda e, a=St_b[h].ap[:]: e.memset(a, 0.0), writes=[St_b[h]])
            for step, i in enumerate(order):
                tsl = slice(i * 128, (i + 1) * 128)
                EqT, qp, kpp, S_m = EqT2[step % 2], qp2[step % 2], kpp2[step % 2], S_m2[step % 2]
                if ml:
                    ic, fc = dirn * 8, dirn * 8 + 4
                    P.act(e4.ap[:, 0:4], gr.ap[:, i, fc:fc + 4], AF.Exp, [grb[i]], [e4], scale=-1.0)
                    P.act(e4.ap[:, 4:8], e4.ap[:, 0:4], AF.Ln, [e4], [e4], bias=1.0)
                    P.cp('dve', sp_tm.ap[:, :].rearrange("p (h d) -> p h d", d=64),
                         e4.ap[:, 4:8].unsqueeze(2).to_broadcast([128, 4, 64]), [e4], [sp_tm])
                    ib = gr.ap[:, i, ic:ic + 4].unsqueeze(2).to_broadcast([128, 4, 64])
                    P.cp('dve', posi.ap[:, :].rearrange("p (h d) -> p h d", d=64), ib, [grb[i]], [posi])
                    P.ts('dve', negi.ap[:, :].rearrange("p (h d) -> p h d", d=64), ib, -1.0, None, ALU.mult, ALU.bypass,
                         [grb[i]], [negi])
                else:
                    P.tr(pf[3].ap[0:16, 256:384], gr.ap[:, i, dirn * 16:(dirn + 1) * 16], ident_f.ap[:],
                         [grb[i], ident_f], [pf[3]])
                    P.cp('act', rTa.ap[0:16, :], pf[3].ap[0:16, 256:384], [pf[3]], [rTa])
                    P.mm(pf[3].ap[:, 256:512], rTa.ap[0:17, :], wal.ap[0:17, dirn, :], True, True, [rTa, wal], [pf[3]])
                    P.act(e_tm.ap[:], pf[3].ap[:, 256:512], AF.Exp, [pf[3]], [e_tm], scale=-1.0)
                    P.act(sp_tm.ap[:], e_tm.ap[:], AF.Ln, [e_tm], [sp_tm], bias=1.0)
                P.ckpt("s1")
                pc, px = pf[4], pf[3]
                for j in range(2):
                    P.mm(pc.ap[:, j * 128:(j + 1) * 128], sp_tm.ap[:, j * 128:(j + 1) * 128], tri_c.ap[:], True, True,
                         [sp_tm, tri_c], [pc])
                if ml:
                    for j in range(2):
                        P.mm(pc.ap[:, 256 + j * 128:384 + j * 128], sp_tm.ap[:, j * 128:(j + 1) * 128], tri_c.ap[:],
                             True, False, [sp_tm, tri_c], [pc])
                        P.mm(pc.ap[:, 256 + j * 128:384 + j * 128], posi.ap[:, j * 128:(j + 1) * 128], ident_f.ap[:],
                             False, True, [posi, ident_f], [pc])
                P.mm(px.ap[:, 0:256], tri_x.ap[:], sp_tm.ap[:], True, not ml, [tri_x, sp_tm], [px])
                if ml:
                    P.mm(px.ap[:, 0:256], ident_f.ap[:], negi.ap[:], False, True, [ident_f, negi], [px])
                P.ckpt("s2")
                P.act(EqT.ap[:], pc.ap[:, 0:256], AF.Exp, [pc], [EqT], scale=-cdec)
                P.act(EkT.ap[:], pc.ap[:, 256:512] if ml else pc.ap[:, 0:256], AF.Exp, [pc], [EkT], scale=cdec)
                P.act(Ex.ap[:], px.ap[:, 0:256], AF.Exp, [px], [Ex], scale=-cdec)
                P.tt('dve', qp.ap[:], qT.ap[:, :, tsl], EqT.ap[:, :].rearrange("p (j t) -> p j t", t=128), ALU.mult,
                     [qkb[i], EqT], [qp])
                for h in range(4):
                    j, b0 = h // 2, (h % 2) * 64
                    P.tt('dve', kpz[h].ap[b0:b0 + 64, :], kT.ap[b0:b0 + 64, j, tsl], EkT.ap[b0:b0 + 64, j * 128:(j + 1) * 128],
                         ALU.mult, [qkb[i], EkT], [kpz[h]])
                P.tt('dve', kpp.ap[:], k_tm.ap[:, i, :], Ex.ap[:], ALU.mult, [kvb[i], Ex], [kpp])
                P.ckpt("s3")
                pS = pf[2]
                for h in range(4):
                    j, b0 = h // 2, (h % 2) * 64
                    P.mm(pS.ap[:, h * 128:(h + 1) * 128], kpz[h].ap[:], qp.ap[:, j, :], True, True, [kpz[h], qp], [pS])
                P.tt('dve', S_m.ap[:, :].rearrange("p (h t) -> p h t", t=128),
                     pS.ap[:, :].rearrange("p (h t) -> p h t", t=128),
                     tri_c.ap[:, :].unsqueeze(1).to_broadcast([128, 4, 128]), ALU.mult, [pS, tri_c], [S_m])
                P.ckpt("s4")
                pO = pf[step % 2]
                for h in range(4):
                    j, b0 = h // 2, (h % 2) * 64
                    P.mm(pO.ap[:, h * 128:(h + 1) * 128], S_m.ap[:, h * 128:(h + 1) * 128], v_aug.ap[:, i, h * 128:(h + 1) * 128],
                         True, False, [S_m, kvb[i]], [pO])
                    P.mm(pO.ap[:, h * 128:(h + 1) * 128], qp.ap[:, j, :], St_b[h].ap[:], False, True, [qp, St_b[h]], [pO])
                P.ckpt("s5")
                pD = pf[5]
                for j in range(2):
                    P.mm(pD.ap[:, j * 256:(j + 1) * 256], kpp.ap[:, j * 128:(j + 1) * 128], v_aug.ap[:, i, j * 256:(j + 1) * 256],
                         True, True, [kpp, kvb[i]], [pD])
                for h in range(4):
                    j, b0 = h // 2, (h % 2) * 64
                    P.stt(St_f[j].ap[b0:b0 + 64, :], St_f[j].ap[b0:b0 + 64, :], EqT.ap[b0:b0 + 64, j * 128 + dcol:j * 128 + dcol + 1],
                          pD.ap[b0:b0 + 64, j * 256 + (h % 2) * 128:j * 256 + (h % 2) * 128 + 128], ALU.mult, ALU.add,
                          [St_f[j], EqT, pD], [St_f[j]])
                for h in range(4):
                    j, b0 = h // 2, (h % 2) * 64
                    P.cp('act', St_b[h].ap[b0:b0 + 64, :], St_f[j].ap[b0:b0 + 64, :], [St_f[j]], [St_b[h]])
                P.ckpt("s6")
                pO3 = pO.ap[:, 0:512].rearrange("p (h e) -> p h e", e=128)
                H3 = Hh.ap[:, i, :].rearrange("p (h d) -> p h d", d=64)
                if ml:
                    P.act(dn.ap[:, 0:4], pO3[:, :, 64], AF.Abs, [pO], [dn])
                    P.ts('dve', dn.ap[:, 0:4], dn.ap[:, 0:4], 1.0, None, ALU.max, ALU.bypass, [dn], [dn])
                    P.op('dve', lambda e, o=dn.ap[:, 4:8], a=dn.ap[:, 0:4]: e.reciprocal(o, a), [dn], [dn])
                    rb = dn.ap[:, 4:8].unsqueeze(2).to_broadcast([128, 4, 64])
                    if dirn == 0:
                        P.tt('dve', H3, pO3[:, :, 0:64], rb, ALU.mult, [pO, dn], [Hb[i]])
                    else:
                        P.tt('dve', tmpH.ap[:, :].rearrange("p (h d) -> p h d", d=64), pO3[:, :, 0:64], rb, ALU.mult,
                             [pO, dn], [tmpH])
                        P.tt('dve', Hh.ap[:, i, :], Hh.ap[:, i, :], tmpH.ap[:], ALU.add, [Hb[i], tmpH], [Hb[i]])
                else:
                    if dirn == 0:
                        P.cp('act', H3, pO3[:, :, 0:64], [pO], [Hb[i]])
                    else:
                        P.tt('dve', H3, pO3[:, :, 0:64], H3, ALU.add, [pO, Hb[i]], [Hb[i]])

        P.ckpt("scan")
        ncol = 512 if ml else 256
        wfin = load_w(l, base + 768, ncol)
        sig = P.sb("s_sig", [128, 256], F32)
        sgt = P.sb("s_sgt", [128, 256], F32)
        gs = P.sb("s_gs", [128, 256], F32)
        hh = P.sb("s_hh", [128, 256], F32)
        sq = P.sb("s_sq", [128, 256], F32)
        og = P.sb("s_og", [128, 256], BF16)
        for i in range(NT):
            if last and i < 2:
                continue
            pg = pf[i % 2]
            proj_tm(i, wfin, 0, ncol, pg.ap[:, 0:ncol], pg)
            if ml:
                P.act(sig.ap[:], pg.ap[:, 0:256], AF.Sigmoid, [pg], [sig])
                P.tt('dve', hh.ap[:], Hh.ap[:, i, :], sig.ap[:], ALU.mult, [Hb[i], sig], [hh])
                gate_ap = pg.ap[:, 256:512]
            else:
                P.cp('dve', hh.ap[:], Hh.ap[:, i, :], [Hb[i]], [hh])
                gate_ap = pg.ap[:, 0:256]
            P.act(sgt.ap[:], gate_ap, AF.Silu, [pg], [sgt])
            P.tt('dve', gs.ap[:], sgt.ap[:], normg.ap[:], ALU.mult, [sgt, normg], [gs])
            P.tt('dve', sq.ap[:], hh.ap[:], hh.ap[:], ALU.mult, [hh], [sq])
            P.op('dve', lambda e, o=dn.ap[:, 0:4], a=sq.ap[:, :].rearrange("p (h d) -> p h d", d=64): e.reduce_sum(o, a, AX.X),
                 [sq], [dn])
            P.act(dn.ap[:, 4:8], dn.ap[:, 0:4], AF.Sqrt, [dn], [dn], bias=EPS, scale=1.0 / 64)
            P.op('dve', lambda e, o=dn.ap[:, 0:4], a=dn.ap[:, 4:8]: e.reciprocal(o, a), [dn], [dn])
            P.tt('dve', sq.ap[:, :].rearrange("p (h d) -> p h d", d=64), hh.ap[:, :].rearrange("p (h d) -> p h d", d=64),
                 dn.ap[:, 0:4].unsqueeze(2).to_broadcast([128, 4, 64]), ALU.mult, [hh, dn], [sq])
            P.tt('dve', og.ap[:], sq.ap[:], gs.ap[:], ALU.mult, [sq, gs], [og])
            pt = pT[i % 2]
            for c2 in range(2):
                P.tr(pt.ap[:, c2 * 128:(c2 + 1) * 128], og.ap[:, c2 * 128:(c2 + 1) * 128], ident_bf.ap[:], [og, ident_bf], [pt])
            P.cp('act', mixT.ap[:, mc0:mc0 + 2, i * 128:(i + 1) * 128], pt.ap[:, 0:256].rearrange("p (c t) -> p c t", t=128),
                 [pt], [mixb[mg][i]])
        P.fence()
    P.es = L["es"]


BRANCH_BUILDERS["mlstm"] = lambda L: branch_scan(L, "mlstm")
BRANCH_BUILDERS["gla"] = lambda L: branch_scan(L, "gla")


def hcol(i):
    return 2 + i * 128 if i < 2 else 262 + (i - 2) * 128


def branch_hyena(L):
    P, nc, l, last, hy = L["P"], L["nc"], L["l"], L["last"], L["hy"]
    hT, hTb, mixT, mixb, pT, pf = L["hT"], L["hTb"], L["mixT"], L["mixb"], L["pT"], L["pf"]
    ident_bf, ident_f = L["ident_bf"], L["ident_f"]
    load_w, proj_fm = L["load_w"], L["proj_fm"]
    PI = 3.1415925
    TWO_PI = 6.283185307179586
    W = 2312
    with ExitStack() as es2:
        P.es = es2
        RC = P.sb("h_RC", [128, 16, 512], BF16)
        RS = P.sb("h_RS", [128, 16, 512], BF16)
        RCc = P.sb("h_RCc", [128, 2, 512], BF16)
        RSc = P.sb("h_RSc", [128, 2, 512], BF16)
        uT = P.sb("h_uT", [128, 2, W], BF16)
        x0c = P.sb("h_x0c", [128, 2, W], BF16)
        sgT = P.sb("h_sgT", [128, 2, W], BF16)
        vcol = P.sb("h_vcol", [128, 26], F32)
        fb = P.sb("h_fb", [64, 6], F32)
        wfl = P.sb("h_wfl", [128, 16], F32)
        wfc = P.sb("h_wfc", [128, 2], F32)
        ntl = P.sb("h_ntl", [128, 16], F32)
        ntc = P.sb("h_ntc", [128, 2], F32)
        delt = P.sb("h_delt", [128, 256], F32)
        P.dma('sp', wfl.ap[:], hy["wf_l"], writes=[wfl])
        P.dma('sp', wfc.ap[:], hy["wf_c"], writes=[wfc])
        P.dma('sp', ntl.ap[:], hy["ntcol_l"], writes=[ntl])
        P.dma('sp', ntc.ap[:], hy["ntcol_c"], writes=[ntc])
        P.dma('sp', delt.ap[:], hy["deltas"].partition_broadcast(128), writes=[delt])
        do_ctx = not last

        with ExitStack() as es3:
            P.es = es3
            vr = P.sb("h_vr", [26, 128], F32)
            fr = P.sb("h_fr", [4, 64], F32)
            P.dma('sp', vr.ap[0:18, :], hy["hyena_conv_w"][l].rearrange("j (c p) -> (j c) p", p=128), writes=[vr])
            P.dma('sp', vr.ap[18:24, :], hy["hyena_conv_b"][l].rearrange("(c p) -> c p", p=128), writes=[vr])
            P.dma('sp', vr.ap[24:26, :], hy["hyena_d"][l].rearrange("(c p) -> c p", p=128), writes=[vr])
            P.dma('sp', fr.ap[0:1, :], hy["hyena_b1"][l:l + 1, :], writes=[fr])
            P.dma('sp', fr.ap[1:2, :], hy["hyena_b2"][l:l + 1, :], writes=[fr])
            P.dma('sp', fr.ap[2:4, :], hy["hyena_freq"][l], writes=[fr])
            P.tr(pf[0].ap[:, 0:26], vr.ap[:], ident_f.ap[0:26, 0:26], [vr, ident_f], [pf[0]])
            P.cp('dve', vcol.ap[:], pf[0].ap[:, 0:26], [pf[0]], [vcol])
            P.tr(pf[1].ap[0:64, 0:4], fr.ap[:], ident_f.ap[0:4, 0:4], [fr, ident_f], [pf[1]])
            P.cp('dve', fb.ap[:, 0:4], pf[1].ap[0:64, 0:4], [pf[1]], [fb])
            P.tt('dve', fb.ap[:, 4:6], fb.ap[:, 0:2], fb.ap[:, 2:4], ALU.mult, [fb], [fb])

            P.ckpt("hv")
            zTl = P.sb("h_zTl", [33, 2048], F32)
            zTc = P.sb("h_zTc", [33, 256], F32)
            w1s = P.sb("h_w1", [33, 64], F32)
            w2s = P.sb("h_w2", [64, 64], F32)
            w3a = P.sb("h_w3a", [65, 512], F32)
            arg = P.sb("h_arg", [64, 512], F32)
            t1 = P.sb("h_t1", [64, 512], F32)
            t2 = P.sb("h_t2", [64, 512], F32)
            h1 = P.sb("h_h1", [64, 512], F32)
            h2 = P.sb("h_h2", [65, 512], F32)
            win = P.sb("h_win", [128, 256], F32)
            hf = P.sb("h_hf", [128, 256], F32)
            hb = P.sb("h_hb", [128, 256], F32)
            P.dma('sp', zTl.ap[:], hy["zT_l"], writes=[zTl])
            P.dma('sp', zTc.ap[:], hy["zT_c"], writes=[zTc])
            P.dma('sp', w1s.ap[:], hy["hyena_w1"][l], writes=[w1s])
            P.dma('sp', w2s.ap[:], hy["hyena_w2"][l], writes=[w2s])
            P.dma('sp', w3a.ap[0:64, :], hy["hyena_w3"][l], writes=[w3a])
            P.dma('sp', w3a.ap[64:65, :], hy["hyena_b3"][l:l + 1, :], writes=[w3a])
            P.op('pool', lambda e: e.memset(h2.ap[:], 1.0), writes=[h2])

            def sin_layer(ps_buf, ps_ap, fc_, fbc_, out_ap, outbuf, nn):
                P.act(arg.ap[:, 0:nn], ps_ap, AF.Identity, [ps_buf, fb], [arg], scale=fb.ap[:, fc_:fc_ + 1],
                      bias=fb.ap[:, fbc_:fbc_ + 1])
                P.ts('dve', t1.ap[:, 0:nn], arg.ap[:, 0:nn], PI, TWO_PI, ALU.is_gt, ALU.mult, [arg], [t1])
                P.ts('dve', t2.ap[:, 0:nn], arg.ap[:, 0:nn], -PI, TWO_PI, ALU.is_lt, ALU.mult, [arg], [t2])
                P.tt('dve', arg.ap[:, 0:nn], arg.ap[:, 0:nn], t1.ap[:, 0:nn], ALU.subtract, [arg, t1], [arg])
                P.tt('dve', arg.ap[:, 0:nn], arg.ap[:, 0:nn], t2.ap[:, 0:nn], ALU.add, [arg, t2], [arg])
                P.act(out_ap, arg.ap[:, 0:nn], AF.Sin, [arg], [outbuf])

            def mlp_block(zsrc, n0, nn, ntc_, RCd, RSd, tile0):
                P.mm(pf[0].ap[0:64, 0:nn], w1s.ap[:], zsrc.ap[:, n0:n0 + nn], True, True, [w1s, zsrc], [pf[0]])
                sin_layer(pf[0], pf[0].ap[0:64, 0:nn], 2, 4, h1.ap[:, 0:nn], h1, nn)
                P.mm(pf[1].ap[0:64, 0:nn], w2s.ap[:], h1.ap[:, 0:nn], True, True, [w2s, h1], [pf[1]])
                sin_layer(pf[1], pf[1].ap[0:64, 0:nn], 3, 5, h2.ap[0:64, 0:nn], h2, nn)
                for tt_ in range(nn // 128):
                    dt = tile0 + tt_
                    pp = pf[2 + tt_ % 2]
                    P.mm(pp.ap[:, 0:512], h2.ap[0:65, tt_ * 128:(tt_ + 1) * 128], w3a.ap[:], True, True, [h2, w3a], [pp])
                    P.act(win.ap[:], delt.ap[:], AF.Exp, [delt, ntc_], [win], scale=ntc_.ap[:, dt:dt + 1])
                    P.tt('dve', hf.ap[:], pp.ap[:, 0:256], win.ap[:], ALU.mult, [pp, win], [hf])
                    P.tt('dve', hb.ap[:], pp.ap[:, 256:512], win.ap[:], ALU.mult, [pp, win], [hb])
                    if dt == 0:
                        P.op('pool', lambda e: e.memset(hb.ap[0:1, :], 0.0), reads=[hb], writes=[hb])
                    P.tt('dve', RCd.ap[:, dt, 256:512], hf.ap[:], hb.ap[:], ALU.add, [hf, hb], [RCd])
                    P.tt('dve', RSd.ap[:, dt, 256:512], hf.ap[:], hb.ap[:], ALU.subtract, [hf, hb], [RSd])

            for blk in range(4):
                mlp_block(zTl, blk * 512, 512, ntl, RC, RS, blk * 4)
                P.ckpt("hm%d" % blk)
            if do_ctx:
                mlp_block(zTc, 0, 256, ntc, RCc, RSc, 0)
            P.fence()
        P.es = es2

        with ExitStack() as es3:
            P.es = es3
            L["alloc_wbuf"]()
            raw = P.sb("h_raw", [128, W], F32)
            cv = P.sb("h_cv", [128, W], F32)
            P.op('pool', lambda e: e.memset(raw.ap[:], 0.0), writes=[raw])
            wA = load_w(l, COL_H, 512)
            wB = load_w(l, COL_H + 512, 512)
            groups = [(0, 256), (256, 512), (768, 512), (1280, 512), (1792, 512)]
            cnt = 0
            for c6 in range(8):
                wsel = wA if c6 < 4 else wB
                coff = (c6 % 4) * 128
                for (t0, nt) in groups:
                    pp = pf[cnt % 2]
                    cnt += 1
                    proj_fm(wsel, coff, 128, t0, nt, pp.ap[:, 0:nt], pp)
                    c0 = 2 + t0 if t0 < 256 else t0 + 6
                    if c6 < 6:
                        P.cp('act', raw.ap[:, c0:c0 + nt], pp.ap[:, 0:nt], [pp], [raw])
                    else:
                        P.act(sgT.ap[:, c6 - 6, c0:c0 + nt], pp.ap[:, 0:nt], AF.Silu, [pp], [sgT])
                if c6 >= 6:
                    continue
                w0c, w1c, w2c = (vcol.ap[:, j * 6 + c6:j * 6 + c6 + 1] for j in range(3))
                bc = vcol.ap[:, 18 + c6:19 + c6]
                P.ts('dve', cv.ap[:, 1:W - 1], raw.ap[:, 1:W - 1], w1c, bc, ALU.mult, ALU.add, [raw, vcol], [cv])
                P.stt(cv.ap[:, 1:W - 1], raw.ap[:, 0:W - 2], w0c, cv.ap[:, 1:W - 1], ALU.mult, ALU.add, [raw, vcol, cv], [cv])
                if c6 < 2:
                    P.stt(uT.ap[:, c6, 1:W - 1], raw.ap[:, 2:W], w2c, cv.ap[:, 1:W - 1], ALU.mult, ALU.add, [raw, vcol, cv], [uT])
                elif c6 < 4:
                    P.stt(cv.ap[:, 1:W - 1], raw.ap[:, 2:W], w2c, cv.ap[:, 1:W - 1], ALU.mult, ALU.add, [raw, vcol, cv], [cv])
                    P.tt('dve', uT.ap[:, c6 - 2, 1:W - 1], uT.ap[:, c6 - 2, 1:W - 1], cv.ap[:, 1:W - 1], ALU.mult, [uT, cv], [uT])
                else:
                    P.stt(x0c.ap[:, c6 - 4, 1:W - 1], raw.ap[:, 2:W], w2c, cv.ap[:, 1:W - 1], ALU.mult, ALU.add,
                          [raw, vcol, cv], [x0c])
            P.fence()
        P.es = es2

        P.ckpt("hp")
        for i in range(NT):
            if i < 2 and not do_ctx:
                continue
            c = hcol(i)
            pt = pT[i % 2]
            for cj in range(2):
                P.tr(pt.ap[:, cj * 128:(cj + 1) * 128], uT.ap[:, cj, c:c + 128], ident_bf.ap[:], [uT, ident_bf], [pt])
            dC, dS, dt = (RCc, RSc, i) if i < 2 else (RC, RS, i - 2)
            P.cp('act', dC.ap[:, dt, 0:256], pt.ap[:, 0:256], [pt], [dC])
            P.cp('dve', dS.ap[:, dt, 0:256], pt.ap[:, 0:256], [pt], [dS])

        P.ckpt("ht")
        Ysp = P.sb("h_Y", [128, 16, 512], BF16)
        Yc = P.sb("h_Yc", [128, 2, 512], BF16)
        kre = P.sb("h_kre", [128, 256], F32)
        kim = P.sb("h_kim", [128, 256], F32)
        ta = [P.sb("h_ta%d" % i, [128, 256], F32) for i in range(4)]
        fin = P.sb("h_fin", [128, 512], F32)
        cbuf = [P.sb("h_cb%d" % i, [128, 16, 128], BF16) for i in range(2)]
        sbuf_ = [P.sb("h_sb%d" % i, [128, 16, 128], BF16) for i in range(2)]
        ibc = [P.sb("h_ibc%d" % i, [128, 512], BF16) for i in range(2)]
        ibs = [P.sb("h_ibs%d" % i, [128, 512], BF16) for i in range(2)]
        rot = [0]

        def dft_conv(RCx, RSx, Yx, nT, tag, wfx, tgroups, col_base, tok_base):
            Ct, St, Cn, Sn = hy["Ct_" + tag], hy["St_" + tag], hy["Cn_" + tag], hy["Sn_" + tag]
            for fc in range(nT):
                b = fc % 2
                P.dma('sp', cbuf[b].ap[:, 0:nT, :], Ct[fc], writes=[cbuf[b]])
                P.dma('sp', sbuf_[b].ap[:, 0:nT, :], St[fc], writes=[sbuf_[b]])
                pc, ps_ = pf[2 * b], pf[2 * b + 1]
                for tc in range(nT):
                    P.mm(pc.ap[:, 0:512], cbuf[b].ap[:, tc, :], RCx.ap[:, tc, :], tc == 0, tc == nT - 1, [cbuf[b], RCx], [pc])
                for tc in range(nT):
                    P.mm(ps_.ap[:, 0:512], sbuf_[b].ap[:, tc, :], RSx.ap[:, tc, :], tc == 0, tc == nT - 1, [sbuf_[b], RSx], [ps_])
                P.act(kre.ap[:], pc.ap[:, 256:512], AF.Identity, [pc, wfx], [kre], scale=wfx.ap[:, fc:fc + 1])
                P.act(kim.ap[:], ps_.ap[:, 256:512], AF.Identity, [ps_, wfx], [kim], scale=wfx.ap[:, fc:fc + 1])
                P.tt('dve', ta[0].ap[:], pc.ap[:, 0:256], kre.ap[:], ALU.mult, [pc, kre], [ta[0]])
                P.tt('dve', ta[1].ap[:], ps_.ap[:, 0:256], kim.ap[:], ALU.mult, [ps_, kim], [ta[1]])
                P.tt('dve', Yx.ap[:, fc, 0:256], ta[0].ap[:], ta[1].ap[:], ALU.subtract, [ta[0], ta[1]], [Yx])
                P.tt('dve', ta[2].ap[:], pc.ap[:, 0:256], kim.ap[:], ALU.mult, [pc, kim], [ta[2]])
                P.tt('dve', ta[3].ap[:], ps_.ap[:, 0:256], kre.ap[:], ALU.mult, [ps_, kre], [ta[3]])
                P.tt('dve', Yx.ap[:, fc, 256:512], ta[2].ap[:], ta[3].ap[:], ALU.add, [ta[2], ta[3]], [Yx])
            P.ckpt("hf" + tag)
            for gi, (t0, nt) in enumerate(tgroups):
                py = [pf[4], pf[5]]
                for fc in range(nT):
                    bb = rot[0] % 2
                    rot[0] += 1
                    P.dma('sp', ibc[bb].ap[:, 0:nt], Cn[fc * 128:(fc + 1) * 128, t0:t0 + nt], writes=[ibc[bb]])
                    P.dma('sp', ibs[bb].ap[:, 0:nt], Sn[fc * 128:(fc + 1) * 128, t0:t0 + nt], writes=[ibs[bb]])
                    for cj in range(2):
                        P.mm(py[cj].ap[:, 0:nt], Yx.ap[:, fc, cj * 128:(cj + 1) * 128], ibc[bb].ap[:, 0:nt], fc == 0, False,
                             [Yx, ibc[bb]], [py[cj]])
                        P.mm(py[cj].ap[:, 0:nt], Yx.ap[:, fc, 256 + cj * 128:384 + cj * 128], ibs[bb].ap[:, 0:nt], False,
                             fc == nT - 1, [Yx, ibs[bb]], [py[cj]])
                for cj in range(2):
                    cs = slice(col_base + t0, col_base + t0 + nt)
                    tk0 = tok_base + t0
                    tiles = list(range(tk0 // 128, (tk0 + nt) // 128))
                    P.stt(fin.ap[:, 0:nt], uT.ap[:, cj, cs], vcol.ap[:, 24 + cj:25 + cj], py[cj].ap[:, 0:nt], ALU.mult, ALU.add,
                          [uT, vcol, py[cj]], [fin])
                    P.tt('dve', fin.ap[:, 0:nt], fin.ap[:, 0:nt], x0c.ap[:, cj, cs], ALU.mult, [fin, x0c], [fin])
                    P.tt('dve', mixT.ap[:, 2 + cj, tk0:tk0 + nt], fin.ap[:, 0:nt], sgT.ap[:, cj, cs], ALU.mult, [fin, sgT],
                         [mixb[1][t] for t in tiles])

        dft_conv(RC, RS, Ysp, 16, "l", wfl, [(0, 512), (512, 512), (1024, 512), (1536, 512)], 262, 256)
        if do_ctx:
            dft_conv(RCc, RSc, Yc, 2, "c", wfc, [(0, 256)], 2, 0)
        P.fence()
    P.es = L["es"]


BRANCH_BUILDERS["hyena"] = branch_hyena


def kernel(**inputs):
    nc = build(n_layers=4)
    maps = make_in_maps(inputs)
    res = run_bass_kernel_spmd(nc, maps, core_ids=list(range(8)))
    out = np.stack([np.asarray(res.results[b]["y"]).astype(np.float32) for b in range(8)], 0)
    return out
```

```python
import numpy as np
from contextlib import ExitStack
import concourse.bass as bass
import concourse.mybir as mybir
from concourse.bass_utils import run_bass_kernel_spmd

F32 = mybir.dt.float32
BF16 = mybir.dt.bfloat16
AF = mybir.ActivationFunctionType
ALU = mybir.AluOpType
AX = mybir.AxisListType

ND_SEMS = 40
VERBOSE = False
STOP = None


class Buf:
    def __init__(self, name, ap=None):
        self.name = name
        self.ap = ap
        self.writer = None
        self.readers = {}
        self.excl = False

    def __getitem__(self, k):
        return self.ap[k]


class Prog:
    def __init__(self, nc, es):
        self.nc = nc
        self.es = es
        self.engs = ['pe', 'act', 'dve', 'pool', 'sp']
        self.lists = {e: [] for e in self.engs}
        self.cnt = {e: 0 for e in self.engs}
        self.sem = {e: es.enter_context(nc.semaphore("s_" + e)) for e in ['pe', 'act', 'dve', 'pool']}
        self.dsem = [es.enter_context(nc.semaphore("d%d" % i)) for i in range(ND_SEMS)]
        self.dcnt = [0] * ND_SEMS
        self.dnext = 0
        self.dnext_sw = 0
        self.waited = {e: {} for e in self.engs}
        self.out_tokens = []
        self.nbuf = 0
        self.dead = False

    def sb(self, name, shape, dtype):
        self.nbuf += 1
        name = "%s_u%d" % (name, self.nbuf)
        t = self.es.enter_context(self.nc.sbuf_tensor(name, shape, dtype))
        assert self.nc.sbuf_bytes_remaining >= 16384 + 256, (name, self.nc.sbuf_bytes_remaining)
        return Buf(name, t)

    def ps(self, name, shape, dtype):
        t = self.es.enter_context(self.nc.psum_tensor(name, shape, dtype))
        b = Buf(name, t)
        b.excl = True
        return b

    def vb(self, name, ap=None):
        return Buf(name, ap)

    def _collect(self, eng, reads, writes):
        deps = []
        for b in reads:
            if b.writer is not None:
                deps.append(b.writer)
        for b in writes:
            if b.writer is not None:
                deps.append(b.writer)
            deps.extend(b.readers.values())
        waits = []
        w = self.waited[eng]
        for tok in deps:
            if tok[0] == 'e':
                if tok[1] == 'pe' and eng == 'pe':
                    continue
                key = tok[1]
                sem = self.sem[tok[1]]
            else:
                key = ('d', tok[1])
                sem = self.dsem[tok[1]]
            if w.get(key, 0) >= tok[2]:
                continue
            w[key] = tok[2]
            waits.append((sem, tok[2], key))
        best = {}
        for sem, val, key in waits:
            if key not in best or best[key][1] < val:
                best[key] = (sem, val)
        return list(best.values())

    def _mark(self, tok, rkey, reads, writes):
        for b in reads:
            b.readers[rkey] = tok
        for b in writes:
            b.writer = tok
            b.readers = {}

    def ckpt(self, name):
        if STOP is not None and STOP == name:
            self.dead = True

    def op(self, eng, fn, reads=(), writes=()):
        if self.dead:
            return
        if eng != 'pe':
            ex = [b for b in reads if b.excl]
            if ex:
                writes = list(writes) + [b for b in ex if b not in writes]
        waits = self._collect(eng, reads, writes)
        self.cnt[eng] += 1
        tok = ('e', eng, self.cnt[eng])
        self.lists[eng].append((waits, fn, ('inc', self.sem[eng])))
        self._mark(tok, eng, reads, writes)

    def dma(self, q, out_ap, in_ap, reads=(), writes=(), out=False, **kw):
        if self.dead and not out:
            return
        waits = self._collect(q, reads, writes)
        if q == 'pool':
            i = ND_SEMS - 8 + self.dnext_sw
            self.dnext_sw = (self.dnext_sw + 1) % 8
        else:
            i = self.dnext
            self.dnext = (self.dnext + 1) % (ND_SEMS - 8)
        if self.dcnt[i] > 0:
            key = ('d', i)
            val = self.dcnt[i] * 16
            if self.waited[q].get(key, 0) < val:
                self.waited[q][key] = val
                waits.append((self.dsem[i], val))
        self.dcnt[i] += 1
        tok = ('d', i, self.dcnt[i] * 16)
        fn = lambda e, o=out_ap, s=in_ap, k=kw: e.dma_start(out=o, in_=s, **k)
        self.lists[q].append((waits, fn, ('dinc', self.dsem[i])))
        self._mark(tok, ('d', i), reads, writes)
        if out:
            self.out_tokens.append(tok)

    def finish(self):
        waits = []
        for tok in self.out_tokens:
            waits.append((self.dsem[tok[1]], tok[2]))
        self.lists['sp'].append((waits, None, None))
        nc = self.nc
        lists = self.lists

        def replay(name, e):
            for waits, fn, inc in lists[name]:
                for sem, val in waits:
                    e.wait_ge(sem, val)
                if fn is None:
                    continue
                ins = fn(e)
                if inc[0] == 'inc':
                    ins.then_inc(inc[1], 1)
                else:
                    ins.then_inc(inc[1], 16)

        with nc.Block() as block:
            @block.sync
            def _(e):
                replay('sp', e)

            @block.tensor
            def _(e):
                replay('pe', e)

            @block.scalar
            def _(e):
                replay('act', e)

            @block.vector
            def _(e):
                replay('dve', e)

            @block.gpsimd
            def _(e):
                replay('pool', e)


    def mm(self, out, lhsT, rhs, start, stop, reads, writes):
        self.op('pe', lambda e: e.matmul(out, lhsT, rhs, start=start, stop=stop), reads, writes)

    def tr(self, out, in_, ident, reads, writes):
        self.op('pe', lambda e: e.transpose(out, in_, ident), reads, writes)

    def act(self, out, in_, func, reads, writes, **kw):
        self.op('act', lambda e: e.activation(out, in_, func, **kw), reads, writes)

    def tt(self, eng, out, in0, in1, op, reads, writes):
        self.op(eng, lambda e: e.tensor_tensor(out, in0, in1, op), reads, writes)

    def ts(self, eng, out, in0, s1, s2, op0, op1, reads, writes):
        self.op(eng, lambda e: e.tensor_scalar(out, in0, s1, s2, op0, op1), reads, writes)

    def stt(self, out, in0, scalar, in1, op0, op1, reads, writes):
        self.op('dve', lambda e: e.scalar_tensor_tensor(out, in0, scalar, in1, op0, op1), reads, writes)

    def cp(self, eng, out, in_, reads, writes):
        if eng == 'act':
            self.op('act', lambda e: e.copy(out, in_), reads, writes)
        else:
            self.op(eng, lambda e: e.tensor_copy(out, in_), reads, writes)

    def fence(self):
        for e in self.engs:
            waits = []
            for o in ['pe', 'act', 'dve', 'pool']:
                if not (o == e == 'pe') and self.cnt[o] > 0 and self.waited[e].get(o, 0) < self.cnt[o]:
                    self.waited[e][o] = self.cnt[o]
                    waits.append((self.sem[o], self.cnt[o]))
            for i in range(ND_SEMS):
                if self.dcnt[i] > 0 and self.waited[e].get(('d', i), 0) < self.dcnt[i] * 16:
                    self.waited[e][('d', i)] = self.dcnt[i] * 16
                    waits.append((self.dsem[i], self.dcnt[i] * 16))
            self.lists[e].append((waits, None, None))


D = 1024
NT = 18
TOK = NT * 128
EPS = 1e-6
N_IN = 4144
COL_A, COL_H, COL_G, COL_D = 0, 1296, 2320, 3376


def host_consts():
    c = {}
    pos = np.arange(2048)
    row = (pos // 64).astype(np.float64)
    col = (pos % 64).astype(np.float64)
    inv = 10000.0 ** (-np.arange(16, dtype=np.float64) / 16)
    ang = np.concatenate([row[:, None] * inv, col[:, None] * inv], -1)
    c["ropec"] = np.ascontiguousarray(np.cos(ang).reshape(16, 128, 32).transpose(1, 0, 2)).astype(np.float32)
    c["ropes"] = np.ascontiguousarray(np.sin(ang).reshape(16, 128, 32).transpose(1, 0, 2)).astype(np.float32)
    t = np.arange(128)[:, None]
    s_ = np.arange(128)[None, :]
    m = np.zeros((128, 384), np.float32)
    m[:, 0:128] = np.where(s_ >= t, 0.0, -30000.0)
    m[:, 256:384] = np.where(s_ <= t, 0.0, -30000.0)
    c["amask"] = m
    import ml_dtypes
    bf = ml_dtypes.bfloat16
    for tag, Lh in (("l", 2048), ("c", 256)):
        pos = np.arange(Lh, dtype=np.float32)
        t = (pos / np.float32(max(Lh - 1, 1))).astype(np.float32)
        w = (np.float32(2.0 * np.pi) * pos / np.float32(Lh)).astype(np.float32)
        f = np.linspace(1e-4, 15, 16, dtype=np.float32)
        z = np.concatenate([t[:, None], np.cos(w[:, None] * f), -np.sin(w[:, None] * f)], -1).astype(np.float32)
        c["zT_" + tag] = np.ascontiguousarray(z.T)
        nt_ = Lh // 128
        c["ntcol_" + tag] = np.ascontiguousarray((-t).reshape(nt_, 128).T)
        N = 2 * Lh - 1
        idx = (np.outer(np.arange(Lh, dtype=np.int64), np.arange(Lh, dtype=np.int64)) % N).astype(np.float64)
        ang = 2.0 * np.pi * idx / N
        C = np.cos(ang)
        S = np.sin(ang)
        c["Cn_" + tag] = np.ascontiguousarray(C.astype(np.float32).astype(bf))
        c["Sn_" + tag] = np.ascontiguousarray(S.astype(np.float32).astype(bf))
        c["Ct_" + tag] = np.ascontiguousarray(C.reshape(nt_, 128, nt_, 128).transpose(2, 1, 0, 3).astype(np.float32).astype(bf))
        c["St_" + tag] = np.ascontiguousarray(S.reshape(nt_, 128, nt_, 128).transpose(2, 1, 0, 3).astype(np.float32).astype(bf))
        wf = np.full(Lh, 2.0 / N, np.float32)
        wf[0] = 1.0 / N
        c["wf_" + tag] = np.ascontiguousarray(wf.reshape(nt_, 128).T)
    import math
    dmin, dmax = math.log(1e-2) / 1.5, math.log(1e-2) / 0.3
    c["deltas"] = np.abs(np.linspace(dmin, dmax, 256, dtype=np.float32)).reshape(1, 256).astype(np.float32)
    return c


def build(n_layers=4, tap=None, branches=("att", "mlstm", "gla", "hyena")):
    nc = bass.Bass("TRN2", target_bir_lowering=False)

    def din(name, shape, dt=F32):
        return nc.dram_tensor(name, shape, dt, kind="ExternalInput").ap()

    x_in = din("x", [2048, D])
    ctx_in = din("ctx", [256, D])
    cc_in = din("cc", [16, 128])
    w_ada = din("w_ada", [4, D, 3 * D])
    b_ada = din("b_ada", [4, 3 * D])
    g_pre = din("g_pre", [4, D])
    g_post = din("g_post", [4, D])
    w_in = din("w_in", [4, D, N_IN])
    w_out = din("w_out", [4, D, D])
    mlstm_gate_b = din("mlstm_gate_b", [4, 16])
    mlstm_norm_g = din("mlstm_norm_g", [4, 256])
    gla_norm_g = din("gla_norm_g", [4, 256])
    gla_w_alpha = din("gla_w_alpha", [4, 2, 16, 256])
    gla_b_alpha = din("gla_b_alpha", [4, 2, 256])
    attn_sink = din("attn_sink", [4, 4])
    ropec_in = din("ropec", [128, 16, 32])
    ropes_in = din("ropes", [128, 16, 32])
    amask_in = din("amask", [128, 384])
    hy = {}
    for nm, shp in (("hyena_conv_w", [4, 3, 768]), ("hyena_conv_b", [4, 768]), ("hyena_w1", [4, 33, 64]),
                    ("hyena_b1", [4, 64]), ("hyena_w2", [4, 64, 64]), ("hyena_b2", [4, 64]), ("hyena_w3", [4, 64, 512]),
                    ("hyena_b3", [4, 512]), ("hyena_freq", [4, 2, 64]), ("hyena_d", [4, 256]), ("deltas", [1, 256])):
        hy[nm] = din(nm, shp)
    for tag, Lh in (("l", 2048), ("c", 256)):
        nt_ = Lh // 128
        hy["zT_" + tag] = din("zT_" + tag, [33, Lh])
        hy["ntcol_" + tag] = din("ntcol_" + tag, [128, nt_])
        hy["wf_" + tag] = din("wf_" + tag, [128, nt_])
        hy["Cn_" + tag] = din("Cn_" + tag, [Lh, Lh], BF16)
        hy["Sn_" + tag] = din("Sn_" + tag, [Lh, Lh], BF16)
        hy["Ct_" + tag] = din("Ct_" + tag, [nt_, 128, nt_, 128], BF16)
        hy["St_" + tag] = din("St_" + tag, [nt_, 128, nt_, 128], BF16)
    y_out = nc.dram_tensor("y", [2048, D], F32, kind="ExternalOutput").ap()
    ctxs = nc.dram_tensor("ctxs", [256, D], F32).ap()
    tap_out = None
    if tap is not None:
        tap_out = nc.dram_tensor("tap", [128, 8, TOK], BF16, kind="ExternalOutput").ap()

    w_in_v = [w_in[l].rearrange("(kc p) n -> p kc n", p=128) for l in range(4)]
    w_out_v = [w_out[l].rearrange("(kc p) n -> p kc n", p=128) for l in range(4)]
    w_ada_v = [w_ada[l].rearrange("(kc p) n -> p kc n", p=128) for l in range(4)]

    with ExitStack() as es:
        P = Prog(nc, es)
        ident_bf = P.sb("ident_bf", [128, 128], BF16)
        ident_f = P.sb("ident_f", [128, 128], F32)
        tri_le = P.sb("tri_le", [128, 128], F32)
        tri_ge = P.sb("tri_ge", [128, 128], F32)
        tri_gt = P.sb("tri_gt", [128, 128], F32)
        tri_lt = P.sb("tri_lt", [128, 128], F32)
        ropec = P.sb("ropec_sb", [128, 16, 32], F32)
        ropes = P.sb("ropes_sb", [128, 16, 32], F32)
        amask = P.sb("amask_sb", [128, 384], F32)

        def mk_affine(buf, pattern, cm, cmp_op, fill_in=1.0):
            P.op('pool', lambda e: e.memset(buf.ap[:], fill_in), writes=[buf])
            P.op('pool', lambda e: e.affine_select(buf.ap[:], buf.ap[:], pattern=pattern, compare_op=cmp_op,
                                                   fill=0.0, base=0, channel_multiplier=cm), reads=[buf], writes=[buf])

        mk_affine(ident_bf, [[-1, 128]], 1, ALU.is_equal)
        mk_affine(ident_f, [[-1, 128]], 1, ALU.is_equal)
        mk_affine(tri_le, [[1, 128]], -1, ALU.is_ge)
        mk_affine(tri_ge, [[-1, 128]], 1, ALU.is_ge)
        mk_affine(tri_gt, [[-1, 128]], 1, ALU.is_gt)
        mk_affine(tri_lt, [[1, 128]], -1, ALU.is_gt)
        P.dma('sp', ropec.ap[:], ropec_in, writes=[ropec])
        P.dma('sp', ropes.ap[:], ropes_in, writes=[ropes])
        P.dma('sp', amask.ap[:], amask_in, writes=[amask])

        pT = [P.ps("pT%d" % i, [128, 1024], BF16) for i in range(2)]
        pf = [P.ps("pf%d" % i, [128, 512], F32) for i in range(6)]

        MA = P.sb("MA", [128, 4, 8, 2], F32)
        MB = P.sb("MB", [128, 4, 8, 2], F32)
        MG = P.sb("MG", [128, 4, 8, 2], F32)

        with ExitStack() as es2:
            P.es = es2
            ccr = P.sb("ccr", [16, 128], F32)
            scs = P.sb("scs", [128, 16], F32)
            sc2 = P.sb("sc2", [128, 8, 2], F32)
            VR1 = P.sb("VR1", [128, 128], F32)
            VR2 = P.sb("VR2", [32, 128], F32)
            VC1 = P.sb("VC1", [128, 128], F32)
            VC2 = P.sb("VC2", [128, 32], F32)
            modc = P.sb("modc", [128, 24, 2], F32)
            wa = [P.sb("wa%d" % i, [128, 8, 512], F32) for i in range(2)]
            P.dma('sp', ccr.ap[:], cc_in, writes=[ccr])
            P.dma('sp', VR1.ap[0:96, :], b_ada.rearrange("l (c p) -> (l c) p", p=128), writes=[VR1])
            P.dma('sp', VR1.ap[96:128, :], g_pre.rearrange("l (c p) -> (l c) p", p=128), writes=[VR1])
            P.dma('sp', VR2.ap[:], g_post.rearrange("l (c p) -> (l c) p", p=128), writes=[VR2])
            P.tr(pf[0].ap[:, 0:16], ccr.ap[:], ident_f.ap[0:16, 0:16], [ccr, ident_f], [pf[0]])
            P.act(scs.ap[:], pf[0].ap[:, 0:16], AF.Silu, [pf[0]], [scs])
            P.cp('dve', sc2.ap[:, :, 0], scs.ap[:, 0:8], [scs], [sc2])
            P.cp('dve', sc2.ap[:, :, 1], scs.ap[:, 8:16], [scs], [sc2])
            P.tr(pf[1].ap[:, 0:128], VR1.ap[:], ident_f.ap[:], [VR1, ident_f], [pf[1]])
            P.cp('dve', VC1.ap[:], pf[1].ap[:, 0:128], [pf[1]], [VC1])
            P.tr(pf[2].ap[:, 0:32], VR2.ap[:], ident_f.ap[0:32, 0:32], [VR2, ident_f], [pf[2]])
            P.cp('dve', VC2.ap[:], pf[2].ap[:, 0:32], [pf[2]], [VC2])
            gi = 0
            for l in range(n_layers):
                pm = pf[3 + (l % 2)]
                for g6 in range(6):
                    wb_ = wa[gi % 2]
                    gi += 1
                    P.dma('sp', wb_.ap[:], w_ada_v[l][:, :, g6 * 512:(g6 + 1) * 512], writes=[wb_])
                    for jj in range(4):
                        j = g6 * 4 + jj
                        for kc in range(8):
                            P.mm(pm.ap[:, j * 2:j * 2 + 2], wb_.ap[:, kc, jj * 128:(jj + 1) * 128], sc2.ap[:, kc, :],
                                 kc == 0, kc == 7, [wb_, sc2], [pm])
                pm3 = pm.ap[:, 0:48].rearrange("p (j s) -> p j s", s=2)
                P.tt('dve', modc.ap[:], pm3, VC1.ap[:, l * 24:(l + 1) * 24].unsqueeze(2).to_broadcast([128, 24, 2]),
                     ALU.add, [pm, VC1], [modc])
                gpre_bc = VC1.ap[:, 96 + l * 8:96 + (l + 1) * 8].unsqueeze(2).to_broadcast([128, 8, 2])
                gpost_bc = VC2.ap[:, l * 8:(l + 1) * 8].unsqueeze(2).to_broadcast([128, 8, 2])
                P.stt(MA.ap[:, l], modc.ap[:, 8:16, :], 1.0, gpre_bc, ALU.add, ALU.mult, [modc, VC1], [MA])
                P.cp('dve', MB.ap[:, l], modc.ap[:, 0:8, :], [modc], [MB])
                P.tt('dve', MG.ap[:, l], modc.ap[:, 16:24, :], gpost_bc, ALU.mult, [modc, VC2], [MG])
            P.fence()
        P.es = es

        hT = P.sb("hT", [128, 8, TOK], BF16)
        mixT = P.sb("mixT", [128, 8, TOK], BF16)
        hTb = [P.vb("hT_t%d" % i) for i in range(NT)]
        mixb = [[P.vb("mix_%d_%d" % (g, i)) for i in range(NT)] for g in range(4)]
        if tap is not None:
            P.op('pool', lambda e: e.memset(mixT.ap[:], 0.0), writes=[b for g in mixb for b in g])
        wbuf = []
        wrot = [0]

        def alloc_wbuf():
            wbuf[:] = [P.sb("wbuf%d" % i, [128, 8, 512], BF16) for i in range(2)]
        xs = [P.vb("xs%d" % i) for i in range(NT)]
        if VERBOSE:
            print("sbuf bytes remaining after main alloc:", nc.sbuf_bytes_remaining)

        def load_w(l, c0, ncols, q='pool'):
            b = wbuf[wrot[0] % 2]
            wrot[0] += 1
            P.dma(q, b.ap[:, :, 0:ncols], w_in_v[l][:, :, c0:c0 + ncols], writes=[b])
            return b

        def proj_tm(i, wb_, c0, ncols, out_ap, outbuf):
            for kc in range(8):
                P.mm(out_ap, hT.ap[:, kc, i * 128:(i + 1) * 128], wb_.ap[:, kc, c0:c0 + ncols], kc == 0, kc == 7,
                     [hTb[i], wb_], [outbuf])

        def proj_fm(wb_, c0, ncols, t0, nt, out_ap, outbuf):
            rd = [wb_] + [hTb[i] for i in range(t0 // 128, (t0 + nt + 127) // 128)]
            for kc in range(8):
                P.mm(out_ap, wb_.ap[:, kc, c0:c0 + ncols], hT.ap[:, kc, t0:t0 + nt], kc == 0, kc == 7, rd, [outbuf])

        def state_src(l, i):
            if i < 2:
                return (ctx_in if l == 0 else ctxs)[i * 128:(i + 1) * 128, :]
            return (x_in if l == 0 else y_out)[(i - 2) * 128:(i - 1) * 128, :]

        def state_dst(i):
            if i < 2:
                return ctxs[i * 128:(i + 1) * 128, :]
            return y_out[(i - 2) * 128:(i - 1) * 128, :]

        for l in range(n_layers):
            last = (l == 3)
            esC = ExitStack()
            P.es = esC
            xt = [P.sb("xt%d" % i, [128, 1024], F32) for i in range(2)]
            xn = [P.sb("xn%d" % i, [128, 1024], BF16) for i in range(2)]
            tmpf = P.sb("tmpf", [128, 1024], F32)
            st4 = [P.sb("st4_%d" % i, [128, 8], F32) for i in range(2)]
            for i in range(NT):
                s = 1 if i < 2 else 0
                xb_, xnb, stb = xt[i % 2], xn[i % 2], st4[i % 2]
                P.dma('sp', xb_.ap[:], state_src(l, i), reads=[xs[i]], writes=[xb_])
                P.act(tmpf.ap[:], xb_.ap[:], AF.Square, [xb_], [tmpf, stb], accum_out=stb.ap[:, 0:1])
                P.act(stb.ap[:, 1:2], stb.ap[:, 0:1], AF.Sqrt, [stb], [stb], bias=EPS, scale=1.0 / D)
                P.op('dve', lambda e, o=stb.ap[:, 2:3], a=stb.ap[:, 1:2]: e.reciprocal(o, a), [stb], [stb])
                P.act(xnb.ap[:], xb_.ap[:], AF.Identity, [xb_, stb], [xnb], scale=stb.ap[:, 2:3])
                pt = pT[i % 2]
                for kc in range(8):
                    P.tr(pt.ap[:, kc * 128:(kc + 1) * 128], xnb.ap[:, kc * 128:(kc + 1) * 128], ident_bf.ap[:],
                         [xnb, ident_bf], [pt])
                pt3 = pt.ap[:, :].rearrange("p (k t) -> p k t", t=128)
                tm3 = tmpf.ap[:, :].rearrange("p (k t) -> p k t", t=128)
                P.tt('dve', tm3, pt3, MA.ap[:, l, :, s:s + 1].to_broadcast([128, 8, 128]), ALU.mult, [pt, MA], [tmpf])
                P.tt('dve', hT.ap[:, :, i * 128:(i + 1) * 128], tm3, MB.ap[:, l, :, s:s + 1].to_broadcast([128, 8, 128]),
                     ALU.add, [tmpf, MB], [hTb[i]])
            if tap == "h%d" % l:
                P.dma('sp', tap_out, hT.ap[:], reads=hTb, out=True)

            P.fence()
            esC.close()
            P.es = es

            BRANCH_BUILDERS["att"](locals()) if "att" in branches else None
            BRANCH_BUILDERS["mlstm"](locals()) if "mlstm" in branches else None
            BRANCH_BUILDERS["gla"](locals()) if "gla" in branches else None
            BRANCH_BUILDERS["hyena"](locals()) if "hyena" in branches else None
            if tap == "mix%d" % l:
                P.dma('sp', tap_out, mixT.ap[:], reads=[b for g in mixb for b in g], out=True)

            esZ = ExitStack()
            P.es = esZ
            woutb = P.sb("woutb", [128, 8, 1024], BF16)
            Gbc = [P.sb("Gbc%d" % s, [128, 1024], F32) for s in range(2)]
            gbt = P.sb("gbt", [128, 128], F32)
            xt = [P.sb("xt%d" % i, [128, 1024], F32) for i in range(2)]
            tmpf = P.sb("tmpf", [128, 1024], F32)
            st4 = [P.sb("st4_%d" % i, [128, 8], F32) for i in range(2)]
            for h2 in range(2):
                P.dma('pool', woutb.ap[:, :, h2 * 512:(h2 + 1) * 512], w_out_v[l][:, :, h2 * 512:(h2 + 1) * 512],
                      writes=[woutb])
            for s in range(2):
                for j in range(8):
                    P.cp('dve', gbt.ap[:], MG.ap[:, l, j, s:s + 1].to_broadcast([128, 128]), [MG], [gbt])
                    pg = pf[j // 4]
                    P.mm(pg.ap[:, (j % 4) * 128:(j % 4 + 1) * 128], gbt.ap[:], ident_f.ap[:], True, True,
                         [gbt, ident_f], [pg])
                for h2 in range(2):
                    P.cp('act', Gbc[s].ap[:, h2 * 512:(h2 + 1) * 512], pf[h2].ap[:], [pf[h2]], [Gbc[s]])
            for i in range(NT):
                if last and i < 2:
                    continue
                s = 1 if i < 2 else 0
                xb_, stb = xt[i % 2], st4[i % 2]
                P.dma('sp', xb_.ap[:], state_src(l, i), reads=[xs[i]], writes=[xb_])
                py = [pf[2 + 2 * (i % 2)], pf[3 + 2 * (i % 2)]]
                mrd = [mixb[g][i] for g in range(4)]
                for h2 in range(2):
                    for kc in range(8):
                        P.mm(py[h2].ap[:], mixT.ap[:, kc, i * 128:(i + 1) * 128], woutb.ap[:, kc, h2 * 512:(h2 + 1) * 512],
                             kc == 0, kc == 7, mrd + [woutb], [py[h2]])
                for h2 in range(2):
                    P.act(tmpf.ap[:, h2 * 512:(h2 + 1) * 512], py[h2].ap[:], AF.Square, [py[h2]], [tmpf, stb],
                          accum_out=stb.ap[:, 4 + h2:5 + h2])
                P.tt('dve', stb.ap[:, 6:7], stb.ap[:, 4:5], stb.ap[:, 5:6], ALU.add, [stb], [stb])
                P.act(stb.ap[:, 7:8], stb.ap[:, 6:7], AF.Sqrt, [stb], [stb], bias=EPS, scale=1.0 / D)
                P.op('dve', lambda e, o=stb.ap[:, 3:4], a=stb.ap[:, 7:8]: e.reciprocal(o, a), [stb], [stb])
                for h2 in range(2):
                    sl = slice(h2 * 512, (h2 + 1) * 512)
                    P.stt(tmpf.ap[:, sl], py[h2].ap[:], stb.ap[:, 3:4], Gbc[s].ap[:, sl], ALU.mult, ALU.mult,
                          [py[h2], stb, Gbc[s]], [tmpf])
                P.tt('dve', xb_.ap[:], xb_.ap[:], tmpf.ap[:], ALU.add, [xb_, tmpf], [xb_])
                P.dma('sp', state_dst(i), xb_.ap[:], reads=[xb_], writes=[xs[i]], out=(l == n_layers - 1 and i >= 2))
            P.fence()
            esZ.close()
            P.es = es
        P.finish()
    return nc


BRANCH_BUILDERS = {}


def branch_att(L):
    P, nc, l, last = L["P"], L["nc"], L["l"], L["last"]
    hT, hTb, mixT, mixb, pT, pf = L["hT"], L["hTb"], L["mixT"], L["mixb"], L["pT"], L["pf"]
    ident_bf, ropec, ropes, amask = L["ident_bf"], L["ropec"], L["ropes"], L["amask"]
    load_w, proj_tm = L["load_w"], L["proj_tm"]
    with ExitStack() as es2:
        P.es = es2
        L["alloc_wbuf"]()
        QKT = P.sb("QKT", [128, 4, TOK], BF16)
        QKb = [P.vb("QKT_%d" % i) for i in range(NT)]
        v_tm = P.sb("v_tm", [128, NT, 128], BF16)
        sg_tm = P.sb("sg_tm", [128, NT, 256], BF16)
        vb_ = [P.vb("vsg_%d" % i) for i in range(NT)]
        qkf = P.sb("qkf", [128, 384], F32)
        rt = [P.sb("rt%d" % i, [128, 192], F32) for i in range(2)]
        rq = P.sb("rq", [128, 512], BF16)
        sink_bc = P.sb("sink_bc", [128, 4], F32)
        sm2 = [P.sb("sm%d" % r, [128, 640], F32) for r in range(2)]
        Pb2 = [P.sb("Pb%d" % r, [128, 640], BF16) for r in range(2)]
        PTs2 = [P.sb("PTs%d" % r, [128, 5, 128], BF16) for r in range(2)]
        stt2 = [P.sb("att_st%d" % r, [128, 8], F32) for r in range(2)]
        rden2 = [P.sb("rden%d" % r, [128, 4], F32) for r in range(2)]
        og2 = [P.sb("og%d" % r, [128, 256], BF16) for r in range(2)]
        P.dma('sp', sink_bc.ap[:], L["attn_sink"][l:l + 1, :].partition_broadcast(128), writes=[sink_bc])
        w1 = load_w(l, COL_D, 384)
        w2 = load_w(l, COL_D + 384, 384)
        for i in range(NT):
            pq, pv = pf[0], pf[1]
            proj_tm(i, w1, 0, 384, pq.ap[:, 0:384], pq)
            proj_tm(i, w2, 0, 384, pv.ap[:, 0:384], pv)
            P.cp('act', v_tm.ap[:, i, :], pv.ap[:, 0:128], [pv], [vb_[i]])
            P.act(sg_tm.ap[:, i, :], pv.ap[:, 128:384], AF.Silu, [pv], [vb_[i]])
            if i >= 2:
                P.cp('act', qkf.ap[:], pq.ap[:, 0:384], [pq], [qkf])
                q4 = qkf.ap[:, :].rearrange("p (h two j) -> p h two j", two=2, j=32)
                r4 = rq.ap[:, 0:384].rearrange("p (h two j) -> p h two j", two=2, j=32)
                x1, x2 = q4[:, :, 0, :], q4[:, :, 1, :]
                cosb = ropec.ap[:, i - 2, :].unsqueeze(1).to_broadcast([128, 6, 32])
                sinb = ropes.ap[:, i - 2, :].unsqueeze(1).to_broadcast([128, 6, 32])
                ta = rt[0].ap[:, :].rearrange("p (h j) -> p h j", j=32)
                tb = rt[1].ap[:, :].rearrange("p (h j) -> p h j", j=32)
                P.tt('dve', ta, x1, cosb, ALU.mult, [qkf, ropec], [rt[0]])
                P.tt('dve', tb, x2, sinb, ALU.mult, [qkf, ropes], [rt[1]])
                P.tt('dve', r4[:, :, 0, :], ta, tb, ALU.subtract, [rt[0], rt[1]], [rq])
                P.tt('dve', ta, x2, cosb, ALU.mult, [qkf, ropec], [rt[0]])
                P.tt('dve', tb, x1, sinb, ALU.mult, [qkf, ropes], [rt[1]])
                P.tt('dve', r4[:, :, 1, :], ta, tb, ALU.add, [rt[0], rt[1]], [rq])
            else:
                P.cp('act', rq.ap[:, 0:384], pq.ap[:, 0:384], [pq], [rq])
            kd_src = rq.ap[:, 256:384].rearrange("p (g o d) -> p g o d", o=1, d=64).to_broadcast([128, 2, 2, 64])
            P.cp('dve', rt[0].ap[:, 0:128].bitcast(BF16).rearrange("p (g o d) -> p g o d", o=2, d=64), kd_src,
                 [rq], [rt[0]])
            P.cp('dve', rq.ap[:, 256:512], rt[0].ap[:, 0:128].bitcast(BF16), [rt[0]], [rq])
            pt = pT[i % 2]
            for c4 in range(4):
                P.tr(pt.ap[:, c4 * 128:(c4 + 1) * 128], rq.ap[:, c4 * 128:(c4 + 1) * 128], ident_bf.ap[:],
                     [rq, ident_bf], [pt])
            P.cp('dve', QKT.ap[:, :, i * 128:(i + 1) * 128], pt.ap[:, 0:512].rearrange("p (c t) -> p c t", t=128),
                 [pt], [QKb[i]])

        def geom(i):
            has_local = i >= 2
            n = i - 2
            if has_local:
                nlo, nhi = max(n - 1, 0), min(n + 1, 15)
                c0 = (nlo - (n - 1)) * 128
                c1 = c0 + (nhi - nlo + 1) * 128
                ktiles = list(range(2 + nlo, 2 + nhi + 1))
            else:
                c0 = c1 = 384
                ktiles = []
            d0 = 384 - (c1 - c0)
            blocks = [(kt, d0 + 128 * bi) for bi, kt in enumerate(ktiles)] + [(0, 384), (1, 512)]
            return has_local, c0, c1, d0, ktiles, blocks

        def stage1(i, h):
            has_local, c0, c1, d0, ktiles, blocks = geom(i)
            rden = rden2[i % 2]
            g, base = h // 2, (h % 2) * 64
            r = h % 2
            sm, Pb, stt_ = sm2[r], Pb2[r], stt2[r]
            pl, pc = (pf[4], pf[5]) if r == 0 else (pf[0], pf[1])
            qa = QKT.ap[base:base + 64, g, i * 128:(i + 1) * 128]
            if has_local:
                P.mm(pl.ap[:, d0:384], qa, QKT.ap[base:base + 64, 2 + g, ktiles[0] * 128:(ktiles[-1] + 1) * 128],
                     True, True, [QKb[i]] + [QKb[k] for k in ktiles], [pl])
                P.tt('dve', sm.ap[:, d0:384], pl.ap[:, d0:384], amask.ap[:, c0:c1], ALU.add, [pl, amask], [sm])
            P.mm(pc.ap[:, 0:256], qa, QKT.ap[base:base + 64, 2 + g, 0:256], True, True, [QKb[i], QKb[0], QKb[1]], [pc])
            P.cp('act', sm.ap[:, 384:640], pc.ap[:, 0:256], [pc], [sm])
            P.op('dve', lambda e, o=stt_.ap[:, 0:1], a=sm.ap[:, d0:640]: e.reduce_max(o, a, AX.X), [sm], [stt_])
            P.ts('dve', stt_.ap[:, 1:2], stt_.ap[:, 0:1], -0.125, None, ALU.mult, ALU.bypass, [stt_], [stt_])
            P.act(Pb.ap[:, d0:640], sm.ap[:, d0:640], AF.Exp, [sm, stt_], [Pb, stt_], bias=stt_.ap[:, 1:2], scale=0.125,
                  accum_out=stt_.ap[:, 2:3])
            P.act(stt_.ap[:, 3:4], stt_.ap[:, 1:2], AF.Exp, [stt_, sink_bc], [stt_], bias=sink_bc.ap[:, h:h + 1], scale=1.0)
            P.tt('dve', stt_.ap[:, 4:5], stt_.ap[:, 2:3], stt_.ap[:, 3:4], ALU.add, [stt_], [stt_])
            P.op('dve', lambda e, o=rden.ap[:, h:h + 1], a=stt_.ap[:, 4:5]: e.reciprocal(o, a), [stt_], [rden])

        def stage2(i, h):
            has_local, c0, c1, d0, ktiles, blocks = geom(i)
            g = h // 2
            r = h % 2
            Pb, PTs = Pb2[r], PTs2[r]
            pO = pf[2 + (i % 2)]
            pt = pT[h % 2]
            for bi, (kt, cc) in enumerate(blocks):
                P.tr(pt.ap[:, bi * 128:(bi + 1) * 128], Pb.ap[:, cc:cc + 128], ident_bf.ap[:], [Pb, ident_bf], [pt])
            nb = len(blocks)
            P.cp('act', PTs.ap[:, 0:nb, :], pt.ap[:, 0:nb * 128].rearrange("p (c t) -> p c t", t=128), [pt], [PTs])
            for bi, (kt, cc) in enumerate(blocks):
                P.mm(pO.ap[:, h * 64:(h + 1) * 64], PTs.ap[:, bi, :], v_tm.ap[:, kt, g * 64:(g + 1) * 64],
                     bi == 0, bi == nb - 1, [PTs, vb_[kt]], [pO])

        def epilogue(i):
            pO = pf[2 + (i % 2)]
            rden, og = rden2[i % 2], og2[i % 2]
            for h in range(4):
                P.stt(og.ap[:, h * 64:(h + 1) * 64], pO.ap[:, h * 64:(h + 1) * 64], rden.ap[:, h:h + 1],
                      sg_tm.ap[:, i, h * 64:(h + 1) * 64], ALU.mult, ALU.mult, [pO, rden, vb_[i]], [og])
            pt = pT[i % 2]
            for c2 in range(2):
                P.tr(pt.ap[:, c2 * 128:(c2 + 1) * 128], og.ap[:, c2 * 128:(c2 + 1) * 128], ident_bf.ap[:], [og, ident_bf], [pt])
            P.cp('act', mixT.ap[:, 6:8, i * 128:(i + 1) * 128], pt.ap[:, 0:256].rearrange("p (c t) -> p c t", t=128),
                 [pt], [mixb[3][i]])

        items = [(i, h) for i in range(NT) if not (last and i < 2) for h in range(4)]
        stage1(*items[0])
        for n_, (i, h) in enumerate(items):
            if n_ + 1 < len(items):
                stage1(*items[n_ + 1])
            stage2(i, h)
            if h == 3:
                epilogue(i)
        P.fence()
    P.es = L["es"]


BRANCH_BUILDERS["att"] = branch_att


def make_in_maps(inputs):
    c = host_consts()
    shared = {}
    for k in ["w_ada", "b_ada", "g_pre", "g_post", "w_in", "w_out", "mlstm_norm_g", "gla_norm_g", "gla_w_alpha",
              "gla_b_alpha", "attn_sink"]:
        shared[k] = np.ascontiguousarray(np.asarray(inputs[k], dtype=np.float32))
    for k in ["hyena_conv_w", "hyena_conv_b", "hyena_w1", "hyena_b1", "hyena_w2", "hyena_b2", "hyena_w3", "hyena_b3",
              "hyena_freq", "hyena_d"]:
        shared[k] = np.ascontiguousarray(np.asarray(inputs[k], dtype=np.float32))
    shared["mlstm_gate_b"] = np.ascontiguousarray(np.asarray(inputs["mlstm_gate_b"], dtype=np.float32).reshape(4, 16))
    shared.update(c)
    maps = []
    c_ctx = np.asarray(inputs["c_ctx"], dtype=np.float32)
    for b in range(8):
        m = dict(shared)
        m["x"] = np.ascontiguousarray(np.asarray(inputs["x"][b], dtype=np.float32))
        m["ctx"] = np.ascontiguousarray(np.asarray(inputs["ctx"][b], dtype=np.float32))
        m["cc"] = np.ascontiguousarray(np.concatenate([np.asarray(inputs["c"][b], dtype=np.float32), c_ctx]).reshape(16, 128))
        maps.append(m)
    return maps


def branch_scan(L, kind):
    ml = kind == "mlstm"
    P, nc, l, last = L["P"], L["nc"], L["l"], L["last"]
    hT, hTb, mixT, mixb, pT, pf = L["hT"], L["hTb"], L["mixT"], L["mixb"], L["pT"], L["pf"]
    ident_bf, ident_f = L["ident_bf"], L["ident_f"]
    load_w, proj_tm, proj_fm = L["load_w"], L["proj_tm"], L["proj_fm"]
    base = COL_A if ml else COL_G
    cdec = 1.0 if ml else 1.0 / 16
    mc0, mg = (0, 0) if ml else (4, 2)
    qscale, kscale = (1.0, 0.125) if ml else (0.125, 1.0)
    with ExitStack() as es2:
        P.es = es2
        L["alloc_wbuf"]()
        qT = P.sb("s_qT", [128, 2, TOK], BF16)
        kT = P.sb("s_kT", [128, 2, TOK], BF16)
        k_tm = P.sb("s_ktm", [128, NT, 256], BF16)
        v_aug = P.sb("s_vaug", [128, NT, 512], BF16)
        Hh = P.sb("s_H", [128, NT, 256], F32)
        gr = P.sb("s_gr", [128, NT, 32], F32)
        qkb = [P.vb("s_qkb%d" % i) for i in range(NT)]
        kvb = [P.vb("s_kvb%d" % i) for i in range(NT)]
        Hb = [P.vb("s_Hb%d" % i) for i in range(NT)]
        grb = [P.vb("s_grb%d" % i) for i in range(NT)]
        normg = P.sb("s_normg", [128, 256], F32)
        P.dma('sp', normg.ap[:], (L["mlstm_norm_g"] if ml else L["gla_norm_g"])[l:l + 1, :].partition_broadcast(128),
              writes=[normg])
        if ml:
            gbb = P.sb("s_gbb", [128, 16], F32)
            P.dma('sp', gbb.ap[:], L["mlstm_gate_b"][l:l + 1, :].partition_broadcast(128), writes=[gbb])
            posi = P.sb("s_posi", [128, 256], F32)
            negi = P.sb("s_negi", [128, 256], F32)
            e4 = P.sb("s_e4", [128, 8], F32)
        else:
            wal = P.sb("s_wal", [17, 2, 256], F32)
            P.dma('sp', wal.ap[0:16, :, :], L["gla_w_alpha"][l].rearrange("d r c -> r d c"), writes=[wal])
            P.dma('sp', wal.ap[16:17, :, :], L["gla_b_alpha"][l:l + 1, :, :], writes=[wal])
            rTa = P.sb("s_rTa", [17, 128], F32)
            P.op('pool', lambda e: e.memset(rTa.ap[:], 1.0), writes=[rTa])
            e_tm = P.sb("s_etm", [128, 256], F32)
        sp_tm = P.sb("s_sp", [128, 256], F32)
        EqT2 = [P.sb("s_EqT%d" % r, [128, 256], F32) for r in range(2)]
        EkT = P.sb("s_EkT", [128, 256], F32)
        Ex = P.sb("s_Ex", [128, 256], F32)
        qp2 = [P.sb("s_qp%d" % r, [128, 2, 128], BF16) for r in range(2)]
        kpz = [P.sb("s_kpz%d" % h, [128, 128], BF16) for h in range(4)]
        for h in range(4):
            P.op('pool', lambda e, a=kpz[h].ap[:]: e.memset(a, 0.0), writes=[kpz[h]])
        kpp2 = [P.sb("s_kpp%d" % r, [128, 256], BF16) for r in range(2)]
        S_m2 = [P.sb("s_Sm%d" % r, [128, 512], BF16) for r in range(2)]
        St_f = [P.sb("s_Stf%d" % j, [128, 128], F32) for j in range(2)]
        St_b = [P.sb("s_Stb%d" % h, [128, 128], BF16) for h in range(4)]
        dn = P.sb("s_dn", [128, 8], F32)
        tmpH = P.sb("s_tmpH", [128, 256], F32)

        wqk = load_w(l, base, 512)
        groups = [(0, 512), (512, 512), (1024, 512), (1536, 512), (2048, 256)]
        cnt = 0
        for j in range(2):
            for (t0, nt) in groups:
                tiles = list(range(t0 // 128, (t0 + nt) // 128))
                for which, dst, sc_ in ((0, qT, qscale), (1, kT, kscale)):
                    pq = pf[cnt % 2]
                    cnt += 1
                    proj_fm(wqk, which * 256 + j * 128, 128, t0, nt, pq.ap[:, 0:nt], pq)
                    P.act(dst.ap[:, j, t0:t0 + nt], pq.ap[:, 0:nt], AF.Identity, [pq], [qkb[t] for t in tiles], scale=sc_)
        wkv = load_w(l, base + 256, 512)
        ng = 16 if ml else 32
        wg = load_w(l, base + (1024 if ml else 768), 256 + ng)
        P.op('pool', lambda e: e.memset(v_aug.ap[:], 1.0), writes=kvb)
        for i in range(NT):
            pk = pf[2 + i % 2]
            proj_tm(i, wkv, 0, 512, pk.ap[:], pk)
            P.act(k_tm.ap[:, i, :], pk.ap[:, 0:256], AF.Identity, [pk], [kvb[i]], scale=kscale)
            P.cp('dve', v_aug.ap[:, i, :].rearrange("p (h e) -> p h e", e=128)[:, :, 0:64],
                 pk.ap[:, 256:512].rearrange("p (h e) -> p h e", e=64), [pk], [kvb[i]])
            pg = pf[4 + i % 2]
            proj_tm(i, wg, 256, 64, pg.ap[:, 0:64], pg)
            if ml:
                P.tt('dve', gr.ap[:, i, 0:16], pg.ap[:, 0:16], gbb.ap[:], ALU.add, [pg, gbb], [grb[i]])
            else:
                P.cp('dve', gr.ap[:, i, 0:32], pg.ap[:, 0:32], [pg], [grb[i]])

        P.ckpt("pre")
        for dirn in range(2):
            order = ([0, 1] + list(range(2, NT))) if dirn == 0 else ([1, 0] + list(range(NT - 1, 1, -1)))
            tri_c = L["tri_le"] if dirn == 0 else L["tri_ge"]
            tri_x = L["tri_gt"] if dirn == 0 else L["tri_lt"]
            dcol = 127 if dirn == 0 else 0
            for j in range(2):
                P.op('pool', lambda e, a=St_f[j].ap[:]: e.memset(a, 0.0), writes=[St_f[j]])
            for h in range(4):
                P.op('pool', lambda e, a=St_b[h].ap[:]: e.memset(a, 0.0), writes=[St_b[h]])
            def prefix(step, i):
                tsl = slice(i * 128, (i + 1) * 128)
                EqT, qp, kpp, S_m = EqT2[step % 2], qp2[step % 2], kpp2[step % 2], S_m2[step % 2]
                if ml:
                    ic, fc = dirn * 8, dirn * 8 + 4
                    P.act(e4.ap[:, 0:4], gr.ap[:, i, fc:fc + 4], AF.Exp, [grb[i]], [e4], scale=-1.0)
                    P.act(e4.ap[:, 4:8], e4.ap[:, 0:4], AF.Ln, [e4], [e4], bias=1.0)
                    P.cp('dve', sp_tm.ap[:, :].rearrange("p (h d) -> p h d", d=64),
                         e4.ap[:, 4:8].unsqueeze(2).to_broadcast([128, 4, 64]), [e4], [sp_tm])
                    ib = gr.ap[:, i, ic:ic + 4].unsqueeze(2).to_broadcast([128, 4, 64])
                    P.cp('dve', posi.ap[:, :].rearrange("p (h d) -> p h d", d=64), ib, [grb[i]], [posi])
                    P.ts('dve', negi.ap[:, :].rearrange("p (h d) -> p h d", d=64), ib, -1.0, None, ALU.mult, ALU.bypass,
                         [grb[i]], [negi])
                else:
                    P.tr(pf[3].ap[0:16, 256:384], gr.ap[:, i, dirn * 16:(dirn + 1) * 16], ident_f.ap[:],
                         [grb[i], ident_f], [pf[3]])
                    P.cp('act', rTa.ap[0:16, :], pf[3].ap[0:16, 256:384], [pf[3]], [rTa])
                    P.mm(pf[3].ap[:, 256:512], rTa.ap[0:17, :], wal.ap[0:17, dirn, :], True, True, [rTa, wal], [pf[3]])
                    P.act(e_tm.ap[:], pf[3].ap[:, 256:512], AF.Exp, [pf[3]], [e_tm], scale=-1.0)
                    P.act(sp_tm.ap[:], e_tm.ap[:], AF.Ln, [e_tm], [sp_tm], bias=1.0)
                P.ckpt("s1")
                pc, px = pf[4], pf[3]
                for j in range(2):
                    P.mm(pc.ap[:, j * 128:(j + 1) * 128], sp_tm.ap[:, j * 128:(j + 1) * 128], tri_c.ap[:], True, True,
                         [sp_tm, tri_c], [pc])
                if ml:
                    for j in range(2):
                        P.mm(pc.ap[:, 256 + j * 128:384 + j * 128], sp_tm.ap[:, j * 128:(j + 1) * 128], tri_c.ap[:],
                             True, False, [sp_tm, tri_c], [pc])
                        P.mm(pc.ap[:, 256 + j * 128:384 + j * 128], posi.ap[:, j * 128:(j + 1) * 128], ident_f.ap[:],
                             False, True, [posi, ident_f], [pc])
                P.mm(px.ap[:, 0:256], tri_x.ap[:], sp_tm.ap[:], True, not ml, [tri_x, sp_tm], [px])
                if ml:
                    P.mm(px.ap[:, 0:256], ident_f.ap[:], negi.ap[:], False, True, [ident_f, negi], [px])
                P.ckpt("s2")
                P.act(EqT.ap[:], pc.ap[:, 0:256], AF.Exp, [pc], [EqT], scale=-cdec)
                P.act(EkT.ap[:], pc.ap[:, 256:512] if ml else pc.ap[:, 0:256], AF.Exp, [pc], [EkT], scale=cdec)
                P.act(Ex.ap[:], px.ap[:, 0:256], AF.Exp, [px], [Ex], scale=-cdec)
                P.tt('dve', qp.ap[:], qT.ap[:, :, tsl], EqT.ap[:, :].rearrange("p (j t) -> p j t", t=128), ALU.mult,
                     [qkb[i], EqT], [qp])
                for h in range(4):
                    j, b0 = h // 2, (h % 2) * 64
                    P.tt('dve', kpz[h].ap[b0:b0 + 64, :], kT.ap[b0:b0 + 64, j, tsl], EkT.ap[b0:b0 + 64, j * 128:(j + 1) * 128],
                         ALU.mult, [qkb[i], EkT], [kpz[h]])
                P.tt('dve', kpp.ap[:], k_tm.ap[:, i, :], Ex.ap[:], ALU.mult, [kvb[i], Ex], [kpp])
                P.ckpt("s3")
                pS = pf[2]
                for h in range(4):
                    j, b0 = h // 2, (h % 2) * 64
                    P.mm(pS.ap[:, h * 128:(h + 1) * 128], kpz[h].ap[:], qp.ap[:, j, :], True, True, [kpz[h], qp], [pS])
                P.tt('dve', S_m.ap[:, :].rearrange("p (h t) -> p h t", t=128),
                     pS.ap[:, :].rearrange("p (h t) -> p h t", t=128),
                     tri_c.ap[:, :].unsqueeze(1).to_broadcast([128, 4, 128]), ALU.mult, [pS, tri_c], [S_m])

            def suffix(step, i):
                EqT, qp, kpp, S_m = EqT2[step % 2], qp2[step % 2], kpp2[step % 2], S_m2[step % 2]
                pO = pf[step % 2]
                for h in range(4):
                    j, b0 = h // 2, (h % 2) * 64
                    P.mm(pO.ap[:, h * 128:(h + 1) * 128], S_m.ap[:, h * 128:(h + 1) * 128], v_aug.ap[:, i, h * 128:(h + 1) * 128],
                         True, False, [S_m, kvb[i]], [pO])
                    P.mm(pO.ap[:, h * 128:(h + 1) * 128], qp.ap[:, j, :], St_b[h].ap[:], False, True, [qp, St_b[h]], [pO])
                P.ckpt("s5")
                pD = pf[5]
                for j in range(2):
                    P.mm(pD.ap[:, j * 256:(j + 1) * 256], kpp.ap[:, j * 128:(j + 1) * 128], v_aug.ap[:, i, j * 256:(j + 1) * 256],
                         True, True, [kpp, kvb[i]], [pD])
                for h in range(4):
                    j, b0 = h // 2, (h % 2) * 64
                    P.stt(St_f[j].ap[b0:b0 + 64, :], St_f[j].ap[b0:b0 + 64, :], EqT.ap[b0:b0 + 64, j * 128 + dcol:j * 128 + dcol + 1],
                          pD.ap[b0:b0 + 64, j * 256 + (h % 2) * 128:j * 256 + (h % 2) * 128 + 128], ALU.mult, ALU.add,
                          [St_f[j], EqT, pD], [St_f[j]])
                for h in range(4):
                    j, b0 = h // 2, (h % 2) * 64
                    P.cp('act', St_b[h].ap[b0:b0 + 64, :], St_f[j].ap[b0:b0 + 64, :], [St_f[j]], [St_b[h]])
                P.ckpt("s6")
                pO3 = pO.ap[:, 0:512].rearrange("p (h e) -> p h e", e=128)
                H3 = Hh.ap[:, i, :].rearrange("p (h d) -> p h d", d=64)
                if ml:
                    P.act(dn.ap[:, 0:4], pO3[:, :, 64], AF.Abs, [pO], [dn])
                    P.ts('dve', dn.ap[:, 0:4], dn.ap[:, 0:4], 1.0, None, ALU.max, ALU.bypass, [dn], [dn])
                    P.op('dve', lambda e, o=dn.ap[:, 4:8], a=dn.ap[:, 0:4]: e.reciprocal(o, a), [dn], [dn])
                    rb = dn.ap[:, 4:8].unsqueeze(2).to_broadcast([128, 4, 64])
                    if dirn == 0:
                        P.tt('dve', H3, pO3[:, :, 0:64], rb, ALU.mult, [pO, dn], [Hb[i]])
                    else:
                        P.tt('dve', tmpH.ap[:, :].rearrange("p (h d) -> p h d", d=64), pO3[:, :, 0:64], rb, ALU.mult,
                             [pO, dn], [tmpH])
                        P.tt('dve', Hh.ap[:, i, :], Hh.ap[:, i, :], tmpH.ap[:], ALU.add, [Hb[i], tmpH], [Hb[i]])
                else:
                    if dirn == 0:
                        P.cp('act', H3, pO3[:, :, 0:64], [pO], [Hb[i]])
                    else:
                        P.tt('dve', H3, pO3[:, :, 0:64], H3, ALU.add, [pO, Hb[i]], [Hb[i]])

            prefix(0, order[0])
            for step, i in enumerate(order):
                if step + 1 < len(order):
                    prefix(step + 1, order[step + 1])
                suffix(step, i)
        P.ckpt("scan")
        ncol = 512 if ml else 256
        wfin = load_w(l, base + 768, ncol)
        sig = P.sb("s_sig", [128, 256], F32)
        sgt = P.sb("s_sgt", [128, 256], F32)
        gs = P.sb("s_gs", [128, 256], F32)
        hh = P.sb("s_hh", [128, 256], F32)
        sq = P.sb("s_sq", [128, 256], F32)
        og = P.sb("s_og", [128, 256], BF16)
        for i in range(NT):
            if last and i < 2:
                continue
            pg = pf[i % 2]
            proj_tm(i, wfin, 0, ncol, pg.ap[:, 0:ncol], pg)
            if ml:
                P.act(sig.ap[:], pg.ap[:, 0:256], AF.Sigmoid, [pg], [sig])
                P.tt('dve', hh.ap[:], Hh.ap[:, i, :], sig.ap[:], ALU.mult, [Hb[i], sig], [hh])
                gate_ap = pg.ap[:, 256:512]
            else:
                P.cp('dve', hh.ap[:], Hh.ap[:, i, :], [Hb[i]], [hh])
                gate_ap = pg.ap[:, 0:256]
            P.act(sgt.ap[:], gate_ap, AF.Silu, [pg], [sgt])
            P.tt('dve', gs.ap[:], sgt.ap[:], normg.ap[:], ALU.mult, [sgt, normg], [gs])
            P.tt('dve', sq.ap[:], hh.ap[:], hh.ap[:], ALU.mult, [hh], [sq])
            P.op('dve', lambda e, o=dn.ap[:, 0:4], a=sq.ap[:, :].rearrange("p (h d) -> p h d", d=64): e.reduce_sum(o, a, AX.X),
                 [sq], [dn])
            P.act(dn.ap[:, 4:8], dn.ap[:, 0:4], AF.Sqrt, [dn], [dn], bias=EPS, scale=1.0 / 64)
            P.op('dve', lambda e, o=dn.ap[:, 0:4], a=dn.ap[:, 4:8]: e.reciprocal(o, a), [dn], [dn])
            P.tt('dve', sq.ap[:, :].rearrange("p (h d) -> p h d", d=64), hh.ap[:, :].rearrange("p (h d) -> p h d", d=64),
                 dn.ap[:, 0:4].unsqueeze(2).to_broadcast([128, 4, 64]), ALU.mult, [hh, dn], [sq])
            P.tt('dve', og.ap[:], sq.ap[:], gs.ap[:], ALU.mult, [sq, gs], [og])
            pt = pT[i % 2]
            for c2 in range(2):
                P.tr(pt.ap[:, c2 * 128:(c2 + 1) * 128], og.ap[:, c2 * 128:(c2 + 1) * 128], ident_bf.ap[:], [og, ident_bf], [pt])
            P.cp('act', mixT.ap[:, mc0:mc0 + 2, i * 128:(i + 1) * 128], pt.ap[:, 0:256].rearrange("p (c t) -> p c t", t=128),
                 [pt], [mixb[mg][i]])
        P.fence()
    P.es = L["es"]


BRANCH_BUILDERS["mlstm"] = lambda L: branch_scan(L, "mlstm")
BRANCH_BUILDERS["gla"] = lambda L: branch_scan(L, "gla")


def hcol(i):
    return 2 + i * 128 if i < 2 else 262 + (i - 2) * 128


def branch_hyena(L):
    P, nc, l, last, hy = L["P"], L["nc"], L["l"], L["last"], L["hy"]
    hT, hTb, mixT, mixb, pT, pf = L["hT"], L["hTb"], L["mixT"], L["mixb"], L["pT"], L["pf"]
    ident_bf, ident_f = L["ident_bf"], L["ident_f"]
    load_w, proj_fm = L["load_w"], L["proj_fm"]
    PI = 3.1415925
    TWO_PI = 6.283185307179586
    W = 2312
    with ExitStack() as es2:
        P.es = es2
        RC = P.sb("h_RC", [128, 16, 512], BF16)
        RS = P.sb("h_RS", [128, 16, 512], BF16)
        RCc = P.sb("h_RCc", [128, 2, 512], BF16)
        RSc = P.sb("h_RSc", [128, 2, 512], BF16)
        uT = P.sb("h_uT", [128, 2, W], BF16)
        x0c = P.sb("h_x0c", [128, 2, W], BF16)
        sgT = P.sb("h_sgT", [128, 2, W], BF16)
        vcol = P.sb("h_vcol", [128, 26], F32)
        fb = P.sb("h_fb", [64, 6], F32)
        wfl = P.sb("h_wfl", [128, 16], F32)
        wfc = P.sb("h_wfc", [128, 2], F32)
        ntl = P.sb("h_ntl", [128, 16], F32)
        ntc = P.sb("h_ntc", [128, 2], F32)
        delt = P.sb("h_delt", [128, 256], F32)
        P.dma('sp', wfl.ap[:], hy["wf_l"], writes=[wfl])
        P.dma('sp', wfc.ap[:], hy["wf_c"], writes=[wfc])
        P.dma('sp', ntl.ap[:], hy["ntcol_l"], writes=[ntl])
        P.dma('sp', ntc.ap[:], hy["ntcol_c"], writes=[ntc])
        P.dma('sp', delt.ap[:], hy["deltas"].partition_broadcast(128), writes=[delt])
        do_ctx = not last

        with ExitStack() as es3:
            P.es = es3
            vr = P.sb("h_vr", [26, 128], F32)
            fr = P.sb("h_fr", [4, 64], F32)
            P.dma('sp', vr.ap[0:18, :], hy["hyena_conv_w"][l].rearrange("j (c p) -> (j c) p", p=128), writes=[vr])
            P.dma('sp', vr.ap[18:24, :], hy["hyena_conv_b"][l].rearrange("(c p) -> c p", p=128), writes=[vr])
            P.dma('sp', vr.ap[24:26, :], hy["hyena_d"][l].rearrange("(c p) -> c p", p=128), writes=[vr])
            P.dma('sp', fr.ap[0:1, :], hy["hyena_b1"][l:l + 1, :], writes=[fr])
            P.dma('sp', fr.ap[1:2, :], hy["hyena_b2"][l:l + 1, :], writes=[fr])
            P.dma('sp', fr.ap[2:4, :], hy["hyena_freq"][l], writes=[fr])
            P.tr(pf[0].ap[:, 0:26], vr.ap[:], ident_f.ap[0:26, 0:26], [vr, ident_f], [pf[0]])
            P.cp('dve', vcol.ap[:], pf[0].ap[:, 0:26], [pf[0]], [vcol])
            P.tr(pf[1].ap[0:64, 0:4], fr.ap[:], ident_f.ap[0:4, 0:4], [fr, ident_f], [pf[1]])
            P.cp('dve', fb.ap[:, 0:4], pf[1].ap[0:64, 0:4], [pf[1]], [fb])
            P.tt('dve', fb.ap[:, 4:6], fb.ap[:, 0:2], fb.ap[:, 2:4], ALU.mult, [fb], [fb])

            P.ckpt("hv")
            zTl = P.sb("h_zTl", [33, 2048], F32)
            zTc = P.sb("h_zTc", [33, 256], F32)
            w1s = P.sb("h_w1", [33, 64], F32)
            w2s = P.sb("h_w2", [64, 64], F32)
            w3a = P.sb("h_w3a", [65, 512], F32)
            arg = P.sb("h_arg", [64, 512], F32)
            t1 = P.sb("h_t1", [64, 512], F32)
            t2 = P.sb("h_t2", [64, 512], F32)
            h1 = P.sb("h_h1", [64, 512], F32)
            h2 = P.sb("h_h2", [65, 512], F32)
            win = P.sb("h_win", [128, 256], F32)
            hf = P.sb("h_hf", [128, 256], F32)
            hb = P.sb("h_hb", [128, 256], F32)
            P.dma('sp', zTl.ap[:], hy["zT_l"], writes=[zTl])
            P.dma('sp', zTc.ap[:], hy["zT_c"], writes=[zTc])
            P.dma('sp', w1s.ap[:], hy["hyena_w1"][l], writes=[w1s])
            P.dma('sp', w2s.ap[:], hy["hyena_w2"][l], writes=[w2s])
            P.dma('sp', w3a.ap[0:64, :], hy["hyena_w3"][l], writes=[w3a])
            P.dma('sp', w3a.ap[64:65, :], hy["hyena_b3"][l:l + 1, :], writes=[w3a])
            P.op('pool', lambda e: e.memset(h2.ap[:], 1.0), writes=[h2])

            def sin_layer(ps_buf, ps_ap, fc_, fbc_, out_ap, outbuf, nn):
                P.act(arg.ap[:, 0:nn], ps_ap, AF.Identity, [ps_buf, fb], [arg], scale=fb.ap[:, fc_:fc_ + 1],
                      bias=fb.ap[:, fbc_:fbc_ + 1])
                P.ts('dve', t1.ap[:, 0:nn], arg.ap[:, 0:nn], PI, TWO_PI, ALU.is_gt, ALU.mult, [arg], [t1])
                P.ts('dve', t2.ap[:, 0:nn], arg.ap[:, 0:nn], -PI, TWO_PI, ALU.is_lt, ALU.mult, [arg], [t2])
                P.tt('dve', arg.ap[:, 0:nn], arg.ap[:, 0:nn], t1.ap[:, 0:nn], ALU.subtract, [arg, t1], [arg])
                P.tt('dve', arg.ap[:, 0:nn], arg.ap[:, 0:nn], t2.ap[:, 0:nn], ALU.add, [arg, t2], [arg])
                P.act(out_ap, arg.ap[:, 0:nn], AF.Sin, [arg], [outbuf])

            def mlp_block(zsrc, n0, nn, ntc_, RCd, RSd, tile0):
                P.mm(pf[0].ap[0:64, 0:nn], w1s.ap[:], zsrc.ap[:, n0:n0 + nn], True, True, [w1s, zsrc], [pf[0]])
                sin_layer(pf[0], pf[0].ap[0:64, 0:nn], 2, 4, h1.ap[:, 0:nn], h1, nn)
                P.mm(pf[1].ap[0:64, 0:nn], w2s.ap[:], h1.ap[:, 0:nn], True, True, [w2s, h1], [pf[1]])
                sin_layer(pf[1], pf[1].ap[0:64, 0:nn], 3, 5, h2.ap[0:64, 0:nn], h2, nn)
                for tt_ in range(nn // 128):
                    dt = tile0 + tt_
                    pp = pf[2 + tt_ % 2]
                    P.mm(pp.ap[:, 0:512], h2.ap[0:65, tt_ * 128:(tt_ + 1) * 128], w3a.ap[:], True, True, [h2, w3a], [pp])
                    P.act(win.ap[:], delt.ap[:], AF.Exp, [delt, ntc_], [win], scale=ntc_.ap[:, dt:dt + 1])
                    P.tt('dve', hf.ap[:], pp.ap[:, 0:256], win.ap[:], ALU.mult, [pp, win], [hf])
                    P.tt('dve', hb.ap[:], pp.ap[:, 256:512], win.ap[:], ALU.mult, [pp, win], [hb])
                    if dt == 0:
                        P.op('pool', lambda e: e.memset(hb.ap[0:1, :], 0.0), reads=[hb], writes=[hb])
                    P.tt('dve', RCd.ap[:, dt, 256:512], hf.ap[:], hb.ap[:], ALU.add, [hf, hb], [RCd])
                    P.tt('dve', RSd.ap[:, dt, 256:512], hf.ap[:], hb.ap[:], ALU.subtract, [hf, hb], [RSd])

            for blk in range(4):
                mlp_block(zTl, blk * 512, 512, ntl, RC, RS, blk * 4)
                P.ckpt("hm%d" % blk)
            if do_ctx:
                mlp_block(zTc, 0, 256, ntc, RCc, RSc, 0)
            P.fence()
        P.es = es2

        with ExitStack() as es3:
            P.es = es3
            L["alloc_wbuf"]()
            raw = P.sb("h_raw", [128, W], F32)
            cv = P.sb("h_cv", [128, W], F32)
            P.op('pool', lambda e: e.memset(raw.ap[:], 0.0), writes=[raw])
            wA = load_w(l, COL_H, 512)
            wB = load_w(l, COL_H + 512, 512)
            groups = [(0, 256), (256, 512), (768, 512), (1280, 512), (1792, 512)]
            cnt = 0
            for c6 in range(8):
                wsel = wA if c6 < 4 else wB
                coff = (c6 % 4) * 128
                for (t0, nt) in groups:
                    pp = pf[cnt % 2]
                    cnt += 1
                    proj_fm(wsel, coff, 128, t0, nt, pp.ap[:, 0:nt], pp)
                    c0 = 2 + t0 if t0 < 256 else t0 + 6
                    if c6 < 6:
                        P.cp('act', raw.ap[:, c0:c0 + nt], pp.ap[:, 0:nt], [pp], [raw])
                    else:
                        P.act(sgT.ap[:, c6 - 6, c0:c0 + nt], pp.ap[:, 0:nt], AF.Silu, [pp], [sgT])
                if c6 >= 6:
                    continue
                w0c, w1c, w2c = (vcol.ap[:, j * 6 + c6:j * 6 + c6 + 1] for j in range(3))
                bc = vcol.ap[:, 18 + c6:19 + c6]
                P.ts('dve', cv.ap[:, 1:W - 1], raw.ap[:, 1:W - 1], w1c, bc, ALU.mult, ALU.add, [raw, vcol], [cv])
                P.stt(cv.ap[:, 1:W - 1], raw.ap[:, 0:W - 2], w0c, cv.ap[:, 1:W - 1], ALU.mult, ALU.add, [raw, vcol, cv], [cv])
                if c6 < 2:
                    P.stt(uT.ap[:, c6, 1:W - 1], raw.ap[:, 2:W], w2c, cv.ap[:, 1:W - 1], ALU.mult, ALU.add, [raw, vcol, cv], [uT])
                elif c6 < 4:
                    P.stt(cv.ap[:, 1:W - 1], raw.ap[:, 2:W], w2c, cv.ap[:, 1:W - 1], ALU.mult, ALU.add, [raw, vcol, cv], [cv])
                    P.tt('dve', uT.ap[:, c6 - 2, 1:W - 1], uT.ap[:, c6 - 2, 1:W - 1], cv.ap[:, 1:W - 1], ALU.mult, [uT, cv], [uT])
                else:
                    P.stt(x0c.ap[:, c6 - 4, 1:W - 1], raw.ap[:, 2:W], w2c, cv.ap[:, 1:W - 1], ALU.mult, ALU.add,
                          [raw, vcol, cv], [x0c])
            P.fence()
        P.es = es2

        P.ckpt("hp")
        for i in range(NT):
            if i < 2 and not do_ctx:
                continue
            c = hcol(i)
            pt = pT[i % 2]
            for cj in range(2):
                P.tr(pt.ap[:, cj * 128:(cj + 1) * 128], uT.ap[:, cj, c:c + 128], ident_bf.ap[:], [uT, ident_bf], [pt])
            dC, dS, dt = (RCc, RSc, i) if i < 2 else (RC, RS, i - 2)
            P.cp('act', dC.ap[:, dt, 0:256], pt.ap[:, 0:256], [pt], [dC])
            P.cp('dve', dS.ap[:, dt, 0:256], pt.ap[:, 0:256], [pt], [dS])

        P.ckpt("ht")
        Ysp = P.sb("h_Y", [128, 16, 512], BF16)
        Yc = P.sb("h_Yc", [128, 2, 512], BF16)
        kre = P.sb("h_kre", [128, 256], F32)
        kim = P.sb("h_kim", [128, 256], F32)
        ta = [P.sb("h_ta%d" % i, [128, 256], F32) for i in range(4)]
        fin = P.sb("h_fin", [128, 512], F32)
        cbuf = [P.sb("h_cb%d" % i, [128, 16, 128], BF16) for i in range(2)]
        sbuf_ = [P.sb("h_sb%d" % i, [128, 16, 128], BF16) for i in range(2)]
        ibc = [P.sb("h_ibc%d" % i, [128, 512], BF16) for i in range(2)]
        ibs = [P.sb("h_ibs%d" % i, [128, 512], BF16) for i in range(2)]
        rot = [0]

        def dft_conv(RCx, RSx, Yx, nT, tag, wfx, tgroups, col_base, tok_base):
            Ct, St, Cn, Sn = hy["Ct_" + tag], hy["St_" + tag], hy["Cn_" + tag], hy["Sn_" + tag]
            for fc in range(nT):
                b = fc % 2
                P.dma('sp', cbuf[b].ap[:, 0:nT, :], Ct[fc], writes=[cbuf[b]])
                P.dma('sp', sbuf_[b].ap[:, 0:nT, :], St[fc], writes=[sbuf_[b]])
                pc, ps_ = pf[2 * b], pf[2 * b + 1]
                for tc in range(nT):
                    P.mm(pc.ap[:, 0:512], cbuf[b].ap[:, tc, :], RCx.ap[:, tc, :], tc == 0, tc == nT - 1, [cbuf[b], RCx], [pc])
                for tc in range(nT):
                    P.mm(ps_.ap[:, 0:512], sbuf_[b].ap[:, tc, :], RSx.ap[:, tc, :], tc == 0, tc == nT - 1, [sbuf_[b], RSx], [ps_])
                P.act(kre.ap[:], pc.ap[:, 256:512], AF.Identity, [pc, wfx], [kre], scale=wfx.ap[:, fc:fc + 1])
                P.act(kim.ap[:], ps_.ap[:, 256:512], AF.Identity, [ps_, wfx], [kim], scale=wfx.ap[:, fc:fc + 1])
                P.tt('dve', ta[0].ap[:], pc.ap[:, 0:256], kre.ap[:], ALU.mult, [pc, kre], [ta[0]])
                P.tt('dve', ta[1].ap[:], ps_.ap[:, 0:256], kim.ap[:], ALU.mult, [ps_, kim], [ta[1]])
                P.tt('dve', Yx.ap[:, fc, 0:256], ta[0].ap[:], ta[1].ap[:], ALU.subtract, [ta[0], ta[1]], [Yx])
                P.tt('dve', ta[2].ap[:], pc.ap[:, 0:256], kim.ap[:], ALU.mult, [pc, kim], [ta[2]])
                P.tt('dve', ta[3].ap[:], ps_.ap[:, 0:256], kre.ap[:], ALU.mult, [ps_, kre], [ta[3]])
                P.tt('dve', Yx.ap[:, fc, 256:512], ta[2].ap[:], ta[3].ap[:], ALU.add, [ta[2], ta[3]], [Yx])
            P.ckpt("hf" + tag)
            for gi, (t0, nt) in enumerate(tgroups):
                py = [pf[4], pf[5]]
                for fc in range(nT):
                    bb = rot[0] % 2
                    rot[0] += 1
                    P.dma('sp', ibc[bb].ap[:, 0:nt], Cn[fc * 128:(fc + 1) * 128, t0:t0 + nt], writes=[ibc[bb]])
                    P.dma('sp', ibs[bb].ap[:, 0:nt], Sn[fc * 128:(fc + 1) * 128, t0:t0 + nt], writes=[ibs[bb]])
                    for cj in range(2):
                        P.mm(py[cj].ap[:, 0:nt], Yx.ap[:, fc, cj * 128:(cj + 1) * 128], ibc[bb].ap[:, 0:nt], fc == 0, False,
                             [Yx, ibc[bb]], [py[cj]])
                        P.mm(py[cj].ap[:, 0:nt], Yx.ap[:, fc, 256 + cj * 128:384 + cj * 128], ibs[bb].ap[:, 0:nt], False,
                             fc == nT - 1, [Yx, ibs[bb]], [py[cj]])
                for cj in range(2):
                    cs = slice(col_base + t0, col_base + t0 + nt)
                    tk0 = tok_base + t0
                    tiles = list(range(tk0 // 128, (tk0 + nt) // 128))
                    P.stt(fin.ap[:, 0:nt], uT.ap[:, cj, cs], vcol.ap[:, 24 + cj:25 + cj], py[cj].ap[:, 0:nt], ALU.mult, ALU.add,
                          [uT, vcol, py[cj]], [fin])
                    P.tt('dve', fin.ap[:, 0:nt], fin.ap[:, 0:nt], x0c.ap[:, cj, cs], ALU.mult, [fin, x0c], [fin])
                    P.tt('dve', mixT.ap[:, 2 + cj, tk0:tk0 + nt], fin.ap[:, 0:nt], sgT.ap[:, cj, cs], ALU.mult, [fin, sgT],
                         [mixb[1][t] for t in tiles])

        dft_conv(RC, RS, Ysp, 16, "l", wfl, [(0, 512), (512, 512), (1024, 512), (1536, 512)], 262, 256)
        if do_ctx:
            dft_conv(RCc, RSc, Yc, 2, "c", wfc, [(0, 256)], 2, 0)
        P.fence()
    P.es = L["es"]


BRANCH_BUILDERS["hyena"] = branch_hyena


def kernel(**inputs):
    nc = build(n_layers=4)
    maps = make_in_maps(inputs)
    res = run_bass_kernel_spmd(nc, maps, core_ids=list(range(8)))
    out = np.stack([np.asarray(res.results[b]["y"]).astype(np.float32) for b in range(8)], 0)
    return out
```

```python
import numpy as np
from contextlib import ExitStack
import concourse.bass as bass
import concourse.mybir as mybir
from concourse.bass_utils import run_bass_kernel_spmd

F32 = mybir.dt.float32
BF16 = mybir.dt.bfloat16
AF = mybir.ActivationFunctionType
ALU = mybir.AluOpType
AX = mybir.AxisListType

ND_SEMS = 40
VERBOSE = False
STOP = None


class Buf:
    def __init__(self, name, ap=None):
        self.name = name
        self.ap = ap
        self.writer = None
        self.readers = {}
        self.excl = False

    def __getitem__(self, k):
        return self.ap[k]


class Prog:
    def __init__(self, nc, es):
        self.nc = nc
        self.es = es
        self.engs = ['pe', 'act', 'dve', 'pool', 'sp']
        self.lists = {e: [] for e in self.engs}
        self.cnt = {e: 0 for e in self.engs}
        self.sem = {e: es.enter_context(nc.semaphore("s_" + e)) for e in ['pe', 'act', 'dve', 'pool']}
        self.dsem = [es.enter_context(nc.semaphore("d%d" % i)) for i in range(ND_SEMS)]
        self.dcnt = [0] * ND_SEMS
        self.dnext = 0
        self.dnext_sw = 0
        self.waited = {e: {} for e in self.engs}
        self.out_tokens = []
        self.nbuf = 0
        self.dead = False

    def sb(self, name, shape, dtype):
        self.nbuf += 1
        name = "%s_u%d" % (name, self.nbuf)
        t = self.es.enter_context(self.nc.sbuf_tensor(name, shape, dtype))
        assert self.nc.sbuf_bytes_remaining >= 16384 + 256, (name, self.nc.sbuf_bytes_remaining)
        return Buf(name, t)

    def ps(self, name, shape, dtype):
        t = self.es.enter_context(self.nc.psum_tensor(name, shape, dtype))
        b = Buf(name, t)
        b.excl = True
        return b

    def vb(self, name, ap=None):
        return Buf(name, ap)

    def _collect(self, eng, reads, writes):
        deps = []
        for b in reads:
            if b.writer is not None:
                deps.append(b.writer)
        for b in writes:
            if b.writer is not None:
                deps.append(b.writer)
            deps.extend(b.readers.values())
        waits = []
        w = self.waited[eng]
        for tok in deps:
            if tok[0] == 'e':
                if tok[1] == 'pe' and eng == 'pe':
                    continue
                key = tok[1]
                sem = self.sem[tok[1]]
            else:
                key = ('d', tok[1])
                sem = self.dsem[tok[1]]
            if w.get(key, 0) >= tok[2]:
                continue
            w[key] = tok[2]
            waits.append((sem, tok[2], key))
        best = {}
        for sem, val, key in waits:
            if key not in best or best[key][1] < val:
                best[key] = (sem, val)
        return list(best.values())

    def _mark(self, tok, rkey, reads, writes):
        for b in reads:
            b.readers[rkey] = tok
        for b in writes:
            b.writer = tok
            b.readers = {}

    def ckpt(self, name):
        if STOP is not None and STOP == name:
            self.dead = True

    def op(self, eng, fn, reads=(), writes=()):
        if self.dead:
            return
        if eng != 'pe':
            ex = [b for b in reads if b.excl]
            if ex:
                writes = list(writes) + [b for b in ex if b not in writes]
        waits = self._collect(eng, reads, writes)
        self.cnt[eng] += 1
        tok = ('e', eng, self.cnt[eng])
        self.lists[eng].append((waits, fn, ('inc', self.sem[eng])))
        self._mark(tok, eng, reads, writes)

    def dma(self, q, out_ap, in_ap, reads=(), writes=(), out=False, **kw):
        if self.dead and not out:
            return
        waits = self._collect(q, reads, writes)
        if q == 'pool':
            i = ND_SEMS - 8 + self.dnext_sw
            self.dnext_sw = (self.dnext_sw + 1) % 8
        else:
            i = self.dnext
            self.dnext = (self.dnext + 1) % (ND_SEMS - 8)
        if self.dcnt[i] > 0:
            key = ('d', i)
            val = self.dcnt[i] * 16
            if self.waited[q].get(key, 0) < val:
                self.waited[q][key] = val
                waits.append((self.dsem[i], val))
        self.dcnt[i] += 1
        tok = ('d', i, self.dcnt[i] * 16)
        fn = lambda e, o=out_ap, s=in_ap, k=kw: e.dma_start(out=o, in_=s, **k)
        self.lists[q].append((waits, fn, ('dinc', self.dsem[i])))
        self._mark(tok, ('d', i), reads, writes)
        if out:
            self.out_tokens.append(tok)

    def finish(self):
        waits = []
        for tok in self.out_tokens:
            waits.append((self.dsem[tok[1]], tok[2]))
        self.lists['sp'].append((waits, None, None))
        nc = self.nc
        lists = self.lists

        def replay(name, e):
            for waits, fn, inc in lists[name]:
                for sem, val in waits:
                    e.wait_ge(sem, val)
                if fn is None:
                    continue
                ins = fn(e)
                if inc[0] == 'inc':
                    ins.then_inc(inc[1], 1)
                else:
                    ins.then_inc(inc[1], 16)

        with nc.Block() as block:
            @block.sync
            def _(e):
                replay('sp', e)

            @block.tensor
            def _(e):
                replay('pe', e)

            @block.scalar
            def _(e):
                replay('act', e)

            @block.vector
            def _(e):
                replay('dve', e)

            @block.gpsimd
            def _(e):
                replay('pool', e)


    def mm(self, out, lhsT, rhs, start, stop, reads, writes):
        self.op('pe', lambda e: e.matmul(out, lhsT, rhs, start=start, stop=stop), reads, writes)

    def tr(self, out, in_, ident, reads, writes):
        self.op('pe', lambda e: e.transpose(out, in_, ident), reads, writes)

    def act(self, out, in_, func, reads, writes, **kw):
        self.op('act', lambda e: e.activation(out, in_, func, **kw), reads, writes)

    def tt(self, eng, out, in0, in1, op, reads, writes):
        self.op(eng, lambda e: e.tensor_tensor(out, in0, in1, op), reads, writes)

    def ts(self, eng, out, in0, s1, s2, op0, op1, reads, writes):
        self.op(eng, lambda e: e.tensor_scalar(out, in0, s1, s2, op0, op1), reads, writes)

    def stt(self, out, in0, scalar, in1, op0, op1, reads, writes):
        self.op('dve', lambda e: e.scalar_tensor_tensor(out, in0, scalar, in1, op0, op1), reads, writes)

    def cp(self, eng, out, in_, reads, writes):
        if eng == 'act':
            self.op('act', lambda e: e.copy(out, in_), reads, writes)
        else:
            self.op(eng, lambda e: e.tensor_copy(out, in_), reads, writes)

    def fence(self):
        for e in self.engs:
            waits = []
            for o in ['pe', 'act', 'dve', 'pool']:
                if not (o == e == 'pe') and self.cnt[o] > 0 and self.waited[e].get(o, 0) < self.cnt[o]:
                    self.waited[e][o] = self.cnt[o]
                    waits.append((self.sem[o], self.cnt[o]))
            for i in range(ND_SEMS):
                if self.dcnt[i] > 0 and self.waited[e].get(('d', i), 0) < self.dcnt[i] * 16:
                    self.waited[e][('d', i)] = self.dcnt[i] * 16
                    waits.append((self.dsem[i], self.dcnt[i] * 16))
            self.lists[e].append((waits, None, None))


D = 1024
NT = 18
TOK = NT * 128
EPS = 1e-6
N_IN = 4144
COL_A, COL_H, COL_G, COL_D = 0, 1296, 2320, 3376


def host_consts():
    c = {}
    pos = np.arange(2048)
    row = (pos // 64).astype(np.float64)
    col = (pos % 64).astype(np.float64)
    inv = 10000.0 ** (-np.arange(16, dtype=np.float64) / 16)
    ang = np.concatenate([row[:, None] * inv, col[:, None] * inv], -1)
    c["ropec"] = np.ascontiguousarray(np.cos(ang).reshape(16, 128, 32).transpose(1, 0, 2)).astype(np.float32)
    c["ropes"] = np.ascontiguousarray(np.sin(ang).reshape(16, 128, 32).transpose(1, 0, 2)).astype(np.float32)
    t = np.arange(128)[:, None]
    s_ = np.arange(128)[None, :]
    m = np.zeros((128, 384), np.float32)
    m[:, 0:128] = np.where(s_ >= t, 0.0, -30000.0)
    m[:, 256:384] = np.where(s_ <= t, 0.0, -30000.0)
    c["amask"] = m
    import ml_dtypes
    bf = ml_dtypes.bfloat16
    for tag, Lh in (("l", 2048), ("c", 256)):
        pos = np.arange(Lh, dtype=np.float32)
        t = (pos / np.float32(max(Lh - 1, 1))).astype(np.float32)
        w = (np.float32(2.0 * np.pi) * pos / np.float32(Lh)).astype(np.float32)
        f = np.linspace(1e-4, 15, 16, dtype=np.float32)
        z = np.concatenate([t[:, None], np.cos(w[:, None] * f), -np.sin(w[:, None] * f)], -1).astype(np.float32)
        c["zT_" + tag] = np.ascontiguousarray(z.T)
        nt_ = Lh // 128
        c["ntcol_" + tag] = np.ascontiguousarray((-t).reshape(nt_, 128).T)
        N = 2 * Lh - 1
        idx = (np.outer(np.arange(Lh, dtype=np.int64), np.arange(Lh, dtype=np.int64)) % N).astype(np.float64)
        ang = 2.0 * np.pi * idx / N
        C = np.cos(ang)
        S = np.sin(ang)
        c["Cn_" + tag] = np.ascontiguousarray(C.astype(np.float32).astype(bf))
        c["Sn_" + tag] = np.ascontiguousarray(S.astype(np.float32).astype(bf))
        c["Ct_" + tag] = np.ascontiguousarray(C.reshape(nt_, 128, nt_, 128).transpose(2, 1, 0, 3).astype(np.float32).astype(bf))
        c["St_" + tag] = np.ascontiguousarray(S.reshape(nt_, 128, nt_, 128).transpose(2, 1, 0, 3).astype(np.float32).astype(bf))
        wf = np.full(Lh, 2.0 / N, np.float32)
        wf[0] = 1.0 / N
        c["wf_" + tag] = np.ascontiguousarray(wf.reshape(nt_, 128).T)
    import math
    dmin, dmax = math.log(1e-2) / 1.5, math.log(1e-2) / 0.3
    c["deltas"] = np.abs(np.linspace(dmin, dmax, 256, dtype=np.float32)).reshape(1, 256).astype(np.float32)
    return c


def build(n_layers=4, tap=None, branches=("att", "mlstm", "gla", "hyena")):
    nc = bass.Bass("TRN2", target_bir_lowering=False)

    def din(name, shape, dt=F32):
        return nc.dram_tensor(name, shape, dt, kind="ExternalInput").ap()

    x_in = din("x", [2048, D])
    ctx_in = din("ctx", [256, D])
    cc_in = din("cc", [16, 128])
    w_ada = din("w_ada", [4, D, 3 * D])
    b_ada = din("b_ada", [4, 3 * D])
    g_pre = din("g_pre", [4, D])
    g_post = din("g_post", [4, D])
    w_in = din("w_in", [4, D, N_IN])
    w_out = din("w_out", [4, D, D])
    mlstm_gate_b = din("mlstm_gate_b", [4, 16])
    mlstm_norm_g = din("mlstm_norm_g", [4, 256])
    gla_norm_g = din("gla_norm_g", [4, 256])
    gla_w_alpha = din("gla_w_alpha", [4, 2, 16, 256])
    gla_b_alpha = din("gla_b_alpha", [4, 2, 256])
    attn_sink = din("attn_sink", [4, 4])
    ropec_in = din("ropec", [128, 16, 32])
    ropes_in = din("ropes", [128, 16, 32])
    amask_in = din("amask", [128, 384])
    hy = {}
    for nm, shp in (("hyena_conv_w", [4, 3, 768]), ("hyena_conv_b", [4, 768]), ("hyena_w1", [4, 33, 64]),
                    ("hyena_b1", [4, 64]), ("hyena_w2", [4, 64, 64]), ("hyena_b2", [4, 64]), ("hyena_w3", [4, 64, 512]),
                    ("hyena_b3", [4, 512]), ("hyena_freq", [4, 2, 64]), ("hyena_d", [4, 256]), ("deltas", [1, 256])):
        hy[nm] = din(nm, shp)
    for tag, Lh in (("l", 2048), ("c", 256)):
        nt_ = Lh // 128
        hy["zT_" + tag] = din("zT_" + tag, [33, Lh])
        hy["ntcol_" + tag] = din("ntcol_" + tag, [128, nt_])
        hy["wf_" + tag] = din("wf_" + tag, [128, nt_])
        hy["Cn_" + tag] = din("Cn_" + tag, [Lh, Lh], BF16)
        hy["Sn_" + tag] = din("Sn_" + tag, [Lh, Lh], BF16)
        hy["Ct_" + tag] = din("Ct_" + tag, [nt_, 128, nt_, 128], BF16)
        hy["St_" + tag] = din("St_" + tag, [nt_, 128, nt_, 128], BF16)
    y_out = nc.dram_tensor("y", [2048, D], F32, kind="ExternalOutput").ap()
    ctxs = nc.dram_tensor("ctxs", [256, D], F32).ap()
    tap_out = None
    if tap is not None:
        tap_out = nc.dram_tensor("tap", [128, 8, TOK], BF16, kind="ExternalOutput").ap()

    w_in_v = [w_in[l].rearrange("(kc p) n -> p kc n", p=128) for l in range(4)]
    w_out_v = [w_out[l].rearrange("(kc p) n -> p kc n", p=128) for l in range(4)]
    w_ada_v = [w_ada[l].rearrange("(kc p) n -> p kc n", p=128) for l in range(4)]

    with ExitStack() as es:
        P = Prog(nc, es)
        ident_bf = P.sb("ident_bf", [128, 128], BF16)
        ident_f = P.sb("ident_f", [128, 128], F32)
        tri_le = P.sb("tri_le", [128, 128], F32)
        tri_ge = P.sb("tri_ge", [128, 128], F32)
        tri_gt = P.sb("tri_gt", [128, 128], F32)
        tri_lt = P.sb("tri_lt", [128, 128], F32)
        ropec = P.sb("ropec_sb", [128, 16, 32], F32)
        ropes = P.sb("ropes_sb", [128, 16, 32], F32)
        amask = P.sb("amask_sb", [128, 384], F32)

        def mk_affine(buf, pattern, cm, cmp_op, fill_in=1.0):
            P.op('pool', lambda e: e.memset(buf.ap[:], fill_in), writes=[buf])
            P.op('pool', lambda e: e.affine_select(buf.ap[:], buf.ap[:], pattern=pattern, compare_op=cmp_op,
                                                   fill=0.0, base=0, channel_multiplier=cm), reads=[buf], writes=[buf])

        mk_affine(ident_bf, [[-1, 128]], 1, ALU.is_equal)
        mk_affine(ident_f, [[-1, 128]], 1, ALU.is_equal)
        mk_affine(tri_le, [[1, 128]], -1, ALU.is_ge)
        mk_affine(tri_ge, [[-1, 128]], 1, ALU.is_ge)
        mk_affine(tri_gt, [[-1, 128]], 1, ALU.is_gt)
        mk_affine(tri_lt, [[1, 128]], -1, ALU.is_gt)
        tri_le_b = P.sb("tri_le_b", [128, 128], BF16)
        tri_ge_b = P.sb("tri_ge_b", [128, 128], BF16)
        tri_gt_b = P.sb("tri_gt_b", [128, 128], BF16)
        tri_lt_b = P.sb("tri_lt_b", [128, 128], BF16)
        mk_affine(tri_le_b, [[1, 128]], -1, ALU.is_ge)
        mk_affine(tri_ge_b, [[-1, 128]], 1, ALU.is_ge)
        mk_affine(tri_gt_b, [[-1, 128]], 1, ALU.is_gt)
        mk_affine(tri_lt_b, [[1, 128]], -1, ALU.is_gt)
        P.dma('sp', ropec.ap[:], ropec_in, writes=[ropec])
        P.dma('sp', ropes.ap[:], ropes_in, writes=[ropes])
        P.dma('sp', amask.ap[:], amask_in, writes=[amask])

        pT = [P.ps("pT%d" % i, [128, 1024], BF16) for i in range(2)]
        pf = [P.ps("pf%d" % i, [128, 512], F32) for i in range(6)]

        MA = P.sb("MA", [128, 4, 8, 2], F32)
        MB = P.sb("MB", [128, 4, 8, 2], F32)
        MG = P.sb("MG", [128, 4, 8, 2], F32)

        with ExitStack() as es2:
            P.es = es2
            ccr = P.sb("ccr", [16, 128], F32)
            scs = P.sb("scs", [128, 16], F32)
            sc2 = P.sb("sc2", [128, 8, 2], F32)
            VR1 = P.sb("VR1", [128, 128], F32)
            VR2 = P.sb("VR2", [32, 128], F32)
            VC1 = P.sb("VC1", [128, 128], F32)
            VC2 = P.sb("VC2", [128, 32], F32)
            modc = P.sb("modc", [128, 24, 2], F32)
            wa = [P.sb("wa%d" % i, [128, 8, 512], F32) for i in range(2)]
            P.dma('sp', ccr.ap[:], cc_in, writes=[ccr])
            P.dma('sp', VR1.ap[0:96, :], b_ada.rearrange("l (c p) -> (l c) p", p=128), writes=[VR1])
            P.dma('sp', VR1.ap[96:128, :], g_pre.rearrange("l (c p) -> (l c) p", p=128), writes=[VR1])
            P.dma('sp', VR2.ap[:], g_post.rearrange("l (c p) -> (l c) p", p=128), writes=[VR2])
            P.tr(pf[0].ap[:, 0:16], ccr.ap[:], ident_f.ap[0:16, 0:16], [ccr, ident_f], [pf[0]])
            P.act(scs.ap[:], pf[0].ap[:, 0:16], AF.Silu, [pf[0]], [scs])
            P.cp('dve', sc2.ap[:, :, 0], scs.ap[:, 0:8], [scs], [sc2])
            P.cp('dve', sc2.ap[:, :, 1], scs.ap[:, 8:16], [scs], [sc2])
            P.tr(pf[1].ap[:, 0:128], VR1.ap[:], ident_f.ap[:], [VR1, ident_f], [pf[1]])
            P.cp('dve', VC1.ap[:], pf[1].ap[:, 0:128], [pf[1]], [VC1])
            P.tr(pf[2].ap[:, 0:32], VR2.ap[:], ident_f.ap[0:32, 0:32], [VR2, ident_f], [pf[2]])
            P.cp('dve', VC2.ap[:], pf[2].ap[:, 0:32], [pf[2]], [VC2])
            gi = 0
            for l in range(n_layers):
                pm = pf[3 + (l % 2)]
                for g6 in range(6):
                    wb_ = wa[gi % 2]
                    gi += 1
                    P.dma('sp', wb_.ap[:], w_ada_v[l][:, :, g6 * 512:(g6 + 1) * 512], writes=[wb_])
                    for jj in range(4):
                        j = g6 * 4 + jj
                        for kc in range(8):
                            P.mm(pm.ap[:, j * 2:j * 2 + 2], wb_.ap[:, kc, jj * 128:(jj + 1) * 128], sc2.ap[:, kc, :],
                                 kc == 0, kc == 7, [wb_, sc2], [pm])
                pm3 = pm.ap[:, 0:48].rearrange("p (j s) -> p j s", s=2)
                P.tt('dve', modc.ap[:], pm3, VC1.ap[:, l * 24:(l + 1) * 24].unsqueeze(2).to_broadcast([128, 24, 2]),
                     ALU.add, [pm, VC1], [modc])
                gpre_bc = VC1.ap[:, 96 + l * 8:96 + (l + 1) * 8].unsqueeze(2).to_broadcast([128, 8, 2])
                gpost_bc = VC2.ap[:, l * 8:(l + 1) * 8].unsqueeze(2).to_broadcast([128, 8, 2])
                P.stt(MA.ap[:, l], modc.ap[:, 8:16, :], 1.0, gpre_bc, ALU.add, ALU.mult, [modc, VC1], [MA])
                P.cp('dve', MB.ap[:, l], modc.ap[:, 0:8, :], [modc], [MB])
                P.tt('dve', MG.ap[:, l], modc.ap[:, 16:24, :], gpost_bc, ALU.mult, [modc, VC2], [MG])
            P.fence()
        P.es = es

        hT = P.sb("hT", [128, 8, TOK], BF16)
        mixT = P.sb("mixT", [128, 8, TOK], BF16)
        hTb = [P.vb("hT_t%d" % i) for i in range(NT)]
        mixb = [[P.vb("mix_%d_%d" % (g, i)) for i in range(NT)] for g in range(4)]
        if tap is not None:
            P.op('pool', lambda e: e.memset(mixT.ap[:], 0.0), writes=[b for g in mixb for b in g])
        wbuf = []
        wrot = [0]

        def alloc_wbuf():
            wbuf[:] = [P.sb("wbuf%d" % i, [128, 8, 512], BF16) for i in range(2)]
        xs = [P.vb("xs%d" % i) for i in range(NT)]
        if VERBOSE:
            print("sbuf bytes remaining after main alloc:", nc.sbuf_bytes_remaining)

        def load_w(l, c0, ncols, q='pool'):
            b = wbuf[wrot[0] % 2]
            wrot[0] += 1
            P.dma(q, b.ap[:, :, 0:ncols], w_in_v[l][:, :, c0:c0 + ncols], writes=[b])
            return b

        def proj_tm(i, wb_, c0, ncols, out_ap, outbuf):
            for kc in range(8):
                P.mm(out_ap, hT.ap[:, kc, i * 128:(i + 1) * 128], wb_.ap[:, kc, c0:c0 + ncols], kc == 0, kc == 7,
                     [hTb[i], wb_], [outbuf])

        def proj_fm(wb_, c0, ncols, t0, nt, out_ap, outbuf):
            rd = [wb_] + [hTb[i] for i in range(t0 // 128, (t0 + nt + 127) // 128)]
            for kc in range(8):
                P.mm(out_ap, wb_.ap[:, kc, c0:c0 + ncols], hT.ap[:, kc, t0:t0 + nt], kc == 0, kc == 7, rd, [outbuf])

        def state_src(l, i):
            if i < 2:
                return (ctx_in if l == 0 else ctxs)[i * 128:(i + 1) * 128, :]
            return (x_in if l == 0 else y_out)[(i - 2) * 128:(i - 1) * 128, :]

        def state_dst(i):
            if i < 2:
                return ctxs[i * 128:(i + 1) * 128, :]
            return y_out[(i - 2) * 128:(i - 1) * 128, :]

        for l in range(n_layers):
            last = (l == 3)
            esC = ExitStack()
            P.es = esC
            xt = [P.sb("xt%d" % i, [128, 1024], F32) for i in range(2)]
            xn = [P.sb("xn%d" % i, [128, 1024], BF16) for i in range(2)]
            tmpf = P.sb("tmpf", [128, 1024], F32)
            st4 = [P.sb("st4_%d" % i, [128, 8], F32) for i in range(2)]
            for i in range(NT):
                s = 1 if i < 2 else 0
                xb_, xnb, stb = xt[i % 2], xn[i % 2], st4[i % 2]
                P.dma('sp', xb_.ap[:], state_src(l, i), reads=[xs[i]], writes=[xb_])
                P.act(tmpf.ap[:], xb_.ap[:], AF.Square, [xb_], [tmpf, stb], accum_out=stb.ap[:, 0:1])
                P.act(stb.ap[:, 1:2], stb.ap[:, 0:1], AF.Sqrt, [stb], [stb], bias=EPS, scale=1.0 / D)
                P.op('dve', lambda e, o=stb.ap[:, 2:3], a=stb.ap[:, 1:2]: e.reciprocal(o, a), [stb], [stb])
                P.act(xnb.ap[:], xb_.ap[:], AF.Identity, [xb_, stb], [xnb], scale=stb.ap[:, 2:3])
                pt = pT[i % 2]
                for kc in range(8):
                    P.tr(pt.ap[:, kc * 128:(kc + 1) * 128], xnb.ap[:, kc * 128:(kc + 1) * 128], ident_bf.ap[:],
                         [xnb, ident_bf], [pt])
                pt3 = pt.ap[:, :].rearrange("p (k t) -> p k t", t=128)
                tm3 = tmpf.ap[:, :].rearrange("p (k t) -> p k t", t=128)
                P.tt('dve', tm3, pt3, MA.ap[:, l, :, s:s + 1].to_broadcast([128, 8, 128]), ALU.mult, [pt, MA], [tmpf])
                P.tt('dve', hT.ap[:, :, i * 128:(i + 1) * 128], tm3, MB.ap[:, l, :, s:s + 1].to_broadcast([128, 8, 128]),
                     ALU.add, [tmpf, MB], [hTb[i]])
            if tap == "h%d" % l:
                P.dma('sp', tap_out, hT.ap[:], reads=hTb, out=True)

            P.fence()
            esC.close()
            P.es = es

            BRANCH_BUILDERS["att"](locals()) if "att" in branches else None
            BRANCH_BUILDERS["mlstm"](locals()) if "mlstm" in branches else None
            BRANCH_BUILDERS["gla"](locals()) if "gla" in branches else None
            BRANCH_BUILDERS["hyena"](locals()) if "hyena" in branches else None
            if tap == "mix%d" % l:
                P.dma('sp', tap_out, mixT.ap[:], reads=[b for g in mixb for b in g], out=True)

            esZ = ExitStack()
            P.es = esZ
            woutb = P.sb("woutb", [128, 8, 1024], BF16)
            Gbc = [P.sb("Gbc%d" % s, [128, 1024], F32) for s in range(2)]
            gbt = P.sb("gbt", [128, 128], F32)
            xt = [P.sb("xt%d" % i, [128, 1024], F32) for i in range(2)]
            tmpf = P.sb("tmpf", [128, 1024], F32)
            st4 = [P.sb("st4_%d" % i, [128, 8], F32) for i in range(2)]
            for h2 in range(2):
                P.dma('pool', woutb.ap[:, :, h2 * 512:(h2 + 1) * 512], w_out_v[l][:, :, h2 * 512:(h2 + 1) * 512],
                      writes=[woutb])
            for s in range(2):
                for j in range(8):
                    P.cp('dve', gbt.ap[:], MG.ap[:, l, j, s:s + 1].to_broadcast([128, 128]), [MG], [gbt])
                    pg = pf[j // 4]
                    P.mm(pg.ap[:, (j % 4) * 128:(j % 4 + 1) * 128], gbt.ap[:], ident_f.ap[:], True, True,
                         [gbt, ident_f], [pg])
                for h2 in range(2):
                    P.cp('act', Gbc[s].ap[:, h2 * 512:(h2 + 1) * 512], pf[h2].ap[:], [pf[h2]], [Gbc[s]])
            for i in range(NT):
                if last and i < 2:
                    continue
                s = 1 if i < 2 else 0
                xb_, stb = xt[i % 2], st4[i % 2]
                P.dma('sp', xb_.ap[:], state_src(l, i), reads=[xs[i]], writes=[xb_])
                py = [pf[2 + 2 * (i % 2)], pf[3 + 2 * (i % 2)]]
                mrd = [mixb[g][i] for g in range(4)]
                for h2 in range(2):
                    for kc in range(8):
                        P.mm(py[h2].ap[:], mixT.ap[:, kc, i * 128:(i + 1) * 128], woutb.ap[:, kc, h2 * 512:(h2 + 1) * 512],
                             kc == 0, kc == 7, mrd + [woutb], [py[h2]])
                for h2 in range(2):
                    P.act(tmpf.ap[:, h2 * 512:(h2 + 1) * 512], py[h2].ap[:], AF.Square, [py[h2]], [tmpf, stb],
                          accum_out=stb.ap[:, 4 + h2:5 + h2])
                P.tt('dve', stb.ap[:, 6:7], stb.ap[:, 4:5], stb.ap[:, 5:6], ALU.add, [stb], [stb])
                P.act(stb.ap[:, 7:8], stb.ap[:, 6:7], AF.Sqrt, [stb], [stb], bias=EPS, scale=1.0 / D)
                P.op('dve', lambda e, o=stb.ap[:, 3:4], a=stb.ap[:, 7:8]: e.reciprocal(o, a), [stb], [stb])
                for h2 in range(2):
                    sl = slice(h2 * 512, (h2 + 1) * 512)
                    P.stt(tmpf.ap[:, sl], py[h2].ap[:], stb.ap[:, 3:4], Gbc[s].ap[:, sl], ALU.mult, ALU.mult,
                          [py[h2], stb, Gbc[s]], [tmpf])
                P.tt('dve', xb_.ap[:], xb_.ap[:], tmpf.ap[:], ALU.add, [xb_, tmpf], [xb_])
                P.dma('sp', state_dst(i), xb_.ap[:], reads=[xb_], writes=[xs[i]], out=(l == n_layers - 1 and i >= 2))
            P.fence()
            esZ.close()
            P.es = es
        P.finish()
    return nc


BRANCH_BUILDERS = {}


def branch_att(L):
    P, nc, l, last = L["P"], L["nc"], L["l"], L["last"]
    hT, hTb, mixT, mixb, pT, pf = L["hT"], L["hTb"], L["mixT"], L["mixb"], L["pT"], L["pf"]
    ident_bf, ropec, ropes, amask = L["ident_bf"], L["ropec"], L["ropes"], L["amask"]
    load_w, proj_tm = L["load_w"], L["proj_tm"]
    with ExitStack() as es2:
        P.es = es2
        L["alloc_wbuf"]()
        QKT = P.sb("QKT", [128, 4, TOK], BF16)
        QKb = [P.vb("QKT_%d" % i) for i in range(NT)]
        v_tm = P.sb("v_tm", [128, NT, 128], BF16)
        sg_tm = P.sb("sg_tm", [128, NT, 256], BF16)
        vb_ = [P.vb("vsg_%d" % i) for i in range(NT)]
        qkf = P.sb("qkf", [128, 384], F32)
        rt = [P.sb("rt%d" % i, [128, 192], F32) for i in range(2)]
        rq = P.sb("rq", [128, 512], BF16)
        sink_bc = P.sb("sink_bc", [128, 4], F32)
        sm2 = [P.sb("sm%d" % r, [128, 640], F32) for r in range(2)]
        Pb2 = [P.sb("Pb%d" % r, [128, 640], BF16) for r in range(2)]
        PTs2 = [P.sb("PTs%d" % r, [128, 5, 128], BF16) for r in range(2)]
        stt2 = [P.sb("att_st%d" % r, [128, 8], F32) for r in range(2)]
        rden2 = [P.sb("rden%d" % r, [128, 4], F32) for r in range(2)]
        og2 = [P.sb("og%d" % r, [128, 256], BF16) for r in range(2)]
        P.dma('sp', sink_bc.ap[:], L["attn_sink"][l:l + 1, :].partition_broadcast(128), writes=[sink_bc])
        w1 = load_w(l, COL_D, 384)
        w2 = load_w(l, COL_D + 384, 384)
        for i in range(NT):
            pq, pv = pf[0], pf[1]
            proj_tm(i, w1, 0, 384, pq.ap[:, 0:384], pq)
            proj_tm(i, w2, 0, 384, pv.ap[:, 0:384], pv)
            P.cp('act', v_tm.ap[:, i, :], pv.ap[:, 0:128], [pv], [vb_[i]])
            P.act(sg_tm.ap[:, i, :], pv.ap[:, 128:384], AF.Silu, [pv], [vb_[i]])
            if i >= 2:
                P.cp('act', qkf.ap[:], pq.ap[:, 0:384], [pq], [qkf])
                q4 = qkf.ap[:, :].rearrange("p (h two j) -> p h two j", two=2, j=32)
                r4 = rq.ap[:, 0:384].rearrange("p (h two j) -> p h two j", two=2, j=32)
                x1, x2 = q4[:, :, 0, :], q4[:, :, 1, :]
                cosb = ropec.ap[:, i - 2, :].unsqueeze(1).to_broadcast([128, 6, 32])
                sinb = ropes.ap[:, i - 2, :].unsqueeze(1).to_broadcast([128, 6, 32])
                ta = rt[0].ap[:, :].rearrange("p (h j) -> p h j", j=32)
                tb = rt[1].ap[:, :].rearrange("p (h j) -> p h j", j=32)
                P.tt('dve', ta, x1, cosb, ALU.mult, [qkf, ropec], [rt[0]])
                P.tt('dve', tb, x2, sinb, ALU.mult, [qkf, ropes], [rt[1]])
                P.tt('dve', r4[:, :, 0, :], ta, tb, ALU.subtract, [rt[0], rt[1]], [rq])
                P.tt('dve', ta, x2, cosb, ALU.mult, [qkf, ropec], [rt[0]])
                P.tt('dve', tb, x1, sinb, ALU.mult, [qkf, ropes], [rt[1]])
                P.tt('dve', r4[:, :, 1, :], ta, tb, ALU.add, [rt[0], rt[1]], [rq])
            else:
                P.cp('act', rq.ap[:, 0:384], pq.ap[:, 0:384], [pq], [rq])
            kd_src = rq.ap[:, 256:384].rearrange("p (g o d) -> p g o d", o=1, d=64).to_broadcast([128, 2, 2, 64])
            P.cp('dve', rt[0].ap[:, 0:128].bitcast(BF16).rearrange("p (g o d) -> p g o d", o=2, d=64), kd_src,
                 [rq], [rt[0]])
            P.cp('dve', rq.ap[:, 256:512], rt[0].ap[:, 0:128].bitcast(BF16), [rt[0]], [rq])
            pt = pT[i % 2]
            for c4 in range(4):
                P.tr(pt.ap[:, c4 * 128:(c4 + 1) * 128], rq.ap[:, c4 * 128:(c4 + 1) * 128], ident_bf.ap[:],
                     [rq, ident_bf], [pt])
            P.cp('dve', QKT.ap[:, :, i * 128:(i + 1) * 128], pt.ap[:, 0:512].rearrange("p (c t) -> p c t", t=128),
                 [pt], [QKb[i]])

        def geom(i):
            has_local = i >= 2
            n = i - 2
            if has_local:
                nlo, nhi = max(n - 1, 0), min(n + 1, 15)
                c0 = (nlo - (n - 1)) * 128
                c1 = c0 + (nhi - nlo + 1) * 128
                ktiles = list(range(2 + nlo, 2 + nhi + 1))
            else:
                c0 = c1 = 384
                ktiles = []
            d0 = 384 - (c1 - c0)
            blocks = [(kt, d0 + 128 * bi) for bi, kt in enumerate(ktiles)] + [(0, 384), (1, 512)]
            return has_local, c0, c1, d0, ktiles, blocks

        def stage1(i, h):
            has_local, c0, c1, d0, ktiles, blocks = geom(i)
            rden = rden2[i % 2]
            g, base = h // 2, (h % 2) * 64
            r = h % 2
            sm, Pb, stt_ = sm2[r], Pb2[r], stt2[r]
            pl, pc = (pf[4], pf[5]) if r == 0 else (pf[0], pf[1])
            qa = QKT.ap[base:base + 64, g, i * 128:(i + 1) * 128]
            if has_local:
                P.mm(pl.ap[:, d0:384], qa, QKT.ap[base:base + 64, 2 + g, ktiles[0] * 128:(ktiles[-1] + 1) * 128],
                     True, True, [QKb[i]] + [QKb[k] for k in ktiles], [pl])
                P.tt('dve', sm.ap[:, d0:384], pl.ap[:, d0:384], amask.ap[:, c0:c1], ALU.add, [pl, amask], [sm])
            P.mm(pc.ap[:, 0:256], qa, QKT.ap[base:base + 64, 2 + g, 0:256], True, True, [QKb[i], QKb[0], QKb[1]], [pc])
            P.cp('act', sm.ap[:, 384:640], pc.ap[:, 0:256], [pc], [sm])
            P.op('dve', lambda e, o=stt_.ap[:, 0:1], a=sm.ap[:, d0:640]: e.reduce_max(o, a, AX.X), [sm], [stt_])
            P.ts('dve', stt_.ap[:, 1:2], stt_.ap[:, 0:1], -0.125, None, ALU.mult, ALU.bypass, [stt_], [stt_])
            P.act(Pb.ap[:, d0:640], sm.ap[:, d0:640], AF.Exp, [sm, stt_], [Pb, stt_], bias=stt_.ap[:, 1:2], scale=0.125,
                  accum_out=stt_.ap[:, 2:3])
            P.act(stt_.ap[:, 3:4], stt_.ap[:, 1:2], AF.Exp, [stt_, sink_bc], [stt_], bias=sink_bc.ap[:, h:h + 1], scale=1.0)
            P.tt('dve', stt_.ap[:, 4:5], stt_.ap[:, 2:3], stt_.ap[:, 3:4], ALU.add, [stt_], [stt_])
            P.op('dve', lambda e, o=rden.ap[:, h:h + 1], a=stt_.ap[:, 4:5]: e.reciprocal(o, a), [stt_], [rden])

        def stage2(i, h):
            has_local, c0, c1, d0, ktiles, blocks = geom(i)
            g = h // 2
            r = h % 2
            Pb, PTs = Pb2[r], PTs2[r]
            pO = pf[2 + (i % 2)]
            pt = pT[h % 2]
            for bi, (kt, cc) in enumerate(blocks):
                P.tr(pt.ap[:, bi * 128:(bi + 1) * 128], Pb.ap[:, cc:cc + 128], ident_bf.ap[:], [Pb, ident_bf], [pt])
            nb = len(blocks)
            P.cp('act', PTs.ap[:, 0:nb, :], pt.ap[:, 0:nb * 128].rearrange("p (c t) -> p c t", t=128), [pt], [PTs])
            for bi, (kt, cc) in enumerate(blocks):
                P.mm(pO.ap[:, h * 64:(h + 1) * 64], PTs.ap[:, bi, :], v_tm.ap[:, kt, g * 64:(g + 1) * 64],
                     bi == 0, bi == nb - 1, [PTs, vb_[kt]], [pO])

        def epilogue(i):
            pO = pf[2 + (i % 2)]
            rden, og = rden2[i % 2], og2[i % 2]
            for h in range(4):
                P.stt(og.ap[:, h * 64:(h + 1) * 64], pO.ap[:, h * 64:(h + 1) * 64], rden.ap[:, h:h + 1],
                      sg_tm.ap[:, i, h * 64:(h + 1) * 64], ALU.mult, ALU.mult, [pO, rden, vb_[i]], [og])
            pt = pT[i % 2]
            for c2 in range(2):
                P.tr(pt.ap[:, c2 * 128:(c2 + 1) * 128], og.ap[:, c2 * 128:(c2 + 1) * 128], ident_bf.ap[:], [og, ident_bf], [pt])
            P.cp('act', mixT.ap[:, 6:8, i * 128:(i + 1) * 128], pt.ap[:, 0:256].rearrange("p (c t) -> p c t", t=128),
                 [pt], [mixb[3][i]])

        items = [(i, h) for i in range(NT) if not (last and i < 2) for h in range(4)]
        stage1(*items[0])
        for n_, (i, h) in enumerate(items):
            if n_ + 1 < len(items):
                stage1(*items[n_ + 1])
            stage2(i, h)
            if h == 3:
                epilogue(i)
        P.fence()
    P.es = L["es"]


BRANCH_BUILDERS["att"] = branch_att


def make_in_maps(inputs):
    c = host_consts()
    shared = {}
    for k in ["w_ada", "b_ada", "g_pre", "g_post", "w_in", "w_out", "mlstm_norm_g", "gla_norm_g", "gla_w_alpha",
              "gla_b_alpha", "attn_sink"]:
        shared[k] = np.ascontiguousarray(np.asarray(inputs[k], dtype=np.float32))
    for k in ["hyena_conv_w", "hyena_conv_b", "hyena_w1", "hyena_b1", "hyena_w2", "hyena_b2", "hyena_w3", "hyena_b3",
              "hyena_freq", "hyena_d"]:
        shared[k] = np.ascontiguousarray(np.asarray(inputs[k], dtype=np.float32))
    shared["mlstm_gate_b"] = np.ascontiguousarray(np.asarray(inputs["mlstm_gate_b"], dtype=np.float32).reshape(4, 16))
    shared.update(c)
    maps = []
    c_ctx = np.asarray(inputs["c_ctx"], dtype=np.float32)
    for b in range(8):
        m = dict(shared)
        m["x"] = np.ascontiguousarray(np.asarray(inputs["x"][b], dtype=np.float32))
        m["ctx"] = np.ascontiguousarray(np.asarray(inputs["ctx"][b], dtype=np.float32))
        m["cc"] = np.ascontiguousarray(np.concatenate([np.asarray(inputs["c"][b], dtype=np.float32), c_ctx]).reshape(16, 128))
        maps.append(m)
    return maps


def branch_scan(L, kind):
    ml = kind == "mlstm"
    P, nc, l, last = L["P"], L["nc"], L["l"], L["last"]
    hT, hTb, mixT, mixb, pT, pf = L["hT"], L["hTb"], L["mixT"], L["mixb"], L["pT"], L["pf"]
    ident_bf, ident_f = L["ident_bf"], L["ident_f"]
    load_w, proj_tm, proj_fm = L["load_w"], L["proj_tm"], L["proj_fm"]
    base = COL_A if ml else COL_G
    cdec = 1.0 if ml else 1.0 / 16
    mc0, mg = (0, 0) if ml else (4, 2)
    qscale, kscale = (1.0, 0.125) if ml else (0.125, 1.0)
    with ExitStack() as es2:
        P.es = es2
        L["alloc_wbuf"]()
        qT = P.sb("s_qT", [128, 2, TOK], BF16)
        kT = P.sb("s_kT", [128, 2, TOK], BF16)
        k_tm = P.sb("s_ktm", [128, NT, 256], BF16)
        v_aug = P.sb("s_vaug", [128, NT, 512], BF16)
        Hh = P.sb("s_H", [128, NT, 256], F32)
        gr = P.sb("s_gr", [128, NT, 32], F32)
        qkb = [P.vb("s_qkb%d" % i) for i in range(NT)]
        kvb = [P.vb("s_kvb%d" % i) for i in range(NT)]
        Hb = [P.vb("s_Hb%d" % i) for i in range(NT)]
        grb = [P.vb("s_grb%d" % i) for i in range(NT)]
        normg = P.sb("s_normg", [128, 256], F32)
        P.dma('sp', normg.ap[:], (L["mlstm_norm_g"] if ml else L["gla_norm_g"])[l:l + 1, :].partition_broadcast(128),
              writes=[normg])
        if ml:
            gbb = P.sb("s_gbb", [128, 16], F32)
            P.dma('sp', gbb.ap[:], L["mlstm_gate_b"][l:l + 1, :].partition_broadcast(128), writes=[gbb])
            posi = P.sb("s_posi", [128, 256], BF16)
            negi = P.sb("s_negi", [128, 256], BF16)
            e4 = P.sb("s_e4", [128, 8], F32)
        else:
            wal = P.sb("s_wal", [17, 2, 256], F32)
            P.dma('sp', wal.ap[0:16, :, :], L["gla_w_alpha"][l].rearrange("d r c -> r d c"), writes=[wal])
            P.dma('sp', wal.ap[16:17, :, :], L["gla_b_alpha"][l:l + 1, :, :], writes=[wal])
            rTa = P.sb("s_rTa", [17, 128], F32)
            P.op('pool', lambda e: e.memset(rTa.ap[:], 1.0), writes=[rTa])
            e_tm = P.sb("s_etm", [128, 256], F32)
        sp_tm = P.sb("s_sp", [128, 256], BF16)
        EqT2 = [P.sb("s_EqT%d" % r, [128, 256], F32) for r in range(2)]
        EkT = P.sb("s_EkT", [128, 256], F32)
        Ex = P.sb("s_Ex", [128, 256], F32)
        qp2 = [P.sb("s_qp%d" % r, [128, 2, 128], BF16) for r in range(2)]
        kpz = [P.sb("s_kpz%d" % h, [128, 128], BF16) for h in range(4)]
        for h in range(4):
            P.op('pool', lambda e, a=kpz[h].ap[:]: e.memset(a, 0.0), writes=[kpz[h]])
        kpp2 = [P.sb("s_kpp%d" % r, [128, 256], BF16) for r in range(2)]
        S_m2 = [P.sb("s_Sm%d" % r, [128, 512], BF16) for r in range(2)]
        St_f = [P.sb("s_Stf%d" % j, [128, 128], F32) for j in range(2)]
        St_b = [P.sb("s_Stb%d" % h, [128, 128], BF16) for h in range(4)]
        dn = P.sb("s_dn", [128, 8], F32)
        tmpH = P.sb("s_tmpH", [128, 256], F32)

        wqk = load_w(l, base, 512)
        groups = [(0, 512), (512, 512), (1024, 512), (1536, 512), (2048, 256)]
        cnt = 0
        for j in range(2):
            for (t0, nt) in groups:
                tiles = list(range(t0 // 128, (t0 + nt) // 128))
                for which, dst, sc_ in ((0, qT, qscale), (1, kT, kscale)):
                    pq = pf[cnt % 2]
                    cnt += 1
                    proj_fm(wqk, which * 256 + j * 128, 128, t0, nt, pq.ap[:, 0:nt], pq)
                    P.act(dst.ap[:, j, t0:t0 + nt], pq.ap[:, 0:nt], AF.Identity, [pq], [qkb[t] for t in tiles], scale=sc_)
        wkv = load_w(l, base + 256, 512)
        ng = 16 if ml else 32
        wg = load_w(l, base + (1024 if ml else 768), 256 + ng)
        P.op('pool', lambda e: e.memset(v_aug.ap[:], 1.0), writes=kvb)
        for i in range(NT):
            pk = pf[2 + i % 2]
            proj_tm(i, wkv, 0, 512, pk.ap[:], pk)
            P.act(k_tm.ap[:, i, :], pk.ap[:, 0:256], AF.Identity, [pk], [kvb[i]], scale=kscale)
            P.cp('dve', v_aug.ap[:, i, :].rearrange("p (h e) -> p h e", e=128)[:, :, 0:64],
                 pk.ap[:, 256:512].rearrange("p (h e) -> p h e", e=64), [pk], [kvb[i]])
            pg = pf[4 + i % 2]
            proj_tm(i, wg, 256, 64, pg.ap[:, 0:64], pg)
            if ml:
                P.tt('dve', gr.ap[:, i, 0:16], pg.ap[:, 0:16], gbb.ap[:], ALU.add, [pg, gbb], [grb[i]])
            else:
                P.cp('dve', gr.ap[:, i, 0:32], pg.ap[:, 0:32], [pg], [grb[i]])

        P.ckpt("pre")
        for dirn in range(2):
            order = ([0, 1] + list(range(2, NT))) if dirn == 0 else ([1, 0] + list(range(NT - 1, 1, -1)))
            tri_c = L["tri_le"] if dirn == 0 else L["tri_ge"]
            tri_x = L["tri_gt"] if dirn == 0 else L["tri_lt"]
            tri_cb = L["tri_le_b"] if dirn == 0 else L["tri_ge_b"]
            tri_xb = L["tri_gt_b"] if dirn == 0 else L["tri_lt_b"]
            dcol = 127 if dirn == 0 else 0
            for j in range(2):
                P.op('pool', lambda e, a=St_f[j].ap[:]: e.memset(a, 0.0), writes=[St_f[j]])
            for h in range(4):
                P.op('pool', lambda e, a=St_b[h].ap[:]: e.memset(a, 0.0), writes=[St_b[h]])
            def prefix(step, i):
                tsl = slice(i * 128, (i + 1) * 128)
                EqT, qp, kpp, S_m = EqT2[step % 2], qp2[step % 2], kpp2[step % 2], S_m2[step % 2]
                if ml:
                    ic, fc = dirn * 8, dirn * 8 + 4
                    P.act(e4.ap[:, 0:4], gr.ap[:, i, fc:fc + 4], AF.Exp, [grb[i]], [e4], scale=-1.0)
                    P.act(e4.ap[:, 4:8], e4.ap[:, 0:4], AF.Ln, [e4], [e4], bias=1.0)
                    P.cp('dve', sp_tm.ap[:, :].rearrange("p (h d) -> p h d", d=64),
                         e4.ap[:, 4:8].unsqueeze(2).to_broadcast([128, 4, 64]), [e4], [sp_tm])
                    ib = gr.ap[:, i, ic:ic + 4].unsqueeze(2).to_broadcast([128, 4, 64])
                    P.cp('dve', posi.ap[:, :].rearrange("p (h d) -> p h d", d=64), ib, [grb[i]], [posi])
                    P.ts('dve', negi.ap[:, :].rearrange("p (h d) -> p h d", d=64), ib, -1.0, None, ALU.mult, ALU.bypass,
                         [grb[i]], [negi])
                else:
                    P.tr(pf[3].ap[0:16, 256:384], gr.ap[:, i, dirn * 16:(dirn + 1) * 16], ident_f.ap[:],
                         [grb[i], ident_f], [pf[3]])
                    P.cp('act', rTa.ap[0:16, :], pf[3].ap[0:16, 256:384], [pf[3]], [rTa])
                    P.mm(pf[3].ap[:, 256:512], rTa.ap[0:17, :], wal.ap[0:17, dirn, :], True, True, [rTa, wal], [pf[3]])
                    P.act(e_tm.ap[:], pf[3].ap[:, 256:512], AF.Exp, [pf[3]], [e_tm], scale=-1.0)
                    P.act(sp_tm.ap[:], e_tm.ap[:], AF.Ln, [e_tm], [sp_tm], bias=1.0)
                P.ckpt("s1")
                pc, px = pf[4], pf[3]
                for j in range(2):
                    P.mm(pc.ap[:, j * 128:(j + 1) * 128], sp_tm.ap[:, j * 128:(j + 1) * 128], tri_cb.ap[:], True, True,
                         [sp_tm, tri_cb], [pc])
                if ml:
                    for j in range(2):
                        P.mm(pc.ap[:, 256 + j * 128:384 + j * 128], sp_tm.ap[:, j * 128:(j + 1) * 128], tri_cb.ap[:],
                             True, False, [sp_tm, tri_cb], [pc])
                        P.mm(pc.ap[:, 256 + j * 128:384 + j * 128], posi.ap[:, j * 128:(j + 1) * 128], ident_bf.ap[:],
                             False, True, [posi, ident_bf], [pc])
                P.mm(px.ap[:, 0:256], tri_xb.ap[:], sp_tm.ap[:], True, not ml, [tri_xb, sp_tm], [px])
                if ml:
                    P.mm(px.ap[:, 0:256], ident_bf.ap[:], negi.ap[:], False, True, [ident_bf, negi], [px])
                P.ckpt("s2")
                P.act(EqT.ap[:], pc.ap[:, 0:256], AF.Exp, [pc], [EqT], scale=-cdec)
                P.act(EkT.ap[:], pc.ap[:, 256:512] if ml else pc.ap[:, 0:256], AF.Exp, [pc], [EkT], scale=cdec)
                P.act(Ex.ap[:], px.ap[:, 0:256], AF.Exp, [px], [Ex], scale=-cdec)
                P.tt('dve', qp.ap[:], qT.ap[:, :, tsl], EqT.ap[:, :].rearrange("p (j t) -> p j t", t=128), ALU.mult,
                     [qkb[i], EqT], [qp])
                for h in range(4):
                    j, b0 = h // 2, (h % 2) * 64
                    P.tt('dve', kpz[h].ap[b0:b0 + 64, :], kT.ap[b0:b0 + 64, j, tsl], EkT.ap[b0:b0 + 64, j * 128:(j + 1) * 128],
                         ALU.mult, [qkb[i], EkT], [kpz[h]])
                P.tt('dve', kpp.ap[:], k_tm.ap[:, i, :], Ex.ap[:], ALU.mult, [kvb[i], Ex], [kpp])
                P.ckpt("s3")
                pS = pf[2]
                for h in range(4):
                    j, b0 = h // 2, (h % 2) * 64
                    P.mm(pS.ap[:, h * 128:(h + 1) * 128], kpz[h].ap[:], qp.ap[:, j, :], True, True, [kpz[h], qp], [pS])
                P.tt('dve', S_m.ap[:, :].rearrange("p (h t) -> p h t", t=128),
                     pS.ap[:, :].rearrange("p (h t) -> p h t", t=128),
                     tri_c.ap[:, :].unsqueeze(1).to_broadcast([128, 4, 128]), ALU.mult, [pS, tri_c], [S_m])

            def suffix(step, i):
                EqT, qp, kpp, S_m = EqT2[step % 2], qp2[step % 2], kpp2[step % 2], S_m2[step % 2]
                pO = pf[step % 2]
                for h in range(4):
                    j, b0 = h // 2, (h % 2) * 64
                    P.mm(pO.ap[:, h * 128:(h + 1) * 128], S_m.ap[:, h * 128:(h + 1) * 128], v_aug.ap[:, i, h * 128:(h + 1) * 128],
                         True, False, [S_m, kvb[i]], [pO])
                    P.mm(pO.ap[:, h * 128:(h + 1) * 128], qp.ap[:, j, :], St_b[h].ap[:], False, True, [qp, St_b[h]], [pO])
                P.ckpt("s5")
                pD = pf[5]
                for j in range(2):
                    P.mm(pD.ap[:, j * 256:(j + 1) * 256], kpp.ap[:, j * 128:(j + 1) * 128], v_aug.ap[:, i, j * 256:(j + 1) * 256],
                         True, True, [kpp, kvb[i]], [pD])
                for h in range(4):
                    j, b0 = h // 2, (h % 2) * 64
                    P.stt(St_f[j].ap[b0:b0 + 64, :], St_f[j].ap[b0:b0 + 64, :], EqT.ap[b0:b0 + 64, j * 128 + dcol:j * 128 + dcol + 1],
                          pD.ap[b0:b0 + 64, j * 256 + (h % 2) * 128:j * 256 + (h % 2) * 128 + 128], ALU.mult, ALU.add,
                          [St_f[j], EqT, pD], [St_f[j]])
                for h in range(4):
                    j, b0 = h // 2, (h % 2) * 64
                    P.cp('act', St_b[h].ap[b0:b0 + 64, :], St_f[j].ap[b0:b0 + 64, :], [St_f[j]], [St_b[h]])
                P.ckpt("s6")
                pO3 = pO.ap[:, 0:512].rearrange("p (h e) -> p h e", e=128)
                H3 = Hh.ap[:, i, :].rearrange("p (h d) -> p h d", d=64)
                if ml:
                    P.act(dn.ap[:, 0:4], pO3[:, :, 64], AF.Abs, [pO], [dn])
                    P.ts('dve', dn.ap[:, 0:4], dn.ap[:, 0:4], 1.0, None, ALU.max, ALU.bypass, [dn], [dn])
                    P.op('dve', lambda e, o=dn.ap[:, 4:8], a=dn.ap[:, 0:4]: e.reciprocal(o, a), [dn], [dn])
                    rb = dn.ap[:, 4:8].unsqueeze(2).to_broadcast([128, 4, 64])
                    if dirn == 0:
                        P.tt('dve', H3, pO3[:, :, 0:64], rb, ALU.mult, [pO, dn], [Hb[i]])
                    else:
                        P.tt('dve', tmpH.ap[:, :].rearrange("p (h d) -> p h d", d=64), pO3[:, :, 0:64], rb, ALU.mult,
                             [pO, dn], [tmpH])
                        P.tt('dve', Hh.ap[:, i, :], Hh.ap[:, i, :], tmpH.ap[:], ALU.add, [Hb[i], tmpH], [Hb[i]])
                else:
                    if dirn == 0:
                        P.cp('act', H3, pO3[:, :, 0:64], [pO], [Hb[i]])
                    else:
                        P.tt('dve', H3, pO3[:, :, 0:64], H3, ALU.add, [pO, Hb[i]], [Hb[i]])

            prefix(0, order[0])
            for step, i in enumerate(order):
                if step + 1 < len(order):
                    prefix(step + 1, order[step + 1])
                suffix(step, i)
        P.ckpt("scan")
        ncol = 512 if ml else 256
        wfin = load_w(l, base + 768, ncol)
        sig = P.sb("s_sig", [128, 256], F32)
        sgt = P.sb("s_sgt", [128, 256], F32)
        gs = P.sb("s_gs", [128, 256], F32)
        hh = P.sb("s_hh", [128, 256], F32)
        sq = P.sb("s_sq", [128, 256], F32)
        og = P.sb("s_og", [128, 256], BF16)
        for i in range(NT):
            if last and i < 2:
                continue
            pg = pf[i % 2]
            proj_tm(i, wfin, 0, ncol, pg.ap[:, 0:ncol], pg)
            if ml:
                P.act(sig.ap[:], pg.ap[:, 0:256], AF.Sigmoid, [pg], [sig])
                P.tt('dve', hh.ap[:], Hh.ap[:, i, :], sig.ap[:], ALU.mult, [Hb[i], sig], [hh])
                gate_ap = pg.ap[:, 256:512]
            else:
                P.cp('dve', hh.ap[:], Hh.ap[:, i, :], [Hb[i]], [hh])
                gate_ap = pg.ap[:, 0:256]
            P.act(sgt.ap[:], gate_ap, AF.Silu, [pg], [sgt])
            P.tt('dve', gs.ap[:], sgt.ap[:], normg.ap[:], ALU.mult, [sgt, normg], [gs])
            P.tt('dve', sq.ap[:], hh.ap[:], hh.ap[:], ALU.mult, [hh], [sq])
            P.op('dve', lambda e, o=dn.ap[:, 0:4], a=sq.ap[:, :].rearrange("p (h d) -> p h d", d=64): e.reduce_sum(o, a, AX.X),
                 [sq], [dn])
            P.act(dn.ap[:, 4:8], dn.ap[:, 0:4], AF.Sqrt, [dn], [dn], bias=EPS, scale=1.0 / 64)
            P.op('dve', lambda e, o=dn.ap[:, 0:4], a=dn.ap[:, 4:8]: e.reciprocal(o, a), [dn], [dn])
            P.tt('dve', sq.ap[:, :].rearrange("p (h d) -> p h d", d=64), hh.ap[:, :].rearrange("p (h d) -> p h d", d=64),
                 dn.ap[:, 0:4].unsqueeze(2).to_broadcast([128, 4, 64]), ALU.mult, [hh, dn], [sq])
            P.tt('dve', og.ap[:], sq.ap[:], gs.ap[:], ALU.mult, [sq, gs], [og])
            pt = pT[i % 2]
            for c2 in range(2):
                P.tr(pt.ap[:, c2 * 128:(c2 + 1) * 128], og.ap[:, c2 * 128:(c2 + 1) * 128], ident_bf.ap[:], [og, ident_bf], [pt])
            P.cp('act', mixT.ap[:, mc0:mc0 + 2, i * 128:(i + 1) * 128], pt.ap[:, 0:256].rearrange("p (c t) -> p c t", t=128),
                 [pt], [mixb[mg][i]])
        P.fence()
    P.es = L["es"]


BRANCH_BUILDERS["mlstm"] = lambda L: branch_scan(L, "mlstm")
BRANCH_BUILDERS["gla"] = lambda L: branch_scan(L, "gla")


def hcol(i):
    return 2 + i * 128 if i < 2 else 262 + (i - 2) * 128


def branch_hyena(L):
    P, nc, l, last, hy = L["P"], L["nc"], L["l"], L["last"], L["hy"]
    hT, hTb, mixT, mixb, pT, pf = L["hT"], L["hTb"], L["mixT"], L["mixb"], L["pT"], L["pf"]
    ident_bf, ident_f = L["ident_bf"], L["ident_f"]
    load_w, proj_fm = L["load_w"], L["proj_fm"]
    PI = 3.1415925
    TWO_PI = 6.283185307179586
    W = 2312
    with ExitStack() as es2:
        P.es = es2
        RC = P.sb("h_RC", [128, 16, 512], BF16)
        RS = P.sb("h_RS", [128, 16, 512], BF16)
        RCc = P.sb("h_RCc", [128, 2, 512], BF16)
        RSc = P.sb("h_RSc", [128, 2, 512], BF16)
        uT = P.sb("h_uT", [128, 2, W], BF16)
        x0c = P.sb("h_x0c", [128, 2, W], BF16)
        sgT = P.sb("h_sgT", [128, 2, W], BF16)
        vcol = P.sb("h_vcol", [128, 26], F32)
        fb = P.sb("h_fb", [64, 6], F32)
        wfl = P.sb("h_wfl", [128, 16], F32)
        wfc = P.sb("h_wfc", [128, 2], F32)
        ntl = P.sb("h_ntl", [128, 16], F32)
        ntc = P.sb("h_ntc", [128, 2], F32)
        delt = P.sb("h_delt", [128, 256], F32)
        P.dma('sp', wfl.ap[:], hy["wf_l"], writes=[wfl])
        P.dma('sp', wfc.ap[:], hy["wf_c"], writes=[wfc])
        P.dma('sp', ntl.ap[:], hy["ntcol_l"], writes=[ntl])
        P.dma('sp', ntc.ap[:], hy["ntcol_c"], writes=[ntc])
        P.dma('sp', delt.ap[:], hy["deltas"].partition_broadcast(128), writes=[delt])
        do_ctx = not last

        with ExitStack() as es3:
            P.es = es3
            vr = P.sb("h_vr", [26, 128], F32)
            fr = P.sb("h_fr", [4, 64], F32)
            P.dma('sp', vr.ap[0:18, :], hy["hyena_conv_w"][l].rearrange("j (c p) -> (j c) p", p=128), writes=[vr])
            P.dma('sp', vr.ap[18:24, :], hy["hyena_conv_b"][l].rearrange("(c p) -> c p", p=128), writes=[vr])
            P.dma('sp', vr.ap[24:26, :], hy["hyena_d"][l].rearrange("(c p) -> c p", p=128), writes=[vr])
            P.dma('sp', fr.ap[0:1, :], hy["hyena_b1"][l:l + 1, :], writes=[fr])
            P.dma('sp', fr.ap[1:2, :], hy["hyena_b2"][l:l + 1, :], writes=[fr])
            P.dma('sp', fr.ap[2:4, :], hy["hyena_freq"][l], writes=[fr])
            P.tr(pf[0].ap[:, 0:26], vr.ap[:], ident_f.ap[0:26, 0:26], [vr, ident_f], [pf[0]])
            P.cp('dve', vcol.ap[:], pf[0].ap[:, 0:26], [pf[0]], [vcol])
            P.tr(pf[1].ap[0:64, 0:4], fr.ap[:], ident_f.ap[0:4, 0:4], [fr, ident_f], [pf[1]])
            P.cp('dve', fb.ap[:, 0:4], pf[1].ap[0:64, 0:4], [pf[1]], [fb])
            P.tt('dve', fb.ap[:, 4:6], fb.ap[:, 0:2], fb.ap[:, 2:4], ALU.mult, [fb], [fb])

            P.ckpt("hv")
            zTl = P.sb("h_zTl", [33, 2048], F32)
            zTc = P.sb("h_zTc", [33, 256], F32)
            w1s = P.sb("h_w1", [33, 64], F32)
            w2s = P.sb("h_w2", [64, 64], F32)
            w3a = P.sb("h_w3a", [65, 512], F32)
            arg = P.sb("h_arg", [64, 512], F32)
            t1 = P.sb("h_t1", [64, 512], F32)
            t2 = P.sb("h_t2", [64, 512], F32)
            h1 = P.sb("h_h1", [64, 512], F32)
            h2 = P.sb("h_h2", [65, 512], F32)
            win = P.sb("h_win", [128, 256], F32)
            hf = P.sb("h_hf", [128, 256], F32)
            hb = P.sb("h_hb", [128, 256], F32)
            P.dma('sp', zTl.ap[:], hy["zT_l"], writes=[zTl])
            P.dma('sp', zTc.ap[:], hy["zT_c"], writes=[zTc])
            P.dma('sp', w1s.ap[:], hy["hyena_w1"][l], writes=[w1s])
            P.dma('sp', w2s.ap[:], hy["hyena_w2"][l], writes=[w2s])
            P.dma('sp', w3a.ap[0:64, :], hy["hyena_w3"][l], writes=[w3a])
            P.dma('sp', w3a.ap[64:65, :], hy["hyena_b3"][l:l + 1, :], writes=[w3a])
            P.op('pool', lambda e: e.memset(h2.ap[:], 1.0), writes=[h2])

            def sin_layer(ps_buf, ps_ap, fc_, fbc_, out_ap, outbuf, nn):
                P.act(arg.ap[:, 0:nn], ps_ap, AF.Identity, [ps_buf, fb], [arg], scale=fb.ap[:, fc_:fc_ + 1],
                      bias=fb.ap[:, fbc_:fbc_ + 1])
                P.ts('dve', t1.ap[:, 0:nn], arg.ap[:, 0:nn], PI, TWO_PI, ALU.is_gt, ALU.mult, [arg], [t1])
                P.ts('dve', t2.ap[:, 0:nn], arg.ap[:, 0:nn], -PI, TWO_PI, ALU.is_lt, ALU.mult, [arg], [t2])
                P.tt('dve', arg.ap[:, 0:nn], arg.ap[:, 0:nn], t1.ap[:, 0:nn], ALU.subtract, [arg, t1], [arg])
                P.tt('dve', arg.ap[:, 0:nn], arg.ap[:, 0:nn], t2.ap[:, 0:nn], ALU.add, [arg, t2], [arg])
                P.act(out_ap, arg.ap[:, 0:nn], AF.Sin, [arg], [outbuf])

            def mlp_block(zsrc, n0, nn, ntc_, RCd, RSd, tile0):
                P.mm(pf[0].ap[0:64, 0:nn], w1s.ap[:], zsrc.ap[:, n0:n0 + nn], True, True, [w1s, zsrc], [pf[0]])
                sin_layer(pf[0], pf[0].ap[0:64, 0:nn], 2, 4, h1.ap[:, 0:nn], h1, nn)
                P.mm(pf[1].ap[0:64, 0:nn], w2s.ap[:], h1.ap[:, 0:nn], True, True, [w2s, h1], [pf[1]])
                sin_layer(pf[1], pf[1].ap[0:64, 0:nn], 3, 5, h2.ap[0:64, 0:nn], h2, nn)
                for tt_ in range(nn // 128):
                    dt = tile0 + tt_
                    pp = pf[2 + tt_ % 2]
                    P.mm(pp.ap[:, 0:512], h2.ap[0:65, tt_ * 128:(tt_ + 1) * 128], w3a.ap[:], True, True, [h2, w3a], [pp])
                    P.act(win.ap[:], delt.ap[:], AF.Exp, [delt, ntc_], [win], scale=ntc_.ap[:, dt:dt + 1])
                    P.tt('dve', hf.ap[:], pp.ap[:, 0:256], win.ap[:], ALU.mult, [pp, win], [hf])
                    P.tt('dve', hb.ap[:], pp.ap[:, 256:512], win.ap[:], ALU.mult, [pp, win], [hb])
                    if dt == 0:
                        P.op('pool', lambda e: e.memset(hb.ap[0:1, :], 0.0), reads=[hb], writes=[hb])
                    P.tt('dve', RCd.ap[:, dt, 256:512], hf.ap[:], hb.ap[:], ALU.add, [hf, hb], [RCd])
                    P.tt('dve', RSd.ap[:, dt, 256:512], hf.ap[:], hb.ap[:], ALU.subtract, [hf, hb], [RSd])

            for blk in range(4):
                mlp_block(zTl, blk * 512, 512, ntl, RC, RS, blk * 4)
                P.ckpt("hm%d" % blk)
            if do_ctx:
                mlp_block(zTc, 0, 256, ntc, RCc, RSc, 0)
            P.fence()
        P.es = es2

        with ExitStack() as es3:
            P.es = es3
            L["alloc_wbuf"]()
            raw = P.sb("h_raw", [128, W], F32)
            cv = P.sb("h_cv", [128, W], F32)
            P.op('pool', lambda e: e.memset(raw.ap[:], 0.0), writes=[raw])
            wA = load_w(l, COL_H, 512)
            wB = load_w(l, COL_H + 512, 512)
            groups = [(0, 256), (256, 512), (768, 512), (1280, 512), (1792, 512)]
            cnt = 0
            for c6 in range(8):
                wsel = wA if c6 < 4 else wB
                coff = (c6 % 4) * 128
                for (t0, nt) in groups:
                    pp = pf[cnt % 2]
                    cnt += 1
                    proj_fm(wsel, coff, 128, t0, nt, pp.ap[:, 0:nt], pp)
                    c0 = 2 + t0 if t0 < 256 else t0 + 6
                    if c6 < 6:
                        P.cp('act', raw.ap[:, c0:c0 + nt], pp.ap[:, 0:nt], [pp], [raw])
                    else:
                        P.act(sgT.ap[:, c6 - 6, c0:c0 + nt], pp.ap[:, 0:nt], AF.Silu, [pp], [sgT])
                if c6 >= 6:
                    continue
                w0c, w1c, w2c = (vcol.ap[:, j * 6 + c6:j * 6 + c6 + 1] for j in range(3))
                bc = vcol.ap[:, 18 + c6:19 + c6]
                P.ts('dve', cv.ap[:, 1:W - 1], raw.ap[:, 1:W - 1], w1c, bc, ALU.mult, ALU.add, [raw, vcol], [cv])
                P.stt(cv.ap[:, 1:W - 1], raw.ap[:, 0:W - 2], w0c, cv.ap[:, 1:W - 1], ALU.mult, ALU.add, [raw, vcol, cv], [cv])
                if c6 < 2:
                    P.stt(uT.ap[:, c6, 1:W - 1], raw.ap[:, 2:W], w2c, cv.ap[:, 1:W - 1], ALU.mult, ALU.add, [raw, vcol, cv], [uT])
                elif c6 < 4:
                    P.stt(cv.ap[:, 1:W - 1], raw.ap[:, 2:W], w2c, cv.ap[:, 1:W - 1], ALU.mult, ALU.add, [raw, vcol, cv], [cv])
                    P.tt('dve', uT.ap[:, c6 - 2, 1:W - 1], uT.ap[:, c6 - 2, 1:W - 1], cv.ap[:, 1:W - 1], ALU.mult, [uT, cv], [uT])
                else:
                    P.stt(x0c.ap[:, c6 - 4, 1:W - 1], raw.ap[:, 2:W], w2c, cv.ap[:, 1:W - 1], ALU.mult, ALU.add,
                          [raw, vcol, cv], [x0c])
            P.fence()
        P.es = es2

        P.ckpt("hp")
        for i in range(NT):
            if i < 2 and not do_ctx:
                continue
            c = hcol(i)
            pt = pT[i % 2]
            for cj in range(2):
                P.tr(pt.ap[:, cj * 128:(cj + 1) * 128], uT.ap[:, cj, c:c + 128], ident_bf.ap[:], [uT, ident_bf], [pt])
            dC, dS, dt = (RCc, RSc, i) if i < 2 else (RC, RS, i - 2)
            P.cp('act', dC.ap[:, dt, 0:256], pt.ap[:, 0:256], [pt], [dC])
            P.cp('dve', dS.ap[:, dt, 0:256], pt.ap[:, 0:256], [pt], [dS])

        P.ckpt("ht")
        Ysp = P.sb("h_Y", [128, 16, 512], BF16)
        Yc = P.sb("h_Yc", [128, 2, 512], BF16)
        kre = P.sb("h_kre", [128, 256], F32)
        kim = P.sb("h_kim", [128, 256], F32)
        ta = [P.sb("h_ta%d" % i, [128, 256], F32) for i in range(2)] * 2
        fin = P.sb("h_fin", [128, 512], F32)
        cbuf = [P.sb("h_cb%d" % i, [128, 16, 128], BF16) for i in range(2)]
        sbuf_ = [P.sb("h_sb%d" % i, [128, 16, 128], BF16) for i in range(2)]
        ibc = [P.sb("h_ibc%d" % i, [128, 512], BF16) for i in range(2)]
        ibs = [P.sb("h_ibs%d" % i, [128, 512], BF16) for i in range(2)]
        rot = [0]

        def dft_conv(RCx, RSx, Yx, nT, tag, wfx, tgroups, col_base, tok_base):
            Ct, St, Cn, Sn = hy["Ct_" + tag], hy["St_" + tag], hy["Cn_" + tag], hy["Sn_" + tag]
            for fc in range(nT):
                b = fc % 2
                P.dma('sp', cbuf[b].ap[:, 0:nT, :], Ct[fc], writes=[cbuf[b]])
                P.dma('sp', sbuf_[b].ap[:, 0:nT, :], St[fc], writes=[sbuf_[b]])
                pc, ps_ = pf[2 * b], pf[2 * b + 1]
                for tc in range(nT):
                    P.mm(pc.ap[:, 0:512], cbuf[b].ap[:, tc, :], RCx.ap[:, tc, :], tc == 0, tc == nT - 1, [cbuf[b], RCx], [pc])
                for tc in range(nT):
                    P.mm(ps_.ap[:, 0:512], sbuf_[b].ap[:, tc, :], RSx.ap[:, tc, :], tc == 0, tc == nT - 1, [sbuf_[b], RSx], [ps_])
                P.act(kre.ap[:], pc.ap[:, 256:512], AF.Identity, [pc, wfx], [kre], scale=wfx.ap[:, fc:fc + 1])
                P.act(kim.ap[:], ps_.ap[:, 256:512], AF.Identity, [ps_, wfx], [kim], scale=wfx.ap[:, fc:fc + 1])
                P.tt('dve', ta[0].ap[:], pc.ap[:, 0:256], kre.ap[:], ALU.mult, [pc, kre], [ta[0]])
                P.tt('dve', ta[1].ap[:], ps_.ap[:, 0:256], kim.ap[:], ALU.mult, [ps_, kim], [ta[1]])
                P.tt('dve', Yx.ap[:, fc, 0:256], ta[0].ap[:], ta[1].ap[:], ALU.subtract, [ta[0], ta[1]], [Yx])
                P.tt('dve', ta[2].ap[:], pc.ap[:, 0:256], kim.ap[:], ALU.mult, [pc, kim], [ta[2]])
                P.tt('dve', ta[3].ap[:], ps_.ap[:, 0:256], kre.ap[:], ALU.mult, [ps_, kre], [ta[3]])
                P.tt('dve', Yx.ap[:, fc, 256:512], ta[2].ap[:], ta[3].ap[:], ALU.add, [ta[2], ta[3]], [Yx])
            P.ckpt("hf" + tag)
            for gi, (t0, nt) in enumerate(tgroups):
                py = [pf[4], pf[5]]
                for fc in range(nT):
                    bb = rot[0] % 2
                    rot[0] += 1
                    P.dma('sp', ibc[bb].ap[:, 0:nt], Cn[fc * 128:(fc + 1) * 128, t0:t0 + nt], writes=[ibc[bb]])
                    P.dma('sp', ibs[bb].ap[:, 0:nt], Sn[fc * 128:(fc + 1) * 128, t0:t0 + nt], writes=[ibs[bb]])
                    for cj in range(2):
                        P.mm(py[cj].ap[:, 0:nt], Yx.ap[:, fc, cj * 128:(cj + 1) * 128], ibc[bb].ap[:, 0:nt], fc == 0, False,
                             [Yx, ibc[bb]], [py[cj]])
                        P.mm(py[cj].ap[:, 0:nt], Yx.ap[:, fc, 256 + cj * 128:384 + cj * 128], ibs[bb].ap[:, 0:nt], False,
                             fc == nT - 1, [Yx, ibs[bb]], [py[cj]])
                for cj in range(2):
                    cs = slice(col_base + t0, col_base + t0 + nt)
                    tk0 = tok_base + t0
                    tiles = list(range(tk0 // 128, (tk0 + nt) // 128))
                    P.stt(fin.ap[:, 0:nt], uT.ap[:, cj, cs], vcol.ap[:, 24 + cj:25 + cj], py[cj].ap[:, 0:nt], ALU.mult, ALU.add,
                          [uT, vcol, py[cj]], [fin])
                    P.tt('dve', fin.ap[:, 0:nt], fin.ap[:, 0:nt], x0c.ap[:, cj, cs], ALU.mult, [fin, x0c], [fin])
                    P.tt('dve', mixT.ap[:, 2 + cj, tk0:tk0 + nt], fin.ap[:, 0:nt], sgT.ap[:, cj, cs], ALU.mult, [fin, sgT],
                         [mixb[1][t] for t in tiles])

        dft_conv(RC, RS, Ysp, 16, "l", wfl, [(0, 512), (512, 512), (1024, 512), (1536, 512)], 262, 256)
        if do_ctx:
            dft_conv(RCc, RSc, Yc, 2, "c", wfc, [(0, 256)], 2, 0)
        P.fence()
    P.es = L["es"]


BRANCH_BUILDERS["hyena"] = branch_hyena


def kernel(**inputs):
    nc = build(n_layers=4)
    maps = make_in_maps(inputs)
    res = run_bass_kernel_spmd(nc, maps, core_ids=list(range(8)))
    out = np.stack([np.asarray(res.results[b]["y"]).astype(np.float32) for b in range(8)], 0)
    return out
```

```python
import numpy as np
from contextlib import ExitStack
import concourse.bass as bass
import concourse.mybir as mybir
from concourse.bass_utils import run_bass_kernel_spmd

F32 = mybir.dt.float32
BF16 = mybir.dt.bfloat16
AF = mybir.ActivationFunctionType
ALU = mybir.AluOpType
AX = mybir.AxisListType

ND_SEMS = 40
VERBOSE = False
STOP = None


class Buf:
    def __init__(self, name, ap=None):
        self.name = name
        self.ap = ap
        self.writer = None
        self.readers = {}
        self.excl = False

    def __getitem__(self, k):
        return self.ap[k]


class Prog:
    def __init__(self, nc, es):
        self.nc = nc
        self.es = es
        self.engs = ['pe', 'act', 'dve', 'pool', 'sp']
        self.lists = {e: [] for e in self.engs}
        self.cnt = {e: 0 for e in self.engs}
        self.sem = {e: es.enter_context(nc.semaphore("s_" + e)) for e in ['pe', 'act', 'dve', 'pool']}
        self.dsem = [es.enter_context(nc.semaphore("d%d" % i)) for i in range(ND_SEMS)]
        self.dcnt = [0] * ND_SEMS
        self.dnext = 0
        self.dnext_sw = 0
        self.waited = {e: {} for e in self.engs}
        self.out_tokens = []
        self.nbuf = 0
        self.dead = False

    def sb(self, name, shape, dtype):
        self.nbuf += 1
        name = "%s_u%d" % (name, self.nbuf)
        t = self.es.enter_context(self.nc.sbuf_tensor(name, shape, dtype))
        assert self.nc.sbuf_bytes_remaining >= 16384 + 256, (name, self.nc.sbuf_bytes_remaining)
        return Buf(name, t)

    def ps(self, name, shape, dtype):
        t = self.es.enter_context(self.nc.psum_tensor(name, shape, dtype))
        b = Buf(name, t)
        b.excl = True
        return b

    def vb(self, name, ap=None):
        return Buf(name, ap)

    def _collect(self, eng, reads, writes):
        deps = []
        for b in reads:
            if b.writer is not None:
                deps.append(b.writer)
        for b in writes:
            if b.writer is not None:
                deps.append(b.writer)
            deps.extend(b.readers.values())
        waits = []
        w = self.waited[eng]
        for tok in deps:
            if tok[0] == 'e':
                if tok[1] == 'pe' and eng == 'pe':
                    continue
                key = tok[1]
                sem = self.sem[tok[1]]
            else:
                key = ('d', tok[1])
                sem = self.dsem[tok[1]]
            if w.get(key, 0) >= tok[2]:
                continue
            w[key] = tok[2]
            waits.append((sem, tok[2], key))
        best = {}
        for sem, val, key in waits:
            if key not in best or best[key][1] < val:
                best[key] = (sem, val)
        return list(best.values())

    def _mark(self, tok, rkey, reads, writes):
        for b in reads:
            b.readers[rkey] = tok
        for b in writes:
            b.writer = tok
            b.readers = {}

    def ckpt(self, name):
        if STOP is not None and STOP == name:
            self.dead = True

    def op(self, eng, fn, reads=(), writes=()):
        if self.dead:
            return
        if eng != 'pe':
            ex = [b for b in reads if b.excl]
            if ex:
                writes = list(writes) + [b for b in ex if b not in writes]
        waits = self._collect(eng, reads, writes)
        self.cnt[eng] += 1
        tok = ('e', eng, self.cnt[eng])
        self.lists[eng].append((waits, fn, ('inc', self.sem[eng])))
        self._mark(tok, eng, reads, writes)

    def dma(self, q, out_ap, in_ap, reads=(), writes=(), out=False, **kw):
        if self.dead and not out:
            return
        waits = self._collect(q, reads, writes)
        if q == 'pool':
            i = ND_SEMS - 8 + self.dnext_sw
            self.dnext_sw = (self.dnext_sw + 1) % 8
        else:
            i = self.dnext
            self.dnext = (self.dnext + 1) % (ND_SEMS - 8)
        if self.dcnt[i] > 0:
            key = ('d', i)
            val = self.dcnt[i] * 16
            if self.waited[q].get(key, 0) < val:
                self.waited[q][key] = val
                waits.append((self.dsem[i], val))
        self.dcnt[i] += 1
        tok = ('d', i, self.dcnt[i] * 16)
        fn = lambda e, o=out_ap, s=in_ap, k=kw: e.dma_start(out=o, in_=s, **k)
        self.lists[q].append((waits, fn, ('dinc', self.dsem[i])))
        self._mark(tok, ('d', i), reads, writes)
        if out:
            self.out_tokens.append(tok)

    def finish(self):
        waits = []
        for tok in self.out_tokens:
            waits.append((self.dsem[tok[1]], tok[2]))
        self.lists['sp'].append((waits, None, None))
        nc = self.nc
        lists = self.lists

        def replay(name, e):
            for waits, fn, inc in lists[name]:
                for sem, val in waits:
                    e.wait_ge(sem, val)
                if fn is None:
                    continue
                ins = fn(e)
                if inc[0] == 'inc':
                    ins.then_inc(inc[1], 1)
                else:
                    ins.then_inc(inc[1], 16)

        with nc.Block() as block:
            @block.sync
            def _(e):
                replay('sp', e)

            @block.tensor
            def _(e):
                replay('pe', e)

            @block.scalar
            def _(e):
                replay('act', e)

            @block.vector
            def _(e):
                replay('dve', e)

            @block.gpsimd
            def _(e):
                replay('pool', e)


    def mm(self, out, lhsT, rhs, start, stop, reads, writes):
        self.op('pe', lambda e: e.matmul(out, lhsT, rhs, start=start, stop=stop), reads, writes)

    def tr(self, out, in_, ident, reads, writes):
        self.op('pe', lambda e: e.transpose(out, in_, ident), reads, writes)

    def act(self, out, in_, func, reads, writes, **kw):
        self.op('act', lambda e: e.activation(out, in_, func, **kw), reads, writes)

    def tt(self, eng, out, in0, in1, op, reads, writes):
        self.op(eng, lambda e: e.tensor_tensor(out, in0, in1, op), reads, writes)

    def ts(self, eng, out, in0, s1, s2, op0, op1, reads, writes):
        self.op(eng, lambda e: e.tensor_scalar(out, in0, s1, s2, op0, op1), reads, writes)

    def stt(self, out, in0, scalar, in1, op0, op1, reads, writes):
        self.op('dve', lambda e: e.scalar_tensor_tensor(out, in0, scalar, in1, op0, op1), reads, writes)

    def cp(self, eng, out, in_, reads, writes):
        if eng == 'act':
            self.op('act', lambda e: e.copy(out, in_), reads, writes)
        else:
            self.op(eng, lambda e: e.tensor_copy(out, in_), reads, writes)

    def fence(self):
        for e in self.engs:
            waits = []
            for o in ['pe', 'act', 'dve', 'pool']:
                if not (o == e == 'pe') and self.cnt[o] > 0 and self.waited[e].get(o, 0) < self.cnt[o]:
                    self.waited[e][o] = self.cnt[o]
                    waits.append((self.sem[o], self.cnt[o]))
            for i in range(ND_SEMS):
                if self.dcnt[i] > 0 and self.waited[e].get(('d', i), 0) < self.dcnt[i] * 16:
                    self.waited[e][('d', i)] = self.dcnt[i] * 16
                    waits.append((self.dsem[i], self.dcnt[i] * 16))
            self.lists[e].append((waits, None, None))


D = 1024
NT = 18
TOK = NT * 128
EPS = 1e-6
N_IN = 4144
COL_A, COL_H, COL_G, COL_D = 0, 1296, 2320, 3376


def host_consts():
    c = {}
    pos = np.arange(2048)
    row = (pos // 64).astype(np.float64)
    col = (pos % 64).astype(np.float64)
    inv = 10000.0 ** (-np.arange(16, dtype=np.float64) / 16)
    ang = np.concatenate([row[:, None] * inv, col[:, None] * inv], -1)
    c["ropec"] = np.ascontiguousarray(np.cos(ang).reshape(16, 128, 32).transpose(1, 0, 2)).astype(np.float32)
    c["ropes"] = np.ascontiguousarray(np.sin(ang).reshape(16, 128, 32).transpose(1, 0, 2)).astype(np.float32)
    t = np.arange(128)[:, None]
    s_ = np.arange(128)[None, :]
    m = np.zeros((128, 384), np.float32)
    m[:, 0:128] = np.where(s_ >= t, 0.0, -30000.0)
    m[:, 256:384] = np.where(s_ <= t, 0.0, -30000.0)
    c["amask"] = m
    import ml_dtypes
    bf = ml_dtypes.bfloat16
    for tag, Lh in (("l", 2048), ("c", 256)):
        pos = np.arange(Lh, dtype=np.float32)
        t = (pos / np.float32(max(Lh - 1, 1))).astype(np.float32)
        w = (np.float32(2.0 * np.pi) * pos / np.float32(Lh)).astype(np.float32)
        f = np.linspace(1e-4, 15, 16, dtype=np.float32)
        z = np.concatenate([t[:, None], np.cos(w[:, None] * f), -np.sin(w[:, None] * f)], -1).astype(np.float32)
        c["zT_" + tag] = np.ascontiguousarray(z.T)
        nt_ = Lh // 128
        c["ntcol_" + tag] = np.ascontiguousarray((-t).reshape(nt_, 128).T)
        N = 2 * Lh - 1
        idx = (np.outer(np.arange(Lh, dtype=np.int64), np.arange(Lh, dtype=np.int64)) % N).astype(np.float64)
        ang = 2.0 * np.pi * idx / N
        C = np.cos(ang)
        S = np.sin(ang)
        c["Cn_" + tag] = np.ascontiguousarray(C.astype(np.float32).astype(bf))
        c["Sn_" + tag] = np.ascontiguousarray(S.astype(np.float32).astype(bf))
        c["Ct_" + tag] = np.ascontiguousarray(C.reshape(nt_, 128, nt_, 128).transpose(2, 1, 0, 3).astype(np.float32).astype(bf))
        c["St_" + tag] = np.ascontiguousarray(S.reshape(nt_, 128, nt_, 128).transpose(2, 1, 0, 3).astype(np.float32).astype(bf))
        wf = np.full(Lh, 2.0 / N, np.float32)
        wf[0] = 1.0 / N
        c["wf_" + tag] = np.ascontiguousarray(wf.reshape(nt_, 128).T)
    import math
    dmin, dmax = math.log(1e-2) / 1.5, math.log(1e-2) / 0.3
    c["deltas"] = np.abs(np.linspace(dmin, dmax, 256, dtype=np.float32)).reshape(1, 256).astype(np.float32)
    return c


def build(n_layers=4, tap=None, branches=("att", "mlstm", "gla", "hyena")):
    nc = bass.Bass("TRN2", target_bir_lowering=False)

    def din(name, shape, dt=F32):
        return nc.dram_tensor(name, shape, dt, kind="ExternalInput").ap()

    x_in = din("x", [2048, D])
    ctx_in = din("ctx", [256, D])
    cc_in = din("cc", [16, 128])
    w_ada = din("w_ada", [4, D, 3 * D])
    b_ada = din("b_ada", [4, 3 * D])
    g_pre = din("g_pre", [4, D])
    g_post = din("g_post", [4, D])
    w_in = din("w_in", [4, D, N_IN])
    w_out = din("w_out", [4, D, D])
    mlstm_gate_b = din("mlstm_gate_b", [4, 16])
    mlstm_norm_g = din("mlstm_norm_g", [4, 256])
    gla_norm_g = din("gla_norm_g", [4, 256])
    gla_w_alpha = din("gla_w_alpha", [4, 2, 16, 256])
    gla_b_alpha = din("gla_b_alpha", [4, 2, 256])
    attn_sink = din("attn_sink", [4, 4])
    ropec_in = din("ropec", [128, 16, 32])
    ropes_in = din("ropes", [128, 16, 32])
    amask_in = din("amask", [128, 384])
    hy = {}
    for nm, shp in (("hyena_conv_w", [4, 3, 768]), ("hyena_conv_b", [4, 768]), ("hyena_w1", [4, 33, 64]),
                    ("hyena_b1", [4, 64]), ("hyena_w2", [4, 64, 64]), ("hyena_b2", [4, 64]), ("hyena_w3", [4, 64, 512]),
                    ("hyena_b3", [4, 512]), ("hyena_freq", [4, 2, 64]), ("hyena_d", [4, 256]), ("deltas", [1, 256])):
        hy[nm] = din(nm, shp)
    for tag, Lh in (("l", 2048), ("c", 256)):
        nt_ = Lh // 128
        hy["zT_" + tag] = din("zT_" + tag, [33, Lh])
        hy["ntcol_" + tag] = din("ntcol_" + tag, [128, nt_])
        hy["wf_" + tag] = din("wf_" + tag, [128, nt_])
        hy["Cn_" + tag] = din("Cn_" + tag, [Lh, Lh], BF16)
        hy["Sn_" + tag] = din("Sn_" + tag, [Lh, Lh], BF16)
        hy["Ct_" + tag] = din("Ct_" + tag, [nt_, 128, nt_, 128], BF16)
        hy["St_" + tag] = din("St_" + tag, [nt_, 128, nt_, 128], BF16)
    y_out = nc.dram_tensor("y", [2048, D], F32, kind="ExternalOutput").ap()
    ctxs = nc.dram_tensor("ctxs", [256, D], F32).ap()
    tap_out = None
    if tap is not None:
        tap_out = nc.dram_tensor("tap", [128, 8, TOK], BF16, kind="ExternalOutput").ap()

    w_in_v = [w_in[l].rearrange("(kc p) n -> p kc n", p=128) for l in range(4)]
    w_out_v = [w_out[l].rearrange("(kc p) n -> p kc n", p=128) for l in range(4)]
    w_ada_v = [w_ada[l].rearrange("(kc p) n -> p kc n", p=128) for l in range(4)]

    with ExitStack() as es:
        P = Prog(nc, es)
        ident_bf = P.sb("ident_bf", [128, 128], BF16)
        ident_f = P.sb("ident_f", [128, 128], F32)
        tri_le = P.sb("tri_le", [128, 128], F32)
        tri_ge = P.sb("tri_ge", [128, 128], F32)
        tri_gt = P.sb("tri_gt", [128, 128], F32)
        tri_lt = P.sb("tri_lt", [128, 128], F32)
        ropec = P.sb("ropec_sb", [128, 16, 32], F32)
        ropes = P.sb("ropes_sb", [128, 16, 32], F32)
        amask = P.sb("amask_sb", [128, 384], F32)

        def mk_affine(buf, pattern, cm, cmp_op, fill_in=1.0):
            P.op('pool', lambda e: e.memset(buf.ap[:], fill_in), writes=[buf])
            P.op('pool', lambda e: e.affine_select(buf.ap[:], buf.ap[:], pattern=pattern, compare_op=cmp_op,
                                                   fill=0.0, base=0, channel_multiplier=cm), reads=[buf], writes=[buf])

        mk_affine(ident_bf, [[-1, 128]], 1, ALU.is_equal)
        mk_affine(ident_f, [[-1, 128]], 1, ALU.is_equal)
        mk_affine(tri_le, [[1, 128]], -1, ALU.is_ge)
        mk_affine(tri_ge, [[-1, 128]], 1, ALU.is_ge)
        mk_affine(tri_gt, [[-1, 128]], 1, ALU.is_gt)
        mk_affine(tri_lt, [[1, 128]], -1, ALU.is_gt)
        tri_le_b = P.sb("tri_le_b", [128, 128], BF16)
        tri_ge_b = P.sb("tri_ge_b", [128, 128], BF16)
        tri_gt_b = P.sb("tri_gt_b", [128, 128], BF16)
        tri_lt_b = P.sb("tri_lt_b", [128, 128], BF16)
        mk_affine(tri_le_b, [[1, 128]], -1, ALU.is_ge)
        mk_affine(tri_ge_b, [[-1, 128]], 1, ALU.is_ge)
        mk_affine(tri_gt_b, [[-1, 128]], 1, ALU.is_gt)
        mk_affine(tri_lt_b, [[1, 128]], -1, ALU.is_gt)
        P.dma('sp', ropec.ap[:], ropec_in, writes=[ropec])
        P.dma('sp', ropes.ap[:], ropes_in, writes=[ropes])
        P.dma('sp', amask.ap[:], amask_in, writes=[amask])

        pT = [P.ps("pT%d" % i, [128, 1024], BF16) for i in range(2)]
        pf = [P.ps("pf%d" % i, [128, 512], F32) for i in range(6)]

        MA = P.sb("MA", [128, 4, 8, 2], F32)
        MB = P.sb("MB", [128, 4, 8, 2], F32)
        MG = P.sb("MG", [128, 4, 8, 2], F32)

        with ExitStack() as es2:
            P.es = es2
            ccr = P.sb("ccr", [16, 128], F32)
            scs = P.sb("scs", [128, 16], F32)
            sc2 = P.sb("sc2", [128, 8, 2], F32)
            VR1 = P.sb("VR1", [128, 128], F32)
            VR2 = P.sb("VR2", [32, 128], F32)
            VC1 = P.sb("VC1", [128, 128], F32)
            VC2 = P.sb("VC2", [128, 32], F32)
            modc = P.sb("modc", [128, 24, 2], F32)
            wa = [P.sb("wa%d" % i, [128, 8, 512], F32) for i in range(2)]
            P.dma('sp', ccr.ap[:], cc_in, writes=[ccr])
            P.dma('sp', VR1.ap[0:96, :], b_ada.rearrange("l (c p) -> (l c) p", p=128), writes=[VR1])
            P.dma('sp', VR1.ap[96:128, :], g_pre.rearrange("l (c p) -> (l c) p", p=128), writes=[VR1])
            P.dma('sp', VR2.ap[:], g_post.rearrange("l (c p) -> (l c) p", p=128), writes=[VR2])
            P.tr(pf[0].ap[:, 0:16], ccr.ap[:], ident_f.ap[0:16, 0:16], [ccr, ident_f], [pf[0]])
            P.act(scs.ap[:], pf[0].ap[:, 0:16], AF.Silu, [pf[0]], [scs])
            P.cp('dve', sc2.ap[:, :, 0], scs.ap[:, 0:8], [scs], [sc2])
            P.cp('dve', sc2.ap[:, :, 1], scs.ap[:, 8:16], [scs], [sc2])
            P.tr(pf[1].ap[:, 0:128], VR1.ap[:], ident_f.ap[:], [VR1, ident_f], [pf[1]])
            P.cp('dve', VC1.ap[:], pf[1].ap[:, 0:128], [pf[1]], [VC1])
            P.tr(pf[2].ap[:, 0:32], VR2.ap[:], ident_f.ap[0:32, 0:32], [VR2, ident_f], [pf[2]])
            P.cp('dve', VC2.ap[:], pf[2].ap[:, 0:32], [pf[2]], [VC2])
            gi = 0
            for l in range(n_layers):
                pm = pf[3 + (l % 2)]
                for g6 in range(6):
                    wb_ = wa[gi % 2]
                    gi += 1
                    P.dma('sp', wb_.ap[:], w_ada_v[l][:, :, g6 * 512:(g6 + 1) * 512], writes=[wb_])
                    for jj in range(4):
                        j = g6 * 4 + jj
                        for kc in range(8):
                            P.mm(pm.ap[:, j * 2:j * 2 + 2], wb_.ap[:, kc, jj * 128:(jj + 1) * 128], sc2.ap[:, kc, :],
                                 kc == 0, kc == 7, [wb_, sc2], [pm])
                pm3 = pm.ap[:, 0:48].rearrange("p (j s) -> p j s", s=2)
                P.tt('dve', modc.ap[:], pm3, VC1.ap[:, l * 24:(l + 1) * 24].unsqueeze(2).to_broadcast([128, 24, 2]),
                     ALU.add, [pm, VC1], [modc])
                gpre_bc = VC1.ap[:, 96 + l * 8:96 + (l + 1) * 8].unsqueeze(2).to_broadcast([128, 8, 2])
                gpost_bc = VC2.ap[:, l * 8:(l + 1) * 8].unsqueeze(2).to_broadcast([128, 8, 2])
                P.stt(MA.ap[:, l], modc.ap[:, 8:16, :], 1.0, gpre_bc, ALU.add, ALU.mult, [modc, VC1], [MA])
                P.cp('dve', MB.ap[:, l], modc.ap[:, 0:8, :], [modc], [MB])
                P.tt('dve', MG.ap[:, l], modc.ap[:, 16:24, :], gpost_bc, ALU.mult, [modc, VC2], [MG])
            P.fence()
        P.es = es

        hT = P.sb("hT", [128, 8, TOK], BF16)
        mixT = P.sb("mixT", [128, 8, TOK], BF16)
        hTb = [P.vb("hT_t%d" % i) for i in range(NT)]
        mixb = [[P.vb("mix_%d_%d" % (g, i)) for i in range(NT)] for g in range(4)]
        if tap is not None:
            P.op('pool', lambda e: e.memset(mixT.ap[:], 0.0), writes=[b for g in mixb for b in g])
        wbuf = []
        wrot = [0]

        def alloc_wbuf():
            wbuf[:] = [P.sb("wbuf%d" % i, [128, 8, 512], BF16) for i in range(2)]
        xs = [P.vb("xs%d" % i) for i in range(NT)]
        if VERBOSE:
            print("sbuf bytes remaining after main alloc:", nc.sbuf_bytes_remaining)

        def load_w(l, c0, ncols, q='pool'):
            b = wbuf[wrot[0] % 2]
            wrot[0] += 1
            P.dma(q, b.ap[:, :, 0:ncols], w_in_v[l][:, :, c0:c0 + ncols], writes=[b])
            return b

        def proj_tm(i, wb_, c0, ncols, out_ap, outbuf):
            for kc in range(8):
                P.mm(out_ap, hT.ap[:, kc, i * 128:(i + 1) * 128], wb_.ap[:, kc, c0:c0 + ncols], kc == 0, kc == 7,
                     [hTb[i], wb_], [outbuf])

        def proj_fm(wb_, c0, ncols, t0, nt, out_ap, outbuf):
            rd = [wb_] + [hTb[i] for i in range(t0 // 128, (t0 + nt + 127) // 128)]
            for kc in range(8):
                P.mm(out_ap, wb_.ap[:, kc, c0:c0 + ncols], hT.ap[:, kc, t0:t0 + nt], kc == 0, kc == 7, rd, [outbuf])

        def state_src(l, i):
            if i < 2:
                return (ctx_in if l == 0 else ctxs)[i * 128:(i + 1) * 128, :]
            return (x_in if l == 0 else y_out)[(i - 2) * 128:(i - 1) * 128, :]

        def state_dst(i):
            if i < 2:
                return ctxs[i * 128:(i + 1) * 128, :]
            return y_out[(i - 2) * 128:(i - 1) * 128, :]

        for l in range(n_layers):
            last = (l == 3)
            esC = ExitStack()
            P.es = esC
            xt = [P.sb("xt%d" % i, [128, 1024], F32) for i in range(2)]
            xn = [P.sb("xn%d" % i, [128, 1024], BF16) for i in range(2)]
            tmpf = P.sb("tmpf", [128, 1024], F32)
            st4 = [P.sb("st4_%d" % i, [128, 8], F32) for i in range(2)]
            for i in range(NT):
                s = 1 if i < 2 else 0
                xb_, xnb, stb = xt[i % 2], xn[i % 2], st4[i % 2]
                P.dma('sp', xb_.ap[:], state_src(l, i), reads=[xs[i]], writes=[xb_])
                P.act(tmpf.ap[:], xb_.ap[:], AF.Square, [xb_], [tmpf, stb], accum_out=stb.ap[:, 0:1])
                P.act(stb.ap[:, 1:2], stb.ap[:, 0:1], AF.Sqrt, [stb], [stb], bias=EPS, scale=1.0 / D)
                P.op('dve', lambda e, o=stb.ap[:, 2:3], a=stb.ap[:, 1:2]: e.reciprocal(o, a), [stb], [stb])
                P.act(xnb.ap[:], xb_.ap[:], AF.Identity, [xb_, stb], [xnb], scale=stb.ap[:, 2:3])
                pt = pT[i % 2]
                for kc in range(8):
                    P.tr(pt.ap[:, kc * 128:(kc + 1) * 128], xnb.ap[:, kc * 128:(kc + 1) * 128], ident_bf.ap[:],
                         [xnb, ident_bf], [pt])
                pt3 = pt.ap[:, :].rearrange("p (k t) -> p k t", t=128)
                tm3 = tmpf.ap[:, :].rearrange("p (k t) -> p k t", t=128)
                P.tt('dve', tm3, pt3, MA.ap[:, l, :, s:s + 1].to_broadcast([128, 8, 128]), ALU.mult, [pt, MA], [tmpf])
                P.tt('dve', hT.ap[:, :, i * 128:(i + 1) * 128], tm3, MB.ap[:, l, :, s:s + 1].to_broadcast([128, 8, 128]),
                     ALU.add, [tmpf, MB], [hTb[i]])
            if tap == "h%d" % l:
                P.dma('sp', tap_out, hT.ap[:], reads=hTb, out=True)

            P.fence()
            esC.close()
            P.es = es

            BRANCH_BUILDERS["att"](locals()) if "att" in branches else None
            BRANCH_BUILDERS["mlstm"](locals()) if "mlstm" in branches else None
            BRANCH_BUILDERS["gla"](locals()) if "gla" in branches else None
            BRANCH_BUILDERS["hyena"](locals()) if "hyena" in branches else None
            if tap == "mix%d" % l:
                P.dma('sp', tap_out, mixT.ap[:], reads=[b for g in mixb for b in g], out=True)

            esZ = ExitStack()
            P.es = esZ
            woutb = P.sb("woutb", [128, 8, 1024], BF16)
            Gbc = [P.sb("Gbc%d" % s, [128, 1024], F32) for s in range(2)]
            gbt = P.sb("gbt", [128, 128], F32)
            xt = [P.sb("xt%d" % i, [128, 1024], F32) for i in range(2)]
            tmpf = P.sb("tmpf", [128, 1024], F32)
            st4 = [P.sb("st4_%d" % i, [128, 8], F32) for i in range(2)]
            for h2 in range(2):
                P.dma('pool', woutb.ap[:, :, h2 * 512:(h2 + 1) * 512], w_out_v[l][:, :, h2 * 512:(h2 + 1) * 512],
                      writes=[woutb])
            for s in range(2):
                for j in range(8):
                    P.cp('dve', gbt.ap[:], MG.ap[:, l, j, s:s + 1].to_broadcast([128, 128]), [MG], [gbt])
                    pg = pf[j // 4]
                    P.mm(pg.ap[:, (j % 4) * 128:(j % 4 + 1) * 128], gbt.ap[:], ident_f.ap[:], True, True,
                         [gbt, ident_f], [pg])
                for h2 in range(2):
                    P.cp('act', Gbc[s].ap[:, h2 * 512:(h2 + 1) * 512], pf[h2].ap[:], [pf[h2]], [Gbc[s]])
            for i in range(NT):
                if last and i < 2:
                    continue
                s = 1 if i < 2 else 0
                xb_, stb = xt[i % 2], st4[i % 2]
                P.dma('sp', xb_.ap[:], state_src(l, i), reads=[xs[i]], writes=[xb_])
                py = [pf[2 + 2 * (i % 2)], pf[3 + 2 * (i % 2)]]
                mrd = [mixb[g][i] for g in range(4)]
                for h2 in range(2):
                    for kc in range(8):
                        P.mm(py[h2].ap[:], mixT.ap[:, kc, i * 128:(i + 1) * 128], woutb.ap[:, kc, h2 * 512:(h2 + 1) * 512],
                             kc == 0, kc == 7, mrd + [woutb], [py[h2]])
                for h2 in range(2):
                    P.act(tmpf.ap[:, h2 * 512:(h2 + 1) * 512], py[h2].ap[:], AF.Square, [py[h2]], [tmpf, stb],
                          accum_out=stb.ap[:, 4 + h2:5 + h2])
                P.tt('dve', stb.ap[:, 6:7], stb.ap[:, 4:5], stb.ap[:, 5:6], ALU.add, [stb], [stb])
                P.act(stb.ap[:, 7:8], stb.ap[:, 6:7], AF.Sqrt, [stb], [stb], bias=EPS, scale=1.0 / D)
                P.op('dve', lambda e, o=stb.ap[:, 3:4], a=stb.ap[:, 7:8]: e.reciprocal(o, a), [stb], [stb])
                for h2 in range(2):
                    sl = slice(h2 * 512, (h2 + 1) * 512)
                    P.stt(tmpf.ap[:, sl], py[h2].ap[:], stb.ap[:, 3:4], Gbc[s].ap[:, sl], ALU.mult, ALU.mult,
                          [py[h2], stb, Gbc[s]], [tmpf])
                P.tt('dve', xb_.ap[:], xb_.ap[:], tmpf.ap[:], ALU.add, [xb_, tmpf], [xb_])
                P.dma('sp', state_dst(i), xb_.ap[:], reads=[xb_], writes=[xs[i]], out=(l == n_layers - 1 and i >= 2))
            P.fence()
            esZ.close()
            P.es = es
        P.finish()
    return nc


BRANCH_BUILDERS = {}


def branch_att(L):
    P, nc, l, last = L["P"], L["nc"], L["l"], L["last"]
    hT, hTb, mixT, mixb, pT, pf = L["hT"], L["hTb"], L["mixT"], L["mixb"], L["pT"], L["pf"]
    ident_bf, ropec, ropes, amask = L["ident_bf"], L["ropec"], L["ropes"], L["amask"]
    load_w, proj_tm = L["load_w"], L["proj_tm"]
    with ExitStack() as es2:
        P.es = es2
        L["alloc_wbuf"]()
        QKT = P.sb("QKT", [128, 4, TOK], BF16)
        QKb = [P.vb("QKT_%d" % i) for i in range(NT)]
        v_tm = P.sb("v_tm", [128, NT, 128], BF16)
        sg_tm = P.sb("sg_tm", [128, NT, 256], BF16)
        vb_ = [P.vb("vsg_%d" % i) for i in range(NT)]
        qkf = P.sb("qkf", [128, 384], F32)
        rt = [P.sb("rt%d" % i, [128, 192], F32) for i in range(2)]
        rq = P.sb("rq", [128, 512], BF16)
        sink_bc = P.sb("sink_bc", [128, 4], F32)
        sm2 = [P.sb("sm%d" % r, [128, 640], F32) for r in range(2)]
        Pb2 = [P.sb("Pb%d" % r, [128, 640], BF16) for r in range(2)]
        PTs2 = [P.sb("PTs%d" % r, [128, 5, 128], BF16) for r in range(2)]
        stt2 = [P.sb("att_st%d" % r, [128, 8], F32) for r in range(2)]
        rden2 = [P.sb("rden%d" % r, [128, 4], F32) for r in range(2)]
        og2 = [P.sb("og%d" % r, [128, 256], BF16) for r in range(2)]
        P.dma('sp', sink_bc.ap[:], L["attn_sink"][l:l + 1, :].partition_broadcast(128), writes=[sink_bc])
        w1 = load_w(l, COL_D, 384)
        w2 = load_w(l, COL_D + 384, 384)
        for i in range(NT):
            pq, pv = pf[0], pf[1]
            proj_tm(i, w1, 0, 384, pq.ap[:, 0:384], pq)
            proj_tm(i, w2, 0, 384, pv.ap[:, 0:384], pv)
            P.cp('act', v_tm.ap[:, i, :], pv.ap[:, 0:128], [pv], [vb_[i]])
            P.act(sg_tm.ap[:, i, :], pv.ap[:, 128:384], AF.Silu, [pv], [vb_[i]])
            if i >= 2:
                P.cp('act', qkf.ap[:], pq.ap[:, 0:384], [pq], [qkf])
                q4 = qkf.ap[:, :].rearrange("p (h two j) -> p h two j", two=2, j=32)
                r4 = rq.ap[:, 0:384].rearrange("p (h two j) -> p h two j", two=2, j=32)
                x1, x2 = q4[:, :, 0, :], q4[:, :, 1, :]
                cosb = ropec.ap[:, i - 2, :].unsqueeze(1).to_broadcast([128, 6, 32])
                sinb = ropes.ap[:, i - 2, :].unsqueeze(1).to_broadcast([128, 6, 32])
                ta = rt[0].ap[:, :].rearrange("p (h j) -> p h j", j=32)
                tb = rt[1].ap[:, :].rearrange("p (h j) -> p h j", j=32)
                P.tt('dve', ta, x1, cosb, ALU.mult, [qkf, ropec], [rt[0]])
                P.tt('dve', tb, x2, sinb, ALU.mult, [qkf, ropes], [rt[1]])
                P.tt('dve', r4[:, :, 0, :], ta, tb, ALU.subtract, [rt[0], rt[1]], [rq])
                P.tt('dve', ta, x2, cosb, ALU.mult, [qkf, ropec], [rt[0]])
                P.tt('dve', tb, x1, sinb, ALU.mult, [qkf, ropes], [rt[1]])
                P.tt('dve', r4[:, :, 1, :], ta, tb, ALU.add, [rt[0], rt[1]], [rq])
            else:
                P.cp('act', rq.ap[:, 0:384], pq.ap[:, 0:384], [pq], [rq])
            kd_src = rq.ap[:, 256:384].rearrange("p (g o d) -> p g o d", o=1, d=64).to_broadcast([128, 2, 2, 64])
            P.cp('dve', rt[0].ap[:, 0:128].bitcast(BF16).rearrange("p (g o d) -> p g o d", o=2, d=64), kd_src,
                 [rq], [rt[0]])
            P.cp('dve', rq.ap[:, 256:512], rt[0].ap[:, 0:128].bitcast(BF16), [rt[0]], [rq])
            pt = pT[i % 2]
            for c4 in range(4):
                P.tr(pt.ap[:, c4 * 128:(c4 + 1) * 128], rq.ap[:, c4 * 128:(c4 + 1) * 128], ident_bf.ap[:],
                     [rq, ident_bf], [pt])
            P.cp('dve', QKT.ap[:, :, i * 128:(i + 1) * 128], pt.ap[:, 0:512].rearrange("p (c t) -> p c t", t=128),
                 [pt], [QKb[i]])

        def geom(i):
            has_local = i >= 2
            n = i - 2
            if has_local:
                nlo, nhi = max(n - 1, 0), min(n + 1, 15)
                c0 = (nlo - (n - 1)) * 128
                c1 = c0 + (nhi - nlo + 1) * 128
                ktiles = list(range(2 + nlo, 2 + nhi + 1))
            else:
                c0 = c1 = 384
                ktiles = []
            d0 = 384 - (c1 - c0)
            blocks = [(kt, d0 + 128 * bi) for bi, kt in enumerate(ktiles)] + [(0, 384), (1, 512)]
            return has_local, c0, c1, d0, ktiles, blocks

        def stage1(i, h):
            has_local, c0, c1, d0, ktiles, blocks = geom(i)
            rden = rden2[i % 2]
            g, base = h // 2, (h % 2) * 64
            r = h % 2
            sm, Pb, stt_ = sm2[r], Pb2[r], stt2[r]
            pl, pc = (pf[4], pf[5]) if r == 0 else (pf[0], pf[1])
            qa = QKT.ap[base:base + 64, g, i * 128:(i + 1) * 128]
            if has_local:
                P.mm(pl.ap[:, d0:384], qa, QKT.ap[base:base + 64, 2 + g, ktiles[0] * 128:(ktiles[-1] + 1) * 128],
                     True, True, [QKb[i]] + [QKb[k] for k in ktiles], [pl])
                P.tt('dve', sm.ap[:, d0:384], pl.ap[:, d0:384], amask.ap[:, c0:c1], ALU.add, [pl, amask], [sm])
            P.mm(pc.ap[:, 0:256], qa, QKT.ap[base:base + 64, 2 + g, 0:256], True, True, [QKb[i], QKb[0], QKb[1]], [pc])
            P.cp('act', sm.ap[:, 384:640], pc.ap[:, 0:256], [pc], [sm])
            P.op('dve', lambda e, o=stt_.ap[:, 0:1], a=sm.ap[:, d0:640]: e.reduce_max(o, a, AX.X), [sm], [stt_])
            P.ts('dve', stt_.ap[:, 1:2], stt_.ap[:, 0:1], -0.125, None, ALU.mult, ALU.bypass, [stt_], [stt_])
            P.act(Pb.ap[:, d0:640], sm.ap[:, d0:640], AF.Exp, [sm, stt_], [Pb, stt_], bias=stt_.ap[:, 1:2], scale=0.125,
                  accum_out=stt_.ap[:, 2:3])
            P.act(stt_.ap[:, 3:4], stt_.ap[:, 1:2], AF.Exp, [stt_, sink_bc], [stt_], bias=sink_bc.ap[:, h:h + 1], scale=1.0)
            P.tt('dve', stt_.ap[:, 4:5], stt_.ap[:, 2:3], stt_.ap[:, 3:4], ALU.add, [stt_], [stt_])
            P.op('dve', lambda e, o=rden.ap[:, h:h + 1], a=stt_.ap[:, 4:5]: e.reciprocal(o, a), [stt_], [rden])

        def stage2(i, h):
            has_local, c0, c1, d0, ktiles, blocks = geom(i)
            g = h // 2
            r = h % 2
            Pb, PTs = Pb2[r], PTs2[r]
            pO = pf[2 + (i % 2)]
            pt = pT[h % 2]
            for bi, (kt, cc) in enumerate(blocks):
                P.tr(pt.ap[:, bi * 128:(bi + 1) * 128], Pb.ap[:, cc:cc + 128], ident_bf.ap[:], [Pb, ident_bf], [pt])
            nb = len(blocks)
            P.cp('act', PTs.ap[:, 0:nb, :], pt.ap[:, 0:nb * 128].rearrange("p (c t) -> p c t", t=128), [pt], [PTs])
            for bi, (kt, cc) in enumerate(blocks):
                P.mm(pO.ap[:, h * 64:(h + 1) * 64], PTs.ap[:, bi, :], v_tm.ap[:, kt, g * 64:(g + 1) * 64],
                     bi == 0, bi == nb - 1, [PTs, vb_[kt]], [pO])

        def epilogue(i):
            pO = pf[2 + (i % 2)]
            rden, og = rden2[i % 2], og2[i % 2]
            for h in range(4):
                P.stt(og.ap[:, h * 64:(h + 1) * 64], pO.ap[:, h * 64:(h + 1) * 64], rden.ap[:, h:h + 1],
                      sg_tm.ap[:, i, h * 64:(h + 1) * 64], ALU.mult, ALU.mult, [pO, rden, vb_[i]], [og])
            pt = pT[i % 2]
            for c2 in range(2):
                P.tr(pt.ap[:, c2 * 128:(c2 + 1) * 128], og.ap[:, c2 * 128:(c2 + 1) * 128], ident_bf.ap[:], [og, ident_bf], [pt])
            P.cp('act', mixT.ap[:, 6:8, i * 128:(i + 1) * 128], pt.ap[:, 0:256].rearrange("p (c t) -> p c t", t=128),
                 [pt], [mixb[3][i]])

        items = [(i, h) for i in range(NT) if not (last and i < 2) for h in range(4)]
        stage1(*items[0])
        for n_, (i, h) in enumerate(items):
            if n_ + 1 < len(items):
                stage1(*items[n_ + 1])
            stage2(i, h)
            if h == 3:
                epilogue(i)
        P.fence()
    P.es = L["es"]


BRANCH_BUILDERS["att"] = branch_att


def make_in_maps(inputs):
    c = host_consts()
    shared = {}
    for k in ["w_ada", "b_ada", "g_pre", "g_post", "w_in", "w_out", "mlstm_norm_g", "gla_norm_g", "gla_w_alpha",
              "gla_b_alpha", "attn_sink"]:
        shared[k] = np.ascontiguousarray(np.asarray(inputs[k], dtype=np.float32))
    for k in ["hyena_conv_w", "hyena_conv_b", "hyena_w1", "hyena_b1", "hyena_w2", "hyena_b2", "hyena_w3", "hyena_b3",
              "hyena_freq", "hyena_d"]:
        shared[k] = np.ascontiguousarray(np.asarray(inputs[k], dtype=np.float32))
    shared["mlstm_gate_b"] = np.ascontiguousarray(np.asarray(inputs["mlstm_gate_b"], dtype=np.float32).reshape(4, 16))
    shared.update(c)
    maps = []
    c_ctx = np.asarray(inputs["c_ctx"], dtype=np.float32)
    for b in range(8):
        m = dict(shared)
        m["x"] = np.ascontiguousarray(np.asarray(inputs["x"][b], dtype=np.float32))
        m["ctx"] = np.ascontiguousarray(np.asarray(inputs["ctx"][b], dtype=np.float32))
        m["cc"] = np.ascontiguousarray(np.concatenate([np.asarray(inputs["c"][b], dtype=np.float32), c_ctx]).reshape(16, 128))
        maps.append(m)
    return maps


def branch_scan(L, kind):
    ml = kind == "mlstm"
    P, nc, l, last = L["P"], L["nc"], L["l"], L["last"]
    hT, hTb, mixT, mixb, pT, pf = L["hT"], L["hTb"], L["mixT"], L["mixb"], L["pT"], L["pf"]
    ident_bf, ident_f = L["ident_bf"], L["ident_f"]
    load_w, proj_tm, proj_fm = L["load_w"], L["proj_tm"], L["proj_fm"]
    base = COL_A if ml else COL_G
    cdec = 1.0 if ml else 1.0 / 16
    mc0, mg = (0, 0) if ml else (4, 2)
    qscale, kscale = (1.0, 0.125) if ml else (0.125, 1.0)
    with ExitStack() as es2:
        P.es = es2
        L["alloc_wbuf"]()
        qT = P.sb("s_qT", [128, 2, TOK], BF16)
        kT = P.sb("s_kT", [128, 2, TOK], BF16)
        k_tm = P.sb("s_ktm", [128, NT, 256], BF16)
        v_aug = P.sb("s_vaug", [128, NT, 512], BF16)
        Hh = P.sb("s_H", [128, NT, 256], F32)
        gr = P.sb("s_gr", [128, NT, 32], F32)
        qkb = [P.vb("s_qkb%d" % i) for i in range(NT)]
        kvb = [P.vb("s_kvb%d" % i) for i in range(NT)]
        Hb = [P.vb("s_Hb%d" % i) for i in range(NT)]
        grb = [P.vb("s_grb%d" % i) for i in range(NT)]
        normg = P.sb("s_normg", [128, 256], F32)
        P.dma('sp', normg.ap[:], (L["mlstm_norm_g"] if ml else L["gla_norm_g"])[l:l + 1, :].partition_broadcast(128),
              writes=[normg])
        if ml:
            gbb = P.sb("s_gbb", [128, 16], F32)
            P.dma('sp', gbb.ap[:], L["mlstm_gate_b"][l:l + 1, :].partition_broadcast(128), writes=[gbb])
            posi = P.sb("s_posi", [128, 256], BF16)
            negi = P.sb("s_negi", [128, 256], BF16)
            e4 = P.sb("s_e4", [128, 8], F32)
        else:
            wal = P.sb("s_wal", [17, 2, 256], F32)
            P.dma('sp', wal.ap[0:16, :, :], L["gla_w_alpha"][l].rearrange("d r c -> r d c"), writes=[wal])
            P.dma('sp', wal.ap[16:17, :, :], L["gla_b_alpha"][l:l + 1, :, :], writes=[wal])
            rTa = P.sb("s_rTa", [17, 128], F32)
            P.op('pool', lambda e: e.memset(rTa.ap[:], 1.0), writes=[rTa])
            e_tm = P.sb("s_etm", [128, 256], F32)
        sp_tm = P.sb("s_sp", [128, 256], BF16)
        EqT2 = [P.sb("s_EqT%d" % r, [128, 256], F32) for r in range(2)]
        EkT = P.sb("s_EkT", [128, 256], F32)
        Ex = P.sb("s_Ex", [128, 256], F32)
        qp2 = [P.sb("s_qp%d" % r, [128, 2, 128], BF16) for r in range(2)]
        kpz = [P.sb("s_kpz%d" % h, [128, 128], BF16) for h in range(4)]
        for h in range(4):
            P.op('pool', lambda e, a=kpz[h].ap[:]: e.memset(a, 0.0), writes=[kpz[h]])
        kpp2 = [P.sb("s_kpp%d" % r, [128, 256], BF16) for r in range(2)]
        S_m2 = [P.sb("s_Sm%d" % r, [128, 512], BF16) for r in range(2)]
        St_f = [P.sb("s_Stf%d" % j, [128, 128], F32) for j in range(2)]
        St_b = [P.sb("s_Stb%d" % h, [128, 128], BF16) for h in range(4)]
        dn = P.sb("s_dn", [128, 8], F32)
        tmpH = P.sb("s_tmpH", [128, 256], F32)

        wqk = load_w(l, base, 512)
        groups = [(0, 512), (512, 512), (1024, 512), (1536, 512), (2048, 256)]
        cnt = 0
        for j in range(2):
            for (t0, nt) in groups:
                tiles = list(range(t0 // 128, (t0 + nt) // 128))
                for which, dst, sc_ in ((0, qT, qscale), (1, kT, kscale)):
                    pq = pf[cnt % 2]
                    cnt += 1
                    proj_fm(wqk, which * 256 + j * 128, 128, t0, nt, pq.ap[:, 0:nt], pq)
                    P.act(dst.ap[:, j, t0:t0 + nt], pq.ap[:, 0:nt], AF.Identity, [pq], [qkb[t] for t in tiles], scale=sc_)
        wkv = load_w(l, base + 256, 512)
        ng = 16 if ml else 32
        wg = load_w(l, base + (1024 if ml else 768), 256 + ng)
        P.op('pool', lambda e: e.memset(v_aug.ap[:], 1.0), writes=kvb)
        for i in range(NT):
            pk = pf[2 + i % 2]
            proj_tm(i, wkv, 0, 512, pk.ap[:], pk)
            P.act(k_tm.ap[:, i, :], pk.ap[:, 0:256], AF.Identity, [pk], [kvb[i]], scale=kscale)
            P.cp('dve', v_aug.ap[:, i, :].rearrange("p (h e) -> p h e", e=128)[:, :, 0:64],
                 pk.ap[:, 256:512].rearrange("p (h e) -> p h e", e=64), [pk], [kvb[i]])
            pg = pf[4 + i % 2]
            proj_tm(i, wg, 256, 64, pg.ap[:, 0:64], pg)
            if ml:
                P.tt('dve', gr.ap[:, i, 0:16], pg.ap[:, 0:16], gbb.ap[:], ALU.add, [pg, gbb], [grb[i]])
            else:
                P.cp('dve', gr.ap[:, i, 0:32], pg.ap[:, 0:32], [pg], [grb[i]])

        P.ckpt("pre")
        for dirn in range(2):
            order = ([0, 1] + list(range(2, NT))) if dirn == 0 else ([1, 0] + list(range(NT - 1, 1, -1)))
            tri_c = L["tri_le"] if dirn == 0 else L["tri_ge"]
            tri_x = L["tri_gt"] if dirn == 0 else L["tri_lt"]
            tri_cb = L["tri_le_b"] if dirn == 0 else L["tri_ge_b"]
            tri_xb = L["tri_gt_b"] if dirn == 0 else L["tri_lt_b"]
            dcol = 127 if dirn == 0 else 0
            for j in range(2):
                P.op('pool', lambda e, a=St_f[j].ap[:]: e.memset(a, 0.0), writes=[St_f[j]])
            for h in range(4):
                P.op('pool', lambda e, a=St_b[h].ap[:]: e.memset(a, 0.0), writes=[St_b[h]])
            def prefix(step, i):
                tsl = slice(i * 128, (i + 1) * 128)
                EqT, qp, kpp, S_m = EqT2[step % 2], qp2[step % 2], kpp2[step % 2], S_m2[step % 2]
                if ml:
                    ic, fc = dirn * 8, dirn * 8 + 4
                    P.act(e4.ap[:, 0:4], gr.ap[:, i, fc:fc + 4], AF.Exp, [grb[i]], [e4], scale=-1.0)
                    P.act(e4.ap[:, 4:8], e4.ap[:, 0:4], AF.Ln, [e4], [e4], bias=1.0)
                    P.cp('dve', sp_tm.ap[:, :].rearrange("p (h d) -> p h d", d=64),
                         e4.ap[:, 4:8].unsqueeze(2).to_broadcast([128, 4, 64]), [e4], [sp_tm])
                    ib = gr.ap[:, i, ic:ic + 4].unsqueeze(2).to_broadcast([128, 4, 64])
                    P.cp('dve', posi.ap[:, :].rearrange("p (h d) -> p h d", d=64), ib, [grb[i]], [posi])
                    P.ts('dve', negi.ap[:, :].rearrange("p (h d) -> p h d", d=64), ib, -1.0, None, ALU.mult, ALU.bypass,
                         [grb[i]], [negi])
                else:
                    P.tr(pf[3].ap[0:16, 256:384], gr.ap[:, i, dirn * 16:(dirn + 1) * 16], ident_f.ap[:],
                         [grb[i], ident_f], [pf[3]])
                    P.cp('act', rTa.ap[0:16, :], pf[3].ap[0:16, 256:384], [pf[3]], [rTa])
                    P.mm(pf[3].ap[:, 256:512], rTa.ap[0:17, :], wal.ap[0:17, dirn, :], True, True, [rTa, wal], [pf[3]])
                    P.act(e_tm.ap[:], pf[3].ap[:, 256:512], AF.Exp, [pf[3]], [e_tm], scale=-1.0)
                    P.act(sp_tm.ap[:], e_tm.ap[:], AF.Ln, [e_tm], [sp_tm], bias=1.0)
                P.ckpt("s1")
                pc, px = pf[4], pf[3]
                for j in range(2):
                    P.mm(pc.ap[:, j * 128:(j + 1) * 128], sp_tm.ap[:, j * 128:(j + 1) * 128], tri_cb.ap[:], True, True,
                         [sp_tm, tri_cb], [pc])
                if ml:
                    for j in range(2):
                        P.mm(pc.ap[:, 256 + j * 128:384 + j * 128], sp_tm.ap[:, j * 128:(j + 1) * 128], tri_cb.ap[:],
                             True, False, [sp_tm, tri_cb], [pc])
                        P.mm(pc.ap[:, 256 + j * 128:384 + j * 128], posi.ap[:, j * 128:(j + 1) * 128], ident_bf.ap[:],
                             False, True, [posi, ident_bf], [pc])
                P.mm(px.ap[:, 0:256], tri_xb.ap[:], sp_tm.ap[:], True, not ml, [tri_xb, sp_tm], [px])
                if ml:
                    P.mm(px.ap[:, 0:256], ident_bf.ap[:], negi.ap[:], False, True, [ident_bf, negi], [px])
                P.ckpt("s2")
                P.act(EqT.ap[:], pc.ap[:, 0:256], AF.Exp, [pc], [EqT], scale=-cdec)
                P.act(EkT.ap[:], pc.ap[:, 256:512] if ml else pc.ap[:, 0:256], AF.Exp, [pc], [EkT], scale=cdec)
                P.act(Ex.ap[:], px.ap[:, 0:256], AF.Exp, [px], [Ex], scale=-cdec)
                P.tt('dve', qp.ap[:], qT.ap[:, :, tsl], EqT.ap[:, :].rearrange("p (j t) -> p j t", t=128), ALU.mult,
                     [qkb[i], EqT], [qp])
                for h in range(4):
                    j, b0 = h // 2, (h % 2) * 64
                    P.tt('dve', kpz[h].ap[b0:b0 + 64, :], kT.ap[b0:b0 + 64, j, tsl], EkT.ap[b0:b0 + 64, j * 128:(j + 1) * 128],
                         ALU.mult, [qkb[i], EkT], [kpz[h]])
                P.tt('dve', kpp.ap[:], k_tm.ap[:, i, :], Ex.ap[:], ALU.mult, [kvb[i], Ex], [kpp])
                P.ckpt("s3")
                pS = pf[2]
                for h in range(4):
                    j, b0 = h // 2, (h % 2) * 64
                    P.mm(pS.ap[:, h * 128:(h + 1) * 128], kpz[h].ap[:], qp.ap[:, j, :], True, True, [kpz[h], qp], [pS])
                P.tt('dve', S_m.ap[:, :].rearrange("p (h t) -> p h t", t=128),
                     pS.ap[:, :].rearrange("p (h t) -> p h t", t=128),
                     tri_c.ap[:, :].unsqueeze(1).to_broadcast([128, 4, 128]), ALU.mult, [pS, tri_c], [S_m])

            def suffix(step, i):
                EqT, qp, kpp, S_m = EqT2[step % 2], qp2[step % 2], kpp2[step % 2], S_m2[step % 2]
                pO = pf[step % 2]
                for h in range(4):
                    j, b0 = h // 2, (h % 2) * 64
                    P.mm(pO.ap[:, h * 128:(h + 1) * 128], S_m.ap[:, h * 128:(h + 1) * 128], v_aug.ap[:, i, h * 128:(h + 1) * 128],
                         True, False, [S_m, kvb[i]], [pO])
                    P.mm(pO.ap[:, h * 128:(h + 1) * 128], qp.ap[:, j, :], St_b[h].ap[:], False, True, [qp, St_b[h]], [pO])
                P.ckpt("s5")
                pD = pf[5]
                for j in range(2):
                    P.mm(pD.ap[:, j * 256:(j + 1) * 256], kpp.ap[:, j * 128:(j + 1) * 128], v_aug.ap[:, i, j * 256:(j + 1) * 256],
                         True, True, [kpp, kvb[i]], [pD])
                for h in range(4):
                    j, b0 = h // 2, (h % 2) * 64
                    P.stt(St_f[j].ap[b0:b0 + 64, :], St_f[j].ap[b0:b0 + 64, :], EqT.ap[b0:b0 + 64, j * 128 + dcol:j * 128 + dcol + 1],
                          pD.ap[b0:b0 + 64, j * 256 + (h % 2) * 128:j * 256 + (h % 2) * 128 + 128], ALU.mult, ALU.add,
                          [St_f[j], EqT, pD], [St_f[j]])
                for h in range(4):
                    j, b0 = h // 2, (h % 2) * 64
                    P.cp('act', St_b[h].ap[b0:b0 + 64, :], St_f[j].ap[b0:b0 + 64, :], [St_f[j]], [St_b[h]])
                P.ckpt("s6")
                pO3 = pO.ap[:, 0:512].rearrange("p (h e) -> p h e", e=128)
                H3 = Hh.ap[:, i, :].rearrange("p (h d) -> p h d", d=64)
                if ml:
                    P.cp('dve', dn.ap[:, 0:4], pO3[:, :, 64], [pO], [dn])
                    P.stt(dn.ap[:, 0:4], dn.ap[:, 0:4], -1.0, dn.ap[:, 0:4], ALU.mult, ALU.max, [dn], [dn])
                    P.ts('dve', dn.ap[:, 0:4], dn.ap[:, 0:4], 1.0, None, ALU.max, ALU.bypass, [dn], [dn])
                    P.op('dve', lambda e, o=dn.ap[:, 4:8], a=dn.ap[:, 0:4]: e.reciprocal(o, a), [dn], [dn])
                    rb = dn.ap[:, 4:8].unsqueeze(2).to_broadcast([128, 4, 64])
                    if dirn == 0:
                        P.tt('dve', H3, pO3[:, :, 0:64], rb, ALU.mult, [pO, dn], [Hb[i]])
                    else:
                        P.tt('dve', tmpH.ap[:, :].rearrange("p (h d) -> p h d", d=64), pO3[:, :, 0:64], rb, ALU.mult,
                             [pO, dn], [tmpH])
                        P.tt('dve', Hh.ap[:, i, :], Hh.ap[:, i, :], tmpH.ap[:], ALU.add, [Hb[i], tmpH], [Hb[i]])
                else:
                    if dirn == 0:
                        P.cp('act', H3, pO3[:, :, 0:64], [pO], [Hb[i]])
                    else:
                        P.tt('dve', H3, pO3[:, :, 0:64], H3, ALU.add, [pO, Hb[i]], [Hb[i]])

            prefix(0, order[0])
            for step, i in enumerate(order):
                if step + 1 < len(order):
                    prefix(step + 1, order[step + 1])
                suffix(step, i)
        P.ckpt("scan")
        ncol = 512 if ml else 256
        wfin = load_w(l, base + 768, ncol)
        sig = P.sb("s_sig", [128, 256], F32)
        sgt = P.sb("s_sgt", [128, 256], F32)
        gs = P.sb("s_gs", [128, 256], F32)
        hh = P.sb("s_hh", [128, 256], F32)
        sq = P.sb("s_sq", [128, 256], F32)
        og = P.sb("s_og", [128, 256], BF16)
        for i in range(NT):
            if last and i < 2:
                continue
            pg = pf[i % 2]
            proj_tm(i, wfin, 0, ncol, pg.ap[:, 0:ncol], pg)
            if ml:
                P.act(sig.ap[:], pg.ap[:, 0:256], AF.Sigmoid, [pg], [sig])
                P.tt('dve', hh.ap[:], Hh.ap[:, i, :], sig.ap[:], ALU.mult, [Hb[i], sig], [hh])
                gate_ap = pg.ap[:, 256:512]
            else:
                P.cp('dve', hh.ap[:], Hh.ap[:, i, :], [Hb[i]], [hh])
                gate_ap = pg.ap[:, 0:256]
            P.act(sgt.ap[:], gate_ap, AF.Silu, [pg], [sgt])
            P.tt('dve', gs.ap[:], sgt.ap[:], normg.ap[:], ALU.mult, [sgt, normg], [gs])
            P.tt('dve', sq.ap[:], hh.ap[:], hh.ap[:], ALU.mult, [hh], [sq])
            P.op('dve', lambda e, o=dn.ap[:, 0:4], a=sq.ap[:, :].rearrange("p (h d) -> p h d", d=64): e.reduce_sum(o, a, AX.X),
                 [sq], [dn])
            P.act(dn.ap[:, 4:8], dn.ap[:, 0:4], AF.Sqrt, [dn], [dn], bias=EPS, scale=1.0 / 64)
            P.op('dve', lambda e, o=dn.ap[:, 0:4], a=dn.ap[:, 4:8]: e.reciprocal(o, a), [dn], [dn])
            P.tt('dve', sq.ap[:, :].rearrange("p (h d) -> p h d", d=64), hh.ap[:, :].rearrange("p (h d) -> p h d", d=64),
                 dn.ap[:, 0:4].unsqueeze(2).to_broadcast([128, 4, 64]), ALU.mult, [hh, dn], [sq])
            P.tt('dve', og.ap[:], sq.ap[:], gs.ap[:], ALU.mult, [sq, gs], [og])
            pt = pT[i % 2]
            for c2 in range(2):
                P.tr(pt.ap[:, c2 * 128:(c2 + 1) * 128], og.ap[:, c2 * 128:(c2 + 1) * 128], ident_bf.ap[:], [og, ident_bf], [pt])
            P.cp('act', mixT.ap[:, mc0:mc0 + 2, i * 128:(i + 1) * 128], pt.ap[:, 0:256].rearrange("p (c t) -> p c t", t=128),
                 [pt], [mixb[mg][i]])
        P.fence()
    P.es = L["es"]


BRANCH_BUILDERS["mlstm"] = lambda L: branch_scan(L, "mlstm")
BRANCH_BUILDERS["gla"] = lambda L: branch_scan(L, "gla")


def hcol(i):
    return 2 + i * 128 if i < 2 else 262 + (i - 2) * 128


def branch_hyena(L):
    P, nc, l, last, hy = L["P"], L["nc"], L["l"], L["last"], L["hy"]
    hT, hTb, mixT, mixb, pT, pf = L["hT"], L["hTb"], L["mixT"], L["mixb"], L["pT"], L["pf"]
    ident_bf, ident_f = L["ident_bf"], L["ident_f"]
    load_w, proj_fm = L["load_w"], L["proj_fm"]
    PI = 3.1415925
    TWO_PI = 6.283185307179586
    W = 2312
    with ExitStack() as es2:
        P.es = es2
        RC = P.sb("h_RC", [128, 16, 512], BF16)
        RS = P.sb("h_RS", [128, 16, 512], BF16)
        RCc = P.sb("h_RCc", [128, 2, 512], BF16)
        RSc = P.sb("h_RSc", [128, 2, 512], BF16)
        uT = P.sb("h_uT", [128, 2, W], BF16)
        x0c = P.sb("h_x0c", [128, 2, W], BF16)
        sgT = P.sb("h_sgT", [128, 2, W], BF16)
        vcol = P.sb("h_vcol", [128, 26], F32)
        fb = P.sb("h_fb", [64, 6], F32)
        wfl = P.sb("h_wfl", [128, 16], F32)
        wfc = P.sb("h_wfc", [128, 2], F32)
        ntl = P.sb("h_ntl", [128, 16], F32)
        ntc = P.sb("h_ntc", [128, 2], F32)
        delt = P.sb("h_delt", [128, 256], F32)
        P.dma('sp', wfl.ap[:], hy["wf_l"], writes=[wfl])
        P.dma('sp', wfc.ap[:], hy["wf_c"], writes=[wfc])
        P.dma('sp', ntl.ap[:], hy["ntcol_l"], writes=[ntl])
        P.dma('sp', ntc.ap[:], hy["ntcol_c"], writes=[ntc])
        P.dma('sp', delt.ap[:], hy["deltas"].partition_broadcast(128), writes=[delt])
        do_ctx = not last

        with ExitStack() as es3:
            P.es = es3
            vr = P.sb("h_vr", [26, 128], F32)
            fr = P.sb("h_fr", [4, 64], F32)
            P.dma('sp', vr.ap[0:18, :], hy["hyena_conv_w"][l].rearrange("j (c p) -> (j c) p", p=128), writes=[vr])
            P.dma('sp', vr.ap[18:24, :], hy["hyena_conv_b"][l].rearrange("(c p) -> c p", p=128), writes=[vr])
            P.dma('sp', vr.ap[24:26, :], hy["hyena_d"][l].rearrange("(c p) -> c p", p=128), writes=[vr])
            P.dma('sp', fr.ap[0:1, :], hy["hyena_b1"][l:l + 1, :], writes=[fr])
            P.dma('sp', fr.ap[1:2, :], hy["hyena_b2"][l:l + 1, :], writes=[fr])
            P.dma('sp', fr.ap[2:4, :], hy["hyena_freq"][l], writes=[fr])
            P.tr(pf[0].ap[:, 0:26], vr.ap[:], ident_f.ap[0:26, 0:26], [vr, ident_f], [pf[0]])
            P.cp('dve', vcol.ap[:], pf[0].ap[:, 0:26], [pf[0]], [vcol])
            P.tr(pf[1].ap[0:64, 0:4], fr.ap[:], ident_f.ap[0:4, 0:4], [fr, ident_f], [pf[1]])
            P.cp('dve', fb.ap[:, 0:4], pf[1].ap[0:64, 0:4], [pf[1]], [fb])
            P.tt('dve', fb.ap[:, 4:6], fb.ap[:, 0:2], fb.ap[:, 2:4], ALU.mult, [fb], [fb])

            P.ckpt("hv")
            zTl = P.sb("h_zTl", [33, 2048], F32)
            zTc = P.sb("h_zTc", [33, 256], F32)
            w1s = P.sb("h_w1", [33, 64], F32)
            w2s = P.sb("h_w2", [64, 64], F32)
            w3a = P.sb("h_w3a", [65, 512], F32)
            arg = P.sb("h_arg", [64, 512], F32)
            t1 = P.sb("h_t1", [64, 512], F32)
            t2 = P.sb("h_t2", [64, 512], F32)
            h1 = P.sb("h_h1", [64, 512], F32)
            h2 = P.sb("h_h2", [65, 512], F32)
            win = P.sb("h_win", [128, 256], F32)
            hf = P.sb("h_hf", [128, 256], F32)
            hb = P.sb("h_hb", [128, 256], F32)
            P.dma('sp', zTl.ap[:], hy["zT_l"], writes=[zTl])
            P.dma('sp', zTc.ap[:], hy["zT_c"], writes=[zTc])
            P.dma('sp', w1s.ap[:], hy["hyena_w1"][l], writes=[w1s])
            P.dma('sp', w2s.ap[:], hy["hyena_w2"][l], writes=[w2s])
            P.dma('sp', w3a.ap[0:64, :], hy["hyena_w3"][l], writes=[w3a])
            P.dma('sp', w3a.ap[64:65, :], hy["hyena_b3"][l:l + 1, :], writes=[w3a])
            P.op('pool', lambda e: e.memset(h2.ap[:], 1.0), writes=[h2])

            def sin_layer(ps_buf, ps_ap, fc_, fbc_, out_ap, outbuf, nn):
                P.act(arg.ap[:, 0:nn], ps_ap, AF.Identity, [ps_buf, fb], [arg], scale=fb.ap[:, fc_:fc_ + 1],
                      bias=fb.ap[:, fbc_:fbc_ + 1])
                P.ts('dve', t1.ap[:, 0:nn], arg.ap[:, 0:nn], PI, TWO_PI, ALU.is_gt, ALU.mult, [arg], [t1])
                P.ts('dve', t2.ap[:, 0:nn], arg.ap[:, 0:nn], -PI, TWO_PI, ALU.is_lt, ALU.mult, [arg], [t2])
                P.tt('dve', arg.ap[:, 0:nn], arg.ap[:, 0:nn], t1.ap[:, 0:nn], ALU.subtract, [arg, t1], [arg])
                P.tt('dve', arg.ap[:, 0:nn], arg.ap[:, 0:nn], t2.ap[:, 0:nn], ALU.add, [arg, t2], [arg])
                P.act(out_ap, arg.ap[:, 0:nn], AF.Sin, [arg], [outbuf])

            def mlp_block(zsrc, n0, nn, ntc_, RCd, RSd, tile0):
                P.mm(pf[0].ap[0:64, 0:nn], w1s.ap[:], zsrc.ap[:, n0:n0 + nn], True, True, [w1s, zsrc], [pf[0]])
                sin_layer(pf[0], pf[0].ap[0:64, 0:nn], 2, 4, h1.ap[:, 0:nn], h1, nn)
                P.mm(pf[1].ap[0:64, 0:nn], w2s.ap[:], h1.ap[:, 0:nn], True, True, [w2s, h1], [pf[1]])
                sin_layer(pf[1], pf[1].ap[0:64, 0:nn], 3, 5, h2.ap[0:64, 0:nn], h2, nn)
                for tt_ in range(nn // 128):
                    dt = tile0 + tt_
                    pp = pf[2 + tt_ % 2]
                    P.mm(pp.ap[:, 0:512], h2.ap[0:65, tt_ * 128:(tt_ + 1) * 128], w3a.ap[:], True, True, [h2, w3a], [pp])
                    P.act(win.ap[:], delt.ap[:], AF.Exp, [delt, ntc_], [win], scale=ntc_.ap[:, dt:dt + 1])
                    P.tt('dve', hf.ap[:], pp.ap[:, 0:256], win.ap[:], ALU.mult, [pp, win], [hf])
                    P.tt('dve', hb.ap[:], pp.ap[:, 256:512], win.ap[:], ALU.mult, [pp, win], [hb])
                    if dt == 0:
                        P.op('pool', lambda e: e.memset(hb.ap[0:1, :], 0.0), reads=[hb], writes=[hb])
                    P.tt('dve', RCd.ap[:, dt, 256:512], hf.ap[:], hb.ap[:], ALU.add, [hf, hb], [RCd])
                    P.tt('dve', RSd.ap[:, dt, 256:512], hf.ap[:], hb.ap[:], ALU.subtract, [hf, hb], [RSd])

            for blk in range(4):
                mlp_block(zTl, blk * 512, 512, ntl, RC, RS, blk * 4)
                P.ckpt("hm%d" % blk)
            if do_ctx:
                mlp_block(zTc, 0, 256, ntc, RCc, RSc, 0)
            P.fence()
        P.es = es2

        with ExitStack() as es3:
            P.es = es3
            L["alloc_wbuf"]()
            raw = P.sb("h_raw", [128, W], F32)
            cv = P.sb("h_cv", [128, W], F32)
            P.op('pool', lambda e: e.memset(raw.ap[:], 0.0), writes=[raw])
            wA = load_w(l, COL_H, 512)
            wB = load_w(l, COL_H + 512, 512)
            groups = [(0, 256), (256, 512), (768, 512), (1280, 512), (1792, 512)]
            cnt = 0
            for c6 in range(8):
                wsel = wA if c6 < 4 else wB
                coff = (c6 % 4) * 128
                for (t0, nt) in groups:
                    pp = pf[cnt % 2]
                    cnt += 1
                    proj_fm(wsel, coff, 128, t0, nt, pp.ap[:, 0:nt], pp)
                    c0 = 2 + t0 if t0 < 256 else t0 + 6
                    if c6 < 6:
                        P.cp('act', raw.ap[:, c0:c0 + nt], pp.ap[:, 0:nt], [pp], [raw])
                    else:
                        P.act(sgT.ap[:, c6 - 6, c0:c0 + nt], pp.ap[:, 0:nt], AF.Silu, [pp], [sgT])
                if c6 >= 6:
                    continue
                w0c, w1c, w2c = (vcol.ap[:, j * 6 + c6:j * 6 + c6 + 1] for j in range(3))
                bc = vcol.ap[:, 18 + c6:19 + c6]
                P.ts('dve', cv.ap[:, 1:W - 1], raw.ap[:, 1:W - 1], w1c, bc, ALU.mult, ALU.add, [raw, vcol], [cv])
                P.stt(cv.ap[:, 1:W - 1], raw.ap[:, 0:W - 2], w0c, cv.ap[:, 1:W - 1], ALU.mult, ALU.add, [raw, vcol, cv], [cv])
                if c6 < 2:
                    P.stt(uT.ap[:, c6, 1:W - 1], raw.ap[:, 2:W], w2c, cv.ap[:, 1:W - 1], ALU.mult, ALU.add, [raw, vcol, cv], [uT])
                elif c6 < 4:
                    P.stt(cv.ap[:, 1:W - 1], raw.ap[:, 2:W], w2c, cv.ap[:, 1:W - 1], ALU.mult, ALU.add, [raw, vcol, cv], [cv])
                    P.tt('dve', uT.ap[:, c6 - 2, 1:W - 1], uT.ap[:, c6 - 2, 1:W - 1], cv.ap[:, 1:W - 1], ALU.mult, [uT, cv], [uT])
                else:
                    P.stt(x0c.ap[:, c6 - 4, 1:W - 1], raw.ap[:, 2:W], w2c, cv.ap[:, 1:W - 1], ALU.mult, ALU.add,
                          [raw, vcol, cv], [x0c])
            P.fence()
        P.es = es2

        P.ckpt("hp")
        for i in range(NT):
            if i < 2 and not do_ctx:
                continue
            c = hcol(i)
            pt = pT[i % 2]
            for cj in range(2):
                P.tr(pt.ap[:, cj * 128:(cj + 1) * 128], uT.ap[:, cj, c:c + 128], ident_bf.ap[:], [uT, ident_bf], [pt])
            dC, dS, dt = (RCc, RSc, i) if i < 2 else (RC, RS, i - 2)
            P.cp('act', dC.ap[:, dt, 0:256], pt.ap[:, 0:256], [pt], [dC])
            P.cp('dve', dS.ap[:, dt, 0:256], pt.ap[:, 0:256], [pt], [dS])

        P.ckpt("ht")
        Ysp = P.sb("h_Y", [128, 16, 512], BF16)
        Yc = P.sb("h_Yc", [128, 2, 512], BF16)
        kre = P.sb("h_kre", [128, 256], F32)
        kim = P.sb("h_kim", [128, 256], F32)
        ta = [P.sb("h_ta%d" % i, [128, 256], F32) for i in range(2)] * 2
        fin = P.sb("h_fin", [128, 512], F32)
        cbuf = [P.sb("h_cb%d" % i, [128, 16, 128], BF16) for i in range(2)]
        sbuf_ = [P.sb("h_sb%d" % i, [128, 16, 128], BF16) for i in range(2)]
        ibc = [P.sb("h_ibc%d" % i, [128, 512], BF16) for i in range(2)]
        ibs = [P.sb("h_ibs%d" % i, [128, 512], BF16) for i in range(2)]
        rot = [0]

        def dft_conv(RCx, RSx, Yx, nT, tag, wfx, tgroups, col_base, tok_base):
            Ct, St, Cn, Sn = hy["Ct_" + tag], hy["St_" + tag], hy["Cn_" + tag], hy["Sn_" + tag]
            for fc in range(nT):
                b = fc % 2
                P.dma('sp', cbuf[b].ap[:, 0:nT, :], Ct[fc], writes=[cbuf[b]])
                P.dma('sp', sbuf_[b].ap[:, 0:nT, :], St[fc], writes=[sbuf_[b]])
                pc, ps_ = pf[2 * b], pf[2 * b + 1]
                for tc in range(nT):
                    P.mm(pc.ap[:, 0:512], cbuf[b].ap[:, tc, :], RCx.ap[:, tc, :], tc == 0, tc == nT - 1, [cbuf[b], RCx], [pc])
                for tc in range(nT):
                    P.mm(ps_.ap[:, 0:512], sbuf_[b].ap[:, tc, :], RSx.ap[:, tc, :], tc == 0, tc == nT - 1, [sbuf_[b], RSx], [ps_])
                P.act(kre.ap[:], pc.ap[:, 256:512], AF.Identity, [pc, wfx], [kre], scale=wfx.ap[:, fc:fc + 1])
                P.act(kim.ap[:], ps_.ap[:, 256:512], AF.Identity, [ps_, wfx], [kim], scale=wfx.ap[:, fc:fc + 1])
                P.tt('dve', ta[0].ap[:], pc.ap[:, 0:256], kre.ap[:], ALU.mult, [pc, kre], [ta[0]])
                P.tt('dve', ta[1].ap[:], ps_.ap[:, 0:256], kim.ap[:], ALU.mult, [ps_, kim], [ta[1]])
                P.tt('dve', Yx.ap[:, fc, 0:256], ta[0].ap[:], ta[1].ap[:], ALU.subtract, [ta[0], ta[1]], [Yx])
                P.tt('dve', ta[2].ap[:], pc.ap[:, 0:256], kim.ap[:], ALU.mult, [pc, kim], [ta[2]])
                P.tt('dve', ta[3].ap[:], ps_.ap[:, 0:256], kre.ap[:], ALU.mult, [ps_, kre], [ta[3]])
                P.tt('dve', Yx.ap[:, fc, 256:512], ta[2].ap[:], ta[3].ap[:], ALU.add, [ta[2], ta[3]], [Yx])
            P.ckpt("hf" + tag)
            for gi, (t0, nt) in enumerate(tgroups):
                py = [pf[4], pf[5]]
                for fc in range(nT):
                    bb = rot[0] % 2
                    rot[0] += 1
                    P.dma('sp', ibc[bb].ap[:, 0:nt], Cn[fc * 128:(fc + 1) * 128, t0:t0 + nt], writes=[ibc[bb]])
                    P.dma('sp', ibs[bb].ap[:, 0:nt], Sn[fc * 128:(fc + 1) * 128, t0:t0 + nt], writes=[ibs[bb]])
                    for cj in range(2):
                        P.mm(py[cj].ap[:, 0:nt], Yx.ap[:, fc, cj * 128:(cj + 1) * 128], ibc[bb].ap[:, 0:nt], fc == 0, False,
                             [Yx, ibc[bb]], [py[cj]])
                        P.mm(py[cj].ap[:, 0:nt], Yx.ap[:, fc, 256 + cj * 128:384 + cj * 128], ibs[bb].ap[:, 0:nt], False,
                             fc == nT - 1, [Yx, ibs[bb]], [py[cj]])
                for cj in range(2):
                    cs = slice(col_base + t0, col_base + t0 + nt)
                    tk0 = tok_base + t0
                    tiles = list(range(tk0 // 128, (tk0 + nt) // 128))
                    P.stt(fin.ap[:, 0:nt], uT.ap[:, cj, cs], vcol.ap[:, 24 + cj:25 + cj], py[cj].ap[:, 0:nt], ALU.mult, ALU.add,
                          [uT, vcol, py[cj]], [fin])
                    P.tt('dve', fin.ap[:, 0:nt], fin.ap[:, 0:nt], x0c.ap[:, cj, cs], ALU.mult, [fin, x0c], [fin])
                    P.tt('dve', mixT.ap[:, 2 + cj, tk0:tk0 + nt], fin.ap[:, 0:nt], sgT.ap[:, cj, cs], ALU.mult, [fin, sgT],
                         [mixb[1][t] for t in tiles])

        dft_conv(RC, RS, Ysp, 16, "l", wfl, [(0, 512), (512, 512), (1024, 512), (1536, 512)], 262, 256)
        if do_ctx:
            dft_conv(RCc, RSc, Yc, 2, "c", wfc, [(0, 256)], 2, 0)
        P.fence()
    P.es = L["es"]


BRANCH_BUILDERS["hyena"] = branch_hyena


def kernel(**inputs):
    nc = build(n_layers=4)
    maps = make_in_maps(inputs)
    res = run_bass_kernel_spmd(nc, maps, core_ids=list(range(8)))
    out = np.stack([np.asarray(res.results[b]["y"]).astype(np.float32) for b in range(8)], 0)
    return out
```
